# Optimizing a Trainium2 kernel written in Bass

```python
import jax
import jax.numpy as jnp
from jax import lax
import numpy as np

D_MODEL = 1024
BATCH = 4
SEQ = 4096
DEPTH = 2

GRID_W = 64
CTX_LEN = 256
HEAD_DIM = 64
N_Q_HEADS = 8
N_KV_HEADS = 2
ATTN_WIDTH = N_Q_HEADS * HEAD_DIM
KV_WIDTH = 2 * N_KV_HEADS * HEAD_DIM
CONV_CH = D_MODEL - ATTN_WIDTH
CONF_K = 31
ROPE_AXIS_DIM = HEAD_DIM // 2
ROPE_THETA = 10000.0
Q_BLOCK = 128
EVEN_IN = ATTN_WIDTH + KV_WIDTH + 2 * CONV_CH
SC_K = 3
N_EXPERTS = 16
EC_CAPACITY = 2
EXPERT_FF = 1024
N_MOD = 6
EPS = 1e-6
N_EVEN = (DEPTH + 1) // 2
N_ODD = DEPTH // 2

kernel_name = "hybrid_attn_conformer_shortconv_ec_moe_dit"


def rms_norm(x, g):
    xf = x.astype(jnp.float32)
    y = xf * lax.rsqrt(jnp.mean(xf * xf, axis=-1, keepdims=True) + EPS)
    return (y * g.astype(jnp.float32)).astype(x.dtype)


def layer_norm(x, g, b):
    xf = x.astype(jnp.float32)
    mu = jnp.mean(xf, axis=-1, keepdims=True)
    var = jnp.mean(jnp.square(xf - mu), axis=-1, keepdims=True)
    y = (xf - mu) * lax.rsqrt(var + EPS)
    return (y * g.astype(jnp.float32) + b.astype(jnp.float32)).astype(x.dtype)


def ada_mod(cvec, w, b):
    m = jax.nn.silu(cvec) @ w + b
    return m.reshape(cvec.shape[:-1] + (N_MOD, cvec.shape[-1]))


def modulate(xn, shift, scale):
    return xn * (1 + scale) + shift


def axial_rope_tables(n):
    rows = n // GRID_W
    row = jnp.repeat(jnp.arange(rows, dtype=jnp.float32), GRID_W)
    col = jnp.tile(jnp.arange(GRID_W, dtype=jnp.float32), rows)
    inv = ROPE_THETA ** (-jnp.arange(0, ROPE_AXIS_DIM, 2, dtype=jnp.float32) / ROPE_AXIS_DIM)
    ang = jnp.concatenate([row[:, None] * inv, col[:, None] * inv], axis=-1)
    return jnp.cos(ang)[None, :, None, :], jnp.sin(ang)[None, :, None, :]


def apply_rope(x, cos, sin):
    half = x.shape[-1] // 2
    xf = x.astype(jnp.float32)
    x1, x2 = xf[..., :half], xf[..., half:]
    return jnp.concatenate([x1 * cos - x2 * sin, x2 * cos + x1 * sin], axis=-1).astype(x.dtype)


def depthwise_conv(x, w):
    k = w.shape[0]
    return lax.conv_general_dilated(
        x, w[:, None, :].astype(x.dtype), window_strides=(1,), padding=[(k // 2, k // 2)],
        dimension_numbers=("NWC", "WIO", "NWC"), feature_group_count=x.shape[-1])


def split_kv(kv, k_g):
    bsz, n, _ = kv.shape
    half = KV_WIDTH // 2
    k = kv[..., :half].reshape(bsz, n, N_KV_HEADS, HEAD_DIM)
    v = kv[..., half:].reshape(bsz, n, N_KV_HEADS, HEAD_DIM)
    return rms_norm(k, k_g), v


def block_attention(q, k, v):
    bsz, n, hq, hd = q.shape
    grp = hq // N_KV_HEADS
    nb = n // Q_BLOCK
    qb = q.reshape(bsz, nb, Q_BLOCK, N_KV_HEADS, grp, hd).transpose(1, 0, 2, 3, 4, 5)
    scale = HEAD_DIM ** -0.5

    def one_block(qblk):
        s = jnp.einsum("bqkgd,btkd->bkgqt", qblk, k).astype(jnp.float32) * scale
        p = jax.nn.softmax(s, axis=-1).astype(v.dtype)
        return jnp.einsum("bkgqt,btkd->bqkgd", p, v)

    o = lax.map(one_block, qb)
    return o.transpose(1, 0, 2, 3, 4, 5).reshape(bsz, n, hq * hd)


def context_kv(hcn, w_in, k_g):
    return split_kv(hcn @ w_in[:, ATTN_WIDTH:ATTN_WIDTH + KV_WIDTH], k_g)


def even_mixer(hm, w_in, q_g, k_g, conv_w, conv_b, ln_g, ln_b, w_out, rope, kv_prefix):
    bsz, n, _ = hm.shape
    proj = hm @ w_in
    q = rms_norm(proj[..., :ATTN_WIDTH].reshape(bsz, n, N_Q_HEADS, HEAD_DIM), q_g)
    k, v = split_kv(proj[..., ATTN_WIDTH:ATTN_WIDTH + KV_WIDTH], k_g)
    if rope is not None:
        q = apply_rope(q, *rope)
        k = apply_rope(k, *rope)
    own_kv = (k, v)
    if kv_prefix is not None:
        k = jnp.concatenate([kv_prefix[0], k], axis=1)
        v = jnp.concatenate([kv_prefix[1], v], axis=1)
    attn = block_attention(q, k, v)
    c0 = ATTN_WIDTH + KV_WIDTH
    u = proj[..., c0:c0 + CONV_CH]
    gate = proj[..., c0 + CONV_CH:]
    g = u * jax.nn.sigmoid(gate)
    g = depthwise_conv(g, conv_w) + conv_b
    g = jax.nn.silu(layer_norm(g, ln_g, ln_b))
    return jnp.concatenate([attn, g], axis=-1) @ w_out, own_kv


def short_conv_mixer(hm, w_in, conv_w, w_out):
    proj = hm @ w_in
    d = hm.shape[-1]
    b_gate, c_gate, xv = proj[..., :d], proj[..., d:2 * d], proj[..., 2 * d:]
    y = depthwise_conv(c_gate * xv, conv_w)
    return (b_gate * y) @ w_out


def ec_moe(h, w_r, w_gate, w_up, w_down):
    bsz, n, _ = h.shape
    cap = max(1, EC_CAPACITY * n // N_EXPERTS)
    aff = jax.nn.softmax(jnp.einsum("bnd,de->bne", h, w_r).astype(jnp.float32), axis=-1)
    gates, idx = lax.top_k(jnp.swapaxes(aff, 1, 2), cap)
    bidx = jnp.broadcast_to(jnp.arange(bsz)[:, None, None], idx.shape)
    xs = h[bidx, idx]
    hid = jax.nn.silu(jnp.einsum("becd,edf->becf", xs, w_gate)) * jnp.einsum("becd,edf->becf", xs, w_up)
    ys = jnp.einsum("becf,efd->becd", hid, w_down) * gates[..., None].astype(h.dtype)
    return jnp.zeros_like(h).at[bidx, idx].add(ys)


def setup_inputs(seed: int = 0) -> dict:
    key = jax.random.key(seed)
    ks = iter(jax.random.split(key, 32))

    def nrm(shape, s):
        return jax.random.normal(next(ks), shape, jnp.float32) * s

    def gain(shape):
        return 1.0 + nrm(shape, 0.1)

    L, D, E, F = DEPTH, D_MODEL, N_EXPERTS, EXPERT_FF
    return {
        "x": nrm((BATCH, SEQ, D), 1.0),
        "c": nrm((BATCH, D), 1.0),
        "ctx": nrm((BATCH, CTX_LEN, D), 1.0),
        "c_ctx": nrm((D,), 1.0),
        "ada_w": nrm((L, D, N_MOD * D), 0.5 * D ** -0.5),
        "ada_b": nrm((L, N_MOD * D), 0.1),
        "norm1_g": gain((L, D)),
        "norm2_g": gain((L, D)),
        "ev_w_in": nrm((N_EVEN, D, EVEN_IN), D ** -0.5),
        "ev_q_g": gain((N_EVEN, HEAD_DIM)),
        "ev_k_g": gain((N_EVEN, HEAD_DIM)),
        "ev_conv_w": nrm((N_EVEN, CONF_K, CONV_CH), CONF_K ** -0.5),
        "ev_conv_b": nrm((N_EVEN, CONV_CH), 0.02),
        "ev_ln_g": gain((N_EVEN, CONV_CH)),
        "ev_ln_b": nrm((N_EVEN, CONV_CH), 0.02),
        "ev_w_out": nrm((N_EVEN, ATTN_WIDTH + CONV_CH, D), (ATTN_WIDTH + CONV_CH) ** -0.5),
        "sc_w_in": nrm((N_ODD, D, 3 * D), D ** -0.5),
        "sc_conv_w": nrm((N_ODD, SC_K, D), SC_K ** -0.5),
        "sc_w_out": nrm((N_ODD, D, D), D ** -0.5),
        "moe_w_r": nrm((L, D, E), D ** -0.5),
        "moe_w_gate": nrm((L, E, D, F), D ** -0.5),
        "moe_w_up": nrm((L, E, D, F), D ** -0.5),
        "moe_w_down": nrm((L, E, F, D), F ** -0.5),
        "final_g": gain((D,)),
    }


def reference(x, c, ctx, c_ctx, ada_w, ada_b, norm1_g, norm2_g,
              ev_w_in, ev_q_g, ev_k_g, ev_conv_w, ev_conv_b, ev_ln_g, ev_ln_b, ev_w_out,
              sc_w_in, sc_conv_w, sc_w_out,
              moe_w_r, moe_w_gate, moe_w_up, moe_w_down, final_g):
    rope = axial_rope_tables(x.shape[1])
    h, hc = x, ctx
    for i in range(DEPTH):
        is_even = i % 2 == 0
        ctx_later = any(j % 2 == 0 for j in range(i + 1, DEPTH))
        m = ada_mod(c, ada_w[i], ada_b[i])[:, None]
        hn = modulate(rms_norm(h, norm1_g[i]), m[:, :, 0], m[:, :, 1])
        if is_even or ctx_later:
            mc = ada_mod(c_ctx, ada_w[i], ada_b[i])
            hcn = modulate(rms_norm(hc, norm1_g[i]), mc[0], mc[1])
        if is_even:
            e = i // 2
            prm = (ev_w_in[e], ev_q_g[e], ev_k_g[e], ev_conv_w[e], ev_conv_b[e],
                   ev_ln_g[e], ev_ln_b[e], ev_w_out[e])
            if ctx_later:
                ctx_out, kv_ctx = even_mixer(hcn, *prm, None, None)
                hc = hc + mc[2] * ctx_out
            else:
                kv_ctx = context_kv(hcn, ev_w_in[e], ev_k_g[e])
            lat_out, _ = even_mixer(hn, *prm, rope, kv_ctx)
            h = h + m[:, :, 2] * lat_out
        else:
            o = i // 2
            prm = (sc_w_in[o], sc_conv_w[o], sc_w_out[o])
            h = h + m[:, :, 2] * short_conv_mixer(hn, *prm)
            if ctx_later:
                hc = hc + mc[2] * short_conv_mixer(hcn, *prm)
        moe = (moe_w_r[i], moe_w_gate[i], moe_w_up[i], moe_w_down[i])
        h = h + m[:, :, 5] * ec_moe(modulate(rms_norm(h, norm2_g[i]), m[:, :, 3], m[:, :, 4]), *moe)
        if ctx_later:
            hc = hc + mc[5] * ec_moe(modulate(rms_norm(hc, norm2_g[i]), mc[3], mc[4]), *moe)
    return rms_norm(h, final_g)
```

```python
import contextlib
import numpy as np
import concourse.bass as bass
import concourse.mybir as mybir
from concourse.bass_utils import run_bass_kernel_spmd

F32 = mybir.dt.float32
BF16 = mybir.dt.bfloat16
U32 = mybir.dt.uint32
I32 = mybir.dt.int32
ALU = mybir.AluOpType
AF = mybir.ActivationFunctionType
AX = mybir.AxisListType

ENGS = ["pe", "dve", "act", "pool", "sp"]
CENGS = ["pe", "dve", "act", "pool"]
NDS = 24
NDS_HW = 16
SAME_ENGINE_SYNC = True

D = 1024
S = 4096
CTX = 256
NT = S // 128
NKT = NT + 2
EPS = 1e-6
NE = 16
CAP = 512
RW = 1072


class Buf:
    def __init__(self, name):
        self.name = name
        self.w = None
        self.r = {}


class Sems:
    def __init__(self, nc, es):
        self.esem = {e: es.enter_context(nc.semaphore(f"s_{e}")) for e in CENGS}
        self.ecnt = {e: 0 for e in CENGS}
        self.dsem = [es.enter_context(nc.semaphore(f"s_d{i}")) for i in range(NDS)]
        self.dcnt = [0] * NDS
        self.dnext = 0
        self.dnext_sw = 0
        self.waited = {e: {} for e in ENGS}


_SEMS = [None]


class Phase:
    def __init__(self, nc, name):
        self.nc = nc
        self.name = name
        sm = _SEMS[0]
        self.sm = sm
        self.q = {e: [] for e in ENGS}
        self.esem = sm.esem
        self.ecnt = sm.ecnt
        self.pending = {e: False for e in CENGS}
        self.dsem = sm.dsem
        self.dcnt = sm.dcnt
        self.waited = sm.waited

    def _sem(self, key):
        return self.esem[key[1]] if key[0] == "e" else self.dsem[key[1]]

    def _wait(self, engine, tok, force=False):
        if tok is None:
            return
        key, val = tok
        if not force and key[0] == "e" and key[1] == engine and (engine == "pe" or not SAME_ENGINE_SYNC):
            return
        if self.waited[engine].get(key, 0) >= val:
            return
        self.waited[engine][key] = val
        sem = self._sem(key)
        self.q[engine].append(lambda eng, sem=sem, val=val: eng.wait_ge(sem, val))

    def _deps(self, engine, reads, writes):
        for b in reads:
            self._wait(engine, b.w)
        for b in writes:
            self._wait(engine, b.w)
            for t in b.r.values():
                self._wait(engine, t)

    def _update(self, tok, reads, writes):
        for b in reads:
            b.r[tok[0]] = tok
        for b in writes:
            b.w = tok
            b.r = {}

    def op(self, engine, fn, reads=(), writes=(), signal=True):
        self._deps(engine, reads, writes)
        if signal:
            self.ecnt[engine] += 1
            tok = (("e", engine), self.ecnt[engine])
            sem = self.esem[engine]
            self.q[engine].append(lambda eng, fn=fn, sem=sem: fn(eng).then_inc(sem, 1))
            self.pending[engine] = False
        else:
            tok = (("e", engine), self.ecnt[engine] + 1)
            self.q[engine].append(lambda eng, fn=fn: fn(eng))
            self.pending[engine] = True
        self._update(tok, reads, writes)
        return tok

    def dma(self, queue, fn, reads=(), writes=()):
        self._deps(queue, reads, writes)
        sm = self.sm
        if queue == "pool":
            i = NDS_HW + sm.dnext_sw
            sm.dnext_sw = (sm.dnext_sw + 1) % (NDS - NDS_HW)
        else:
            i = sm.dnext
            sm.dnext = (sm.dnext + 1) % NDS_HW
        if self.dcnt[i] > 0:
            self._wait(queue, (("d", i), self.dcnt[i]))
        self.dcnt[i] += 16
        tok = (("d", i), self.dcnt[i])
        sem = self.dsem[i]
        self.q[queue].append(lambda eng, fn=fn, sem=sem: fn(eng).then_inc(sem, 16))
        self._update(tok, reads, writes)
        return tok

    def finish(self):
        nc = self.nc
        for e in CENGS:
            assert not self.pending[e], f"engine {e} has pending unsignaled ops in {self.name}"
        for e in ENGS:
            for e2 in CENGS:
                if self.ecnt[e2] > 0:
                    self._wait(e, (("e", e2), self.ecnt[e2]), force=True)
            for i in range(NDS):
                if self.dcnt[i] > 0:
                    self._wait(e, (("d", i), self.dcnt[i]), force=True)
        q = self.q
        with nc.Block() as block:
            @block.tensor
            def _(eng):
                for f in q["pe"]:
                    f(eng)

            @block.vector
            def _(eng):
                for f in q["dve"]:
                    f(eng)

            @block.scalar
            def _(eng):
                for f in q["act"]:
                    f(eng)

            @block.gpsimd
            def _(eng):
                for f in q["pool"]:
                    f(eng)

            @block.sync
            def _(eng):
                for f in q["sp"]:
                    f(eng)


class Ring:
    def __init__(self, items):
        self.items = items
        self.i = 0

    def next(self):
        it = self.items[self.i % len(self.items)]
        self.i += 1
        return it


def v3(ap, d):
    return ap.rearrange("p (h d) -> p h d", d=d)


class Kern:
    def __init__(self, nc, debug=None, stop=None):
        self.nc = nc
        self.debug = debug or []
        self.stop = stop
        self.dbg = {}
        self.uid = 0

    def sb(self, es, name, shape, dt):
        self.uid += 1
        return es.enter_context(self.nc.sbuf_tensor(f"{name}_{self.uid}", shape, dt))

    def ps(self, es, name, shape, dt):
        self.uid += 1
        return es.enter_context(self.nc.psum_tensor(f"{name}_{self.uid}", shape, dt))

    def rb(self, es, name, shape, dt, n):
        return Ring([(self.sb(es, f"{name}{i}", shape, dt), Buf(f"{name}{i}")) for i in range(n)])

    def rps(self, es, name, shape, dt, n):
        return Ring([(self.ps(es, f"{name}{i}", shape, dt), Buf(f"{name}{i}")) for i in range(n)])

    def dram_in(self, name, shape, dt=F32):
        return self.nc.dram_tensor(name, list(shape), dt, kind="ExternalInput").ap()

    def dram_out(self, name, shape, dt=F32):
        return self.nc.dram_tensor(name, list(shape), dt, kind="ExternalOutput").ap()

    def dram_tmp(self, name, shape, dt=F32):
        return self.nc.dram_tensor(name, list(shape), dt, kind="Internal").ap()

    def dbg_out(self, name, shape, dt=F32):
        if name in self.debug:
            self.dbg[name] = self.dram_out("dbg_" + name, shape, dt)
            return self.dbg[name]
        return None

    def declare(self):
        self.x = self.dram_in("x", [S, D])
        self.ctx = self.dram_in("ctx", [CTX, D])
        self.cT = self.dram_in("cT", [128, 8, 2])
        self.ada_w = self.dram_in("ada_w", [2, D, 6 * D])
        self.ada_b = self.dram_in("ada_b", [2, 1, 6 * D])
        self.g1T = self.dram_in("g1T", [2, 128, 8])
        self.g2row = self.dram_in("g2row", [2, 1, D])
        self.ev_w_in = self.dram_in("ev_w_in", [D, 1792])
        self.ev_w_out = self.dram_in("ev_w_out", [D, D])
        self.qkg = self.dram_in("qkg", [1, 640])
        self.qg = self.dram_in("qg", [1, 64])
        self.kg = self.dram_in("kg", [1, 64])
        self.conv_wT = self.dram_in("conv_wT", [128, 4, 31])
        self.conv_vec = self.dram_in("conv_vec", [128, 3, 4])
        self.rope_cos = self.dram_in("rope_cos", [128, NT, 32])
        self.rope_sin = self.dram_in("rope_sin", [128, NT, 32])
        self.sc_w_in = self.dram_in("sc_w_in", [D, 3 * D])
        self.sc_conv_wT = self.dram_in("sc_conv_wT", [128, 8, 3])
        self.sc_w_out = self.dram_in("sc_w_out", [D, D])
        self.w_r = self.dram_in("w_r", [2, 128, 8, NE])
        self.w_gate = self.dram_in("w_gate", [2, NE, D, D])
        self.w_up = self.dram_in("w_up", [2, NE, D, D])
        self.w_down = self.dram_in("w_down", [2, NE, D, D])
        self.final_g = self.dram_in("final_g", [1, D])
        self.out = self.dram_out("out", [S, D])
        self.hbuf = self.dram_tmp("hbuf", [S, D])
        self.hbuf2 = self.dram_tmp("hbuf2", [S, D])
        self.acc = self.dram_tmp("acc", [S, D])
        self.h2aug = self.dram_tmp("h2aug", [S, RW], BF16)

    def consts(self, es):
        nc = self.nc
        self.ident_f = self.sb(es, "ident_f", [128, 128], F32)
        self.ident_b = self.sb(es, "ident_b", [128, 128], BF16)
        self.ones_b = self.sb(es, "ones_b", [128, 128], BF16)
        self.ones_f = self.sb(es, "ones_f", [128, 128], F32)
        self.ustr_b = self.sb(es, "ustr_b", [128, 128], BF16)
        self.sel2 = self.sb(es, "sel2", [2, 128], F32)
        self.iota512 = self.sb(es, "iota512", [128, 512], mybir.dt.float16)
        self.tokval = self.sb(es, "tokval", [128, NT, 2], BF16)
        self.mhalf = self.sb(es, "mhalf", [128, 16], F32)
        self.negC = self.sb(es, "negC", [128, 1], F32)
        self.zeros = self.sb(es, "zeros", [128, 1024], F32)
        self.mT = self.sb(es, "mT", [128, 48, 2], F32)
        self.A1T = self.sb(es, "A1T", [128, 8, 2], F32)
        self.G1bc = self.sb(es, "G1bc", [128, D], F32)
        self.A2bc = self.sb(es, "A2bc", [128, D], F32)
        self.B2bc = self.sb(es, "B2bc", [128, D], F32)
        self.G2bc = self.sb(es, "G2bc", [128, D], F32)
        self.wr_b = self.sb(es, "wr_b", [128, 8, NE], BF16)
        self.affTM = self.sb(es, "affTM", [128, NT, NE], F32)
        with contextlib.ExitStack() as es2:
            ii = self.sb(es2, "ii", [128, 512], I32)
            i2 = self.sb(es2, "i2", [128, NT], I32)
            i3 = self.sb(es2, "i3", [128, 1], I32)
            uf = self.sb(es2, "uf", [128, 128], F32)
            gq = self.sb(es2, "gq", [128, 128], F32)
            mx = self.sb(es2, "mx", [128, 2], F32)
            ph = Phase(nc, "const")
            B = Buf("c")
            W = dict(reads=[B], writes=[B])
            ph.op("pool", lambda e: e.memset(self.ident_f[:], 0.0), **W)
            ph.op("pool", lambda e: e.affine_select(out=self.ident_f[:], in_=self.ident_f[:], pattern=[[-1, 128]],
                                                    compare_op=ALU.not_equal, fill=1.0, base=0, channel_multiplier=1), **W)
            ph.op("dve", lambda e: e.tensor_copy(out=self.ident_b[:], in_=self.ident_f[:]), **W)
            ph.op("dve", lambda e: e.memset(self.ones_b[:], 1.0), **W)
            ph.op("dve", lambda e: e.memset(self.ones_f[:], 1.0), **W)
            ph.op("pool", lambda e: e.memset(uf[:], 0.0), **W)
            ph.op("pool", lambda e: e.affine_select(out=uf[:], in_=uf[:], pattern=[[-1, 128]],
                                                    compare_op=ALU.is_ge, fill=1.0, base=0, channel_multiplier=1), **W)
            ph.op("dve", lambda e: e.tensor_copy(out=self.ustr_b[:], in_=uf[:]), **W)
            ph.op("dve", lambda e: e.memset(self.sel2[:], 0.0), **W)
            ph.op("dve", lambda e: e.memset(self.sel2[0:1, :], 1.0), **W)
            ph.op("pool", lambda e: e.iota(ii[:], pattern=[[1, 512]], base=0, channel_multiplier=0), **W)
            ph.op("dve", lambda e: e.tensor_copy(out=self.iota512[:], in_=ii[:]), **W)
            ph.op("pool", lambda e: e.iota(i2[:], pattern=[[1, NT]], base=0, channel_multiplier=0), **W)
            ph.op("pool", lambda e: e.iota(i3[:], pattern=[[0, 1]], base=0, channel_multiplier=1), **W)
            ph.op("dve", lambda e: e.tensor_copy(out=self.tokval[:, :, 0], in_=i2[:]), **W)
            ph.op("dve", lambda e: e.tensor_copy(out=self.tokval[:, :, 1], in_=i3[:].broadcast_to([128, NT])), **W)
            ph.op("dve", lambda e: e.memset(self.mhalf[:], -0.5), **W)
            ph.op("dve", lambda e: e.memset(self.zeros[:], 0.0), **W)
            ph.dma("sp", lambda e: e.dma_start(out=gq[:, 0:64], in_=self.qg.partition_broadcast(128)), **W)
            ph.dma("sp", lambda e: e.dma_start(out=gq[:, 64:128], in_=self.kg.partition_broadcast(128)), **W)
            ph.op("dve", lambda e: e.tensor_reduce(out=mx[:], in_=v3(gq[:], 64), axis=AX.X, op=ALU.max,
                                                   apply_absolute_value=True), **W)
            ph.op("dve", lambda e: e.scalar_tensor_tensor(out=self.negC[:], in0=mx[:, 0:1], scalar=-8.0, in1=mx[:, 1:2],
                                                          op0=ALU.mult, op1=ALU.mult), **W)
            ph.finish()

    def ada(self, l):
        nc = self.nc
        with contextlib.ExitStack() as es:
            cT_s = self.sb(es, "cT_s", [128, 8, 2], F32)
            scT = self.sb(es, "scT", [128, 8, 2], F32)
            wblk = self.rb(es, "wblk", [128, 8, 512], F32, 2)
            brow = self.sb(es, "brow", [1, 6 * D], F32)
            mrow = self.sb(es, "mrow", [2, 6 * D], F32)
            g2r = self.sb(es, "g2r", [2, D], F32)
            a2row = self.sb(es, "a2row", [2, D], F32)
            g1T_s = self.sb(es, "g1T_s", [128, 8], F32)
            tmpA = self.sb(es, "tmpA", [128, 8, 2], F32)
            wr_f = self.sb(es, "wr_f", [128, 8, NE], F32)
            pm = self.rps(es, "pm", [128, 512], F32, 2)
            pTm = self.ps(es, "pTm", [128, 512], F32)
            pb = self.rps(es, "pb", [128, 512], F32, 2)
            ph = Phase(nc, f"ada{l}")
            Bc, Bsc, Bbrow, Bmrow, Bg2, Ba2, Bg1, BmT, BpT, Bvec, Bwr = [Buf(n) for n in "c sc brow mrow g2 a2 g1 mT pT vec wr".split()]
            ph.dma("sp", lambda e: e.dma_start(out=cT_s[:], in_=self.cT), writes=[Bc])
            ph.dma("sp", lambda e: e.dma_start(out=brow[:], in_=self.ada_b[l]), writes=[Bbrow])
            ph.dma("sp", lambda e: e.dma_start(out=g2r[:], in_=self.g2row[l].partition_broadcast(2)), writes=[Bg2])
            ph.dma("sp", lambda e: e.dma_start(out=g1T_s[:], in_=self.g1T[l]), writes=[Bg1])
            ph.dma("sp", lambda e: e.dma_start(out=wr_f[:], in_=self.w_r[l]), writes=[Bwr])
            ph.op("dve", lambda e: e.tensor_copy(out=self.wr_b[:], in_=wr_f[:]), reads=[Bwr], writes=[Bvec])
            ph.op("act", lambda e: e.activation(out=scT[:], in_=cT_s[:], func=AF.Silu), reads=[Bc], writes=[Bsc])
            for cb in range(12):
                wt, Bw = wblk.next()
                ph.dma("sp", lambda e, wt=wt, cb=cb: e.dma_start(
                    out=wt[:], in_=self.ada_w[l][:, cb * 512:(cb + 1) * 512].rearrange("(k p) n -> p k n", p=128)),
                    writes=[Bw])
                pt, Bp = pm.next()
                for k in range(8):
                    ph.op("pe", lambda e, pt=pt, wt=wt, k=k: e.matmul(pt[0:2, :], lhsT=scT[:, k, :], rhs=wt[:, k, :],
                                                                      start=(k == 0), stop=False),
                          reads=[Bsc, Bw], writes=[Bp], signal=False)
                ph.op("pe", lambda e, pt=pt, cb=cb: e.matmul(pt[0:2, :], lhsT=self.ones_f[0:1, 0:2],
                                                             rhs=brow[0:1, cb * 512:(cb + 1) * 512], start=False, stop=True),
                      reads=[Bbrow], writes=[Bp])
                ph.op("dve", lambda e, pt=pt, cb=cb: e.tensor_copy(out=mrow[:, cb * 512:(cb + 1) * 512], in_=pt[0:2, :]),
                      reads=[Bp], writes=[Bmrow])
            pTv = pTm[:, 0:96].rearrange("p (c t) -> p c t", t=2)
            for c in range(48):
                ph.op("pe", lambda e, c=c: e.transpose(out=pTv[:, c, :], in_=mrow[0:2, c * 128:(c + 1) * 128],
                                                       identity=self.ident_f[0:2, 0:2]),
                      reads=[Bmrow], writes=[BpT], signal=(c == 47))
            ph.op("dve", lambda e: e.tensor_copy(out=self.mT[:], in_=pTv), reads=[BpT], writes=[BmT])
            ph.op("dve", lambda e: e.tensor_scalar(out=tmpA[:], in0=self.mT[:, 8:16, :], scalar1=1.0, scalar2=None, op0=ALU.add),
                  reads=[BmT], writes=[Bvec])
            ph.op("dve", lambda e: e.tensor_tensor(out=self.A1T[:], in0=tmpA[:], in1=g1T_s[:].unsqueeze(2).broadcast_to([128, 8, 2]),
                                                   op=ALU.mult), reads=[Bvec, Bg1], writes=[Bvec])
            ph.op("dve", lambda e: e.tensor_scalar(out=a2row[:], in0=mrow[:, 4 * D:5 * D], scalar1=1.0, scalar2=None, op0=ALU.add),
                  reads=[Bmrow], writes=[Ba2])
            ph.op("dve", lambda e: e.tensor_tensor(out=a2row[:], in0=a2row[:], in1=g2r[:], op=ALU.mult),
                  reads=[Ba2, Bg2], writes=[Ba2])
            srcs = [(self.G1bc, mrow[:, 2 * D:3 * D], Bmrow), (self.A2bc, a2row[:], Ba2),
                    (self.B2bc, mrow[:, 3 * D:4 * D], Bmrow), (self.G2bc, mrow[:, 5 * D:6 * D], Bmrow)]
            for dst, src, Bs in srcs:
                for n in range(2):
                    pt, Bp = pb.next()
                    ph.op("pe", lambda e, pt=pt, src=src, n=n: e.matmul(pt[:], lhsT=self.sel2[:], rhs=src[0:2, n * 512:(n + 1) * 512],
                                                                         start=True, stop=True), reads=[Bs], writes=[Bp])
                    ph.op("act", lambda e, pt=pt, dst=dst, n=n: e.activation(out=dst[:, n * 512:(n + 1) * 512], in_=pt[:], func=AF.Copy),
                          reads=[Bp], writes=[Bvec])
            if "mrow" in self.dbg:
                ph.dma("sp", lambda e: e.dma_start(out=self.dbg["mrow"], in_=mrow[:]), reads=[Bmrow])
            ph.finish()

    def l0a(self, es_out):
        nc = self.nc
        self.gT = self.sb(es_out, "gT", [128, 4, S + 30], BF16)
        self.qT = self.sb(es_out, "qT", [128, 4, S], BF16)
        es_a = contextlib.ExitStack()
        self.es_a = es_a
        self.kT2 = self.sb(es_a, "kT2", [128, 2, NKT * 128], BF16)
        self.Vaug = self.sb(es_a, "Vaug", [128, NKT, 2, 65], BF16)
        with contextlib.ExitStack() as es:
            w_in_b = self.sb(es, "w_in_b", [128, 8, 1792], BF16)
            cos_t = self.sb(es, "cos_t", [128, NT, 32], F32)
            sin_t = self.sb(es, "sin_t", [128, NT, 32], F32)
            gq_bc = self.sb(es, "gq_bc", [128, 640], F32)
            junk = self.sb(es, "junk", [128, D], BF16)
            xt = self.rb(es, "xt", [128, D], F32, 3)
            ssq = self.rb(es, "ssq", [128, 1], F32, 3)
            xn = self.rb(es, "xn", [128, D], BF16, 2)
            hnT = self.rb(es, "hnT", [128, 8, 512], BF16, 2)
            sq = self.rb(es, "sq", [128, 640], F32, 1)
            s10 = self.rb(es, "s10", [128, 10], F32, 2)
            qk = self.rb(es, "qk", [128, 640], F32, 1)
            rt = self.rb(es, "rt", [128, 4, 320], F32, 1)
            qkr = self.rb(es, "qkr", [128, 640], BF16, 2)
            kd = self.rb(es, "kd", [128, 256], BF16, 2)
            sig = self.rb(es, "sig", [128, 512], F32, 1)
            pT = self.rps(es, "pT", [128, 8, 128], BF16, 2)
            pq = self.rps(es, "pq", [128, 512], F32, 2)
            pkv = self.rps(es, "pkv", [128, 512], F32, 1)
            pqk = self.rps(es, "pqk", [128, 8, 128], BF16, 1)
            pu = self.rps(es, "pu", [128, 512], F32, 1)
            pg = self.rps(es, "pg", [128, 512], F32, 1)
            ph = Phase(nc, "l0a")
            Bw, Bcs, Bgq, Bjunk, Bout = [Buf(n) for n in "w cs gq junk out".split()]
            ph.dma("pool", lambda e: e.dma_start(out=w_in_b[:], in_=self.ev_w_in.rearrange("(k p) n -> p k n", p=128)), writes=[Bw])
            ph.dma("sp", lambda e: e.dma_start(out=cos_t[:], in_=self.rope_cos), writes=[Bcs])
            ph.dma("sp", lambda e: e.dma_start(out=sin_t[:], in_=self.rope_sin), writes=[Bcs])
            ph.dma("sp", lambda e: e.dma_start(out=gq_bc[:], in_=self.qkg.partition_broadcast(128)), writes=[Bgq])
            ph.op("pool", lambda e: e.memset(self.gT[:, :, 0:15], 0.0), writes=[])
            ph.op("pool", lambda e: e.memset(self.gT[:, :, S + 15:S + 30], 0.0), writes=[])
            ph.op("pool", lambda e: e.memset(self.Vaug[:, :, :, 64:65], 1.0), writes=[])
            evq = 0
            for st in range(9):
                is_ctx = st == 0
                ntile = 2 if is_ctx else 4
                col = 1 if is_ctx else 0
                hT, BhT = hnT.next()
                BhTa = BhT.__dict__.setdefault("twin", Buf(BhT.name + "a"))
                for t in range(ntile):
                    if is_ctx:
                        src = self.ctx[t * 128:(t + 1) * 128, :]
                        kt = t
                        lt = None
                    else:
                        lt = (st - 1) * 4 + t
                        src = self.x[lt * 128:(lt + 1) * 128, :]
                        kt = 2 + lt
                    x_t, Bx = xt.next()
                    ss, Bss = ssq.next()
                    xn_t, Bxn = xn.next()
                    ph.dma("sp", lambda e, x_t=x_t, src=src: e.dma_start(out=x_t[:], in_=src), writes=[Bx])
                    ph.op("act", lambda e, x_t=x_t, ss=ss: e.activation(out=junk[:], in_=x_t[:], func=AF.Square, accum_out=ss[:]),
                          reads=[Bx], writes=[Bjunk, Bss])
                    ph.op("dve", lambda e, ss=ss: e.tensor_scalar(out=ss[:], in0=ss[:], scalar1=1.0 / D, scalar2=EPS, op0=ALU.mult, op1=ALU.add),
                          reads=[Bss], writes=[Bss])
                    ph.op("pool", lambda e, ss=ss: e.tensor_tensor(out=ss[:], in0=ss[:], in1=self.mhalf[:, 0:1], op=ALU.pow),
                          reads=[Bss], writes=[Bss])
                    ph.op("dve", lambda e, x_t=x_t, ss=ss, xn_t=xn_t: e.tensor_scalar(out=xn_t[:], in0=x_t[:], scalar1=ss[:, 0:1], scalar2=None, op0=ALU.mult),
                          reads=[Bx, Bss], writes=[Bxn])
                    p_t, BpT = pT.next()
                    for k in range(8):
                        ph.op("pe", lambda e, p_t=p_t, xn_t=xn_t, k=k: e.transpose(out=p_t[:, k, :], in_=xn_t[:, k * 128:(k + 1) * 128], identity=self.ident_b[:]),
                              reads=[Bxn], writes=[BpT], signal=(k == 7))
                    for k in range(8):
                        wr = [BhT if t % 2 == 0 else BhTa] if k in (0, 7) else []
                        if t % 2 == 0:
                            ph.op("dve", lambda e, p_t=p_t, hT=hT, k=k, t=t, col=col: e.tensor_scalar(
                                out=hT[:, k, t * 128:(t + 1) * 128], in0=p_t[:, k, :], scalar1=self.A1T[:, k, col:col + 1],
                                scalar2=self.mT[:, k, col:col + 1], op0=ALU.mult, op1=ALU.add), reads=[BpT], writes=wr)
                        else:
                            ph.op("act", lambda e, p_t=p_t, hT=hT, k=k, t=t, col=col: e.activation(
                                out=hT[:, k, t * 128:(t + 1) * 128], in_=p_t[:, k, :], func=AF.Identity,
                                scale=self.A1T[:, k, col:col + 1], bias=self.mT[:, k, col:col + 1]), reads=[BpT], writes=wr)
                    pkv_t, Bpkv = pkv.next()
                    for k in range(8):
                        ph.op("pe", lambda e, pkv_t=pkv_t, hT=hT, k=k, t=t: e.matmul(pkv_t[:, 0:256], lhsT=hT[:, k, t * 128:(t + 1) * 128], rhs=w_in_b[:, k, 512:768],
                                                                                   start=(k == 0), stop=(k == 7)), reads=[BhT, BhTa, Bw], writes=[Bpkv], signal=(k == 7))
                    sq_t, Bsq = sq.next()
                    s_t, Bs10 = s10.next()
                    qk_t, Bqk = qk.next()
                    qkr_t, Bqkr = qkr.next()
                    if not is_ctx:
                        pq_t, Bpq = pq.next()
                        for k in range(8):
                            ph.op("pe", lambda e, pq_t=pq_t, hT=hT, k=k, t=t: e.matmul(pq_t[:], lhsT=hT[:, k, t * 128:(t + 1) * 128], rhs=w_in_b[:, k, 0:512],
                                                                                     start=(k == 0), stop=(k == 7)), reads=[BhT, BhTa, Bw], writes=[Bpq], signal=(k == 7))
                        ph.op("act", lambda e, sq_t=sq_t, pq_t=pq_t: e.activation(out=sq_t[:, 0:512], in_=pq_t[:], func=AF.Square), reads=[Bpq], writes=[Bsq, Bpq])
                    h0 = 8 if is_ctx else 0
                    ph.op("act", lambda e, sq_t=sq_t, pkv_t=pkv_t: e.activation(out=sq_t[:, 512:640], in_=pkv_t[:, 0:128], func=AF.Square), reads=[Bpkv], writes=[Bsq, Bpkv])
                    ph.op("dve", lambda e, s_t=s_t, sq_t=sq_t, h0=h0: e.tensor_reduce(out=s_t[:, h0:10], in_=v3(sq_t[:, h0 * 64:640], 64), axis=AX.X, op=ALU.add),
                          reads=[Bsq], writes=[Bs10])
                    ph.op("dve", lambda e, s_t=s_t, h0=h0: e.tensor_scalar(out=s_t[:, h0:10], in0=s_t[:, h0:10], scalar1=1.0 / 64, scalar2=EPS, op0=ALU.mult, op1=ALU.add),
                          reads=[Bs10], writes=[Bs10])
                    ph.op("pool", lambda e, s_t=s_t, h0=h0: e.tensor_tensor(out=s_t[:, h0:10], in0=s_t[:, h0:10], in1=self.mhalf[:, h0:10], op=ALU.pow),
                          reads=[Bs10], writes=[Bs10])
                    if not is_ctx:
                        ph.op("dve", lambda e, qk_t=qk_t, pq_t=pq_t, s_t=s_t: e.tensor_tensor(out=v3(qk_t[:, 0:512], 64), in0=v3(pq_t[:], 64),
                                                                                            in1=s_t[:, 0:8].unsqueeze(2).broadcast_to([128, 8, 64]), op=ALU.mult),
                              reads=[Bpq, Bs10], writes=[Bqk, Bpq])
                    ph.op("dve", lambda e, qk_t=qk_t, pkv_t=pkv_t, s_t=s_t: e.tensor_tensor(out=v3(qk_t[:, 512:640], 64), in0=v3(pkv_t[:, 0:128], 64),
                                                                                          in1=s_t[:, 8:10].unsqueeze(2).broadcast_to([128, 2, 64]), op=ALU.mult),
                          reads=[Bpkv, Bs10], writes=[Bqk, Bpkv])
                    ph.op("act", lambda e, pkv_t=pkv_t, kt=kt: e.activation(out=self.Vaug[:, kt, :, 0:64], in_=v3(pkv_t[:, 128:256], 64), func=AF.Copy),
                          reads=[Bpkv], writes=[Bpkv])
                    c0 = h0 * 64
                    ph.op("dve", lambda e, qk_t=qk_t, c0=c0: e.tensor_tensor(out=qk_t[:, c0:640], in0=qk_t[:, c0:640], in1=gq_bc[:, c0:640], op=ALU.mult),
                          reads=[Bqk, Bgq], writes=[Bqk])
                    if is_ctx:
                        ph.op("dve", lambda e, qkr_t=qkr_t, qk_t=qk_t: e.tensor_copy(out=qkr_t[:, 512:640], in_=qk_t[:, 512:640]), reads=[Bqk], writes=[Bqkr])
                    else:
                        r_t, Brt = rt.next()
                        q3 = v3(qk_t[:], 64)
                        o3 = v3(qkr_t[:], 64)
                        cb = cos_t[:, lt, :].unsqueeze(1).broadcast_to([128, 10, 32])
                        sbc = sin_t[:, lt, :].unsqueeze(1).broadcast_to([128, 10, 32])
                        r3 = [v3(r_t[:, i, :], 32) for i in range(4)]
                        ph.op("dve", lambda e, r3=r3, q3=q3, cb=cb: e.tensor_tensor(out=r3[0], in0=q3[:, :, 0:32], in1=cb, op=ALU.mult), reads=[Bqk, Bcs], writes=[Brt])
                        ph.op("pool", lambda e, r3=r3, q3=q3, sbc=sbc: e.tensor_tensor(out=r3[1], in0=q3[:, :, 32:64], in1=sbc, op=ALU.mult), reads=[Bqk, Bcs], writes=[Brt])
                        ph.op("dve", lambda e, r3=r3, q3=q3, cb=cb: e.tensor_tensor(out=r3[2], in0=q3[:, :, 32:64], in1=cb, op=ALU.mult), reads=[Bqk, Bcs], writes=[Brt])
                        ph.op("pool", lambda e, r3=r3, q3=q3, sbc=sbc: e.tensor_tensor(out=r3[3], in0=q3[:, :, 0:32], in1=sbc, op=ALU.mult), reads=[Bqk, Bcs], writes=[Brt])
                        ph.op("dve", lambda e, r3=r3, o3=o3: e.tensor_tensor(out=o3[:, :, 0:32], in0=r3[0], in1=r3[1], op=ALU.subtract), reads=[Brt], writes=[Bqkr])
                        ph.op("dve", lambda e, r3=r3, o3=o3: e.tensor_tensor(out=o3[:, :, 32:64], in0=r3[2], in1=r3[3], op=ALU.add), reads=[Brt], writes=[Bqkr])
                    kd_t, Bkd = kd.next()
                    ph.op("pool", lambda e, kd_t=kd_t, qkr_t=qkr_t: e.tensor_copy(
                        out=kd_t[:].rearrange("p (g r d) -> p g r d", g=2, r=2),
                        in_=v3(qkr_t[:, 512:640], 64).unsqueeze(2).broadcast_to([128, 2, 2, 64])), reads=[Bqkr], writes=[Bkd])
                    pqk_t, Bpqk = pqk.next()
                    if not is_ctx:
                        for p in range(4):
                            ph.op("pe", lambda e, pqk_t=pqk_t, qkr_t=qkr_t, p=p: e.transpose(out=pqk_t[:, p, :], in_=qkr_t[:, p * 128:(p + 1) * 128], identity=self.ident_b[:]),
                                  reads=[Bqkr], writes=[Bpqk], signal=False)
                    for g in range(2):
                        ph.op("pe", lambda e, pqk_t=pqk_t, kd_t=kd_t, g=g: e.transpose(out=pqk_t[:, 4 + g, :], in_=kd_t[:, g * 128:(g + 1) * 128], identity=self.ident_b[:]),
                              reads=[Bkd], writes=[Bpqk], signal=(g == 1))
                    if not is_ctx:
                        ph.op("act", lambda e, pqk_t=pqk_t, lt=lt: e.activation(out=self.qT[:, :, lt * 128:(lt + 1) * 128], in_=pqk_t[:, 0:4, :], func=AF.Copy),
                              reads=[Bpqk], writes=[Bpqk])
                    ph.op("dve", lambda e, pqk_t=pqk_t, kt=kt: e.tensor_copy(out=self.kT2[:, :, kt * 128:(kt + 1) * 128], in_=pqk_t[:, 4:6, :]),
                          reads=[Bpqk], writes=[Bpqk])
                if not is_ctx:
                    tok0 = 15 + (st - 1) * 512
                    for c in range(4):
                        pu_t, Bpu = pu.next()
                        pg_t, Bpg = pg.next()
                        sg_t, Bsg = sig.next()
                        for k in range(8):
                            ph.op("pe", lambda e, pu_t=pu_t, hT=hT, k=k, c=c: e.matmul(pu_t[:], lhsT=w_in_b[:, k, 768 + c * 128:768 + (c + 1) * 128], rhs=hT[:, k, :],
                                                                                     start=(k == 0), stop=(k == 7)), reads=[BhT, BhTa, Bw], writes=[Bpu], signal=(k == 7))
                        for k in range(8):
                            ph.op("pe", lambda e, pg_t=pg_t, hT=hT, k=k, c=c: e.matmul(pg_t[:], lhsT=w_in_b[:, k, 1280 + c * 128:1280 + (c + 1) * 128], rhs=hT[:, k, :],
                                                                                     start=(k == 0), stop=(k == 7)), reads=[BhT, BhTa, Bw], writes=[Bpg], signal=(k == 7))
                        ph.op("act", lambda e, sg_t=sg_t, pg_t=pg_t: e.activation(out=sg_t[:], in_=pg_t[:], func=AF.Sigmoid), reads=[Bpg], writes=[Bsg])
                        ph.op("dve", lambda e, sg_t=sg_t, pu_t=pu_t, c=c, tok0=tok0: e.tensor_tensor(out=self.gT[:, c, tok0:tok0 + 512], in0=pu_t[:], in1=sg_t[:], op=ALU.mult),
                              reads=[Bpu, Bsg], writes=[])
            for nm, t in (("qT", self.qT), ("kT2", self.kT2), ("Vaug", self.Vaug), ("gT", self.gT)):
                if nm in self.dbg:
                    ph.dma("sp", lambda e, nm=nm, t=t: e.dma_start(out=self.dbg[nm], in_=t[:]), reads=[Bout])
            ph.finish()

    def attn(self, es_out):
        nc = self.nc
        self.attnT = self.qT
        NJ2 = NKT // 2
        with contextlib.ExitStack() as es:
            kTz = self.sb(es, "kTz", [128, 2, 2, NKT * 128], BF16)
            PT = self.rb(es, "PT", [128, 1024], BF16, 3)
            at2 = self.rb(es, "at2", [128, 4, 128], BF16, 2)
            rec = self.rb(es, "rec", [128, 4], F32, 2)
            pS = self.rps(es, "pS", [128, 1024], F32, 2)
            pO = self.rps(es, "pO", [128, 512], F32, 2)
            pA = self.rps(es, "pA", [128, 8, 128], BF16, 1)
            ph = Phase(nc, "attn")
            Bkz = Buf("kz")
            ph.op("dve", lambda e: e.memset(kTz[:], 0.0), writes=[Bkz])
            for g in range(2):
                ph.op("dve", lambda e, g=g: e.tensor_copy(out=kTz[0:64, g, 0, :], in_=self.kT2[0:64, g, :]), reads=[Bkz], writes=[Bkz])
                ph.op("dve", lambda e, g=g: e.tensor_copy(out=kTz[64:128, g, 1, :], in_=self.kT2[64:128, g, :]), reads=[Bkz], writes=[Bkz])
            for pair in range(4):
                g = pair // 2
                for Q in range(8):
                    at_t, Bat = at2.next()
                    for hl in range(2):
                        pO_t, BpO = pO.next()
                        pOv = pO_t[:, 0:260].rearrange("p (q d) -> p q d", d=65)

                        def emit_st(jj, hl=hl, pair=pair, Q=Q, g=g):
                            pS_t, BpS = pS.next()
                            for u in range(2):
                                j = 2 * jj + u
                                ph.op("pe", lambda e, pS_t=pS_t, j=j, u=u: e.matmul(
                                    pS_t[:, u * 512:(u + 1) * 512], lhsT=kTz[:, g, hl, j * 128:(j + 1) * 128],
                                    rhs=self.qT[:, pair, Q * 512:(Q + 1) * 512], start=True, stop=True), reads=[Bkz], writes=[BpS], signal=(u == 1))
                            return pS_t, BpS
                        nxt = emit_st(0)
                        for jj in range(NJ2):
                            pS_t, BpS = nxt
                            if jj + 1 < NJ2:
                                nxt = emit_st(jj + 1)
                            P_t, BP = PT.next()
                            ph.op("act", lambda e, P_t=P_t, pS_t=pS_t: e.activation(out=P_t[:], in_=pS_t[:], func=AF.Exp, scale=0.125, bias=self.negC[:, 0:1]),
                                  reads=[BpS], writes=[BP])
                            for u in range(2):
                                j = 2 * jj + u
                                for qt in range(4):
                                    ph.op("pe", lambda e, pOv=pOv, P_t=P_t, qt=qt, j=j, u=u, g=g: e.matmul(
                                        pOv[:, qt, :], lhsT=P_t[:, u * 512 + qt * 128:u * 512 + (qt + 1) * 128], rhs=self.Vaug[:, j, g, :],
                                        start=(j == 0 and qt == 0), stop=(j == NKT - 1), skip_group_check=True),
                                        reads=[BP], writes=[BpO], signal=(qt == 3 and (j == NKT - 1)))
                        rc, Brc = rec.next()
                        ph.op("dve", lambda e, rc=rc, pOv=pOv: e.reciprocal(out=rc[:], in_=pOv[:, :, 64]), reads=[BpO], writes=[Brc])
                        ph.op("dve", lambda e, rc=rc, pOv=pOv, at_t=at_t, hl=hl: e.tensor_tensor(
                            out=at_t[:, :, hl * 64:(hl + 1) * 64], in0=pOv[:, :, 0:64], in1=rc[:].unsqueeze(2).broadcast_to([128, 4, 64]), op=ALU.mult),
                            reads=[BpO, Brc], writes=[Bat])
                    pA_t, BpA = pA.next()
                    for qt in range(4):
                        ph.op("pe", lambda e, pA_t=pA_t, at_t=at_t, qt=qt: e.transpose(out=pA_t[:, qt, :], in_=at_t[:, qt, :], identity=self.ident_b[:]),
                              reads=[Bat], writes=[BpA], signal=(qt == 3))
                    ph.op("dve", lambda e, pA_t=pA_t, pair=pair, Q=Q: e.tensor_copy(
                        out=self.attnT[:, pair, Q * 512:(Q + 1) * 512].rearrange("p (q t) -> p q t", t=128), in_=pA_t[:, 0:4, :]),
                        reads=[BpA], writes=[])
            ph.finish()

    def conv(self, es_out):
        nc = self.nc
        self.convT = self.sb(es_out, "convT", [128, 4, S], BF16)
        with contextlib.ExitStack() as es:
            dg = self.sb(es, "dg", [128, 31, 4, 128], BF16)
            cw = self.sb(es, "cw", [128, 4, 31], F32)
            cv = self.sb(es, "cv", [128, 3, 4], F32)
            yf = self.rb(es, "yf", [128, 4, 512], F32, 2)
            ybf = self.rb(es, "ybf", [128, 4, 512], BF16, 2)
            ysq = self.rb(es, "ysq", [128, 4, 512], BF16, 2)
            mean = self.rb(es, "mean", [128, 512], F32, 2)
            msq = self.rb(es, "msq", [128, 512], F32, 1)
            rstd = self.rb(es, "rstd", [128, 512], F32, 2)
            zt = self.rb(es, "zt", [128, 512], F32, 2)
            pc = self.rps(es, "pc", [128, 512], F32, 3)
            pS1 = self.rps(es, "pS1", [128, 512], F32, 1)
            pS2 = self.rps(es, "pS2", [128, 512], F32, 1)
            ph = Phase(nc, "conv")
            Bcw, Bcv, Bdg, Bout = [Buf(n) for n in "cw cv dg out".split()]
            ph.dma("sp", lambda e: e.dma_start(out=cw[:], in_=self.conv_wT), writes=[Bcw])
            ph.dma("sp", lambda e: e.dma_start(out=cv[:], in_=self.conv_vec), writes=[Bcv])
            n = 0
            for j in range(31):
                for c in range(4):
                    eng = "dve"
                    ph.op(eng, lambda e, j=j, c=c: e.tensor_scalar(out=dg[:, j, c, :], in0=self.ident_b[:], scalar1=cw[:, c, j:j + 1], scalar2=None, op0=ALU.mult),
                          reads=[Bcw], writes=[Bdg])
            for tc in range(8):
                yf_t, Byf = yf.next()
                yb_t, Byb = ybf.next()
                ys_t, Bys = ysq.next()
                for c in range(4):
                    pc_t, Bpc = pc.next()
                    for j in range(31):
                        ph.op("pe", lambda e, pc_t=pc_t, j=j, c=c, tc=tc: e.matmul(pc_t[:], lhsT=dg[:, j, c, :], rhs=self.gT[:, c, tc * 512 + j:tc * 512 + j + 512],
                                                                                  start=(j == 0), stop=(j == 30)), reads=[Bdg], writes=[Bpc], signal=(j == 30))
                    ph.op("act", lambda e, pc_t=pc_t, yf_t=yf_t, c=c: e.activation(out=yf_t[:, c, :], in_=pc_t[:], func=AF.Identity, bias=cv[:, 0, c:c + 1]),
                          reads=[Bpc, Bcv], writes=[Byf])
                    ph.op("act", lambda e, pc_t=pc_t, ys_t=ys_t, c=c: e.activation(out=ys_t[:, c, :], in_=pc_t[:], func=AF.Square, bias=cv[:, 0, c:c + 1]),
                          reads=[Bpc, Bcv], writes=[Bys])
                    ph.op("dve", lambda e, yf_t=yf_t, yb_t=yb_t, c=c: e.tensor_copy(out=yb_t[:, c, :], in_=yf_t[:, c, :]), reads=[Byf], writes=[Byb])
                p1, Bp1 = pS1.next()
                p2, Bp2 = pS2.next()
                for c in range(4):
                    ph.op("pe", lambda e, p1=p1, yb_t=yb_t, c=c: e.matmul(p1[:], lhsT=self.ones_b[:], rhs=yb_t[:, c, :], start=(c == 0), stop=(c == 3)),
                          reads=[Byb], writes=[Bp1], signal=(c == 3))
                for c in range(4):
                    ph.op("pe", lambda e, p2=p2, ys_t=ys_t, c=c: e.matmul(p2[:], lhsT=self.ones_b[:], rhs=ys_t[:, c, :], start=(c == 0), stop=(c == 3)),
                          reads=[Bys], writes=[Bp2], signal=(c == 3))
                mn, Bmn = mean.next()
                ms, Bms = msq.next()
                rs, Brs = rstd.next()
                ph.op("act", lambda e, mn=mn, p1=p1: e.activation(out=mn[:], in_=p1[:], func=AF.Copy, scale=1.0 / 512), reads=[Bp1], writes=[Bmn])
                ph.op("dve", lambda e, mn=mn, ms=ms: e.tensor_tensor(out=ms[:], in0=mn[:], in1=mn[:], op=ALU.mult), reads=[Bmn], writes=[Bms])
                ph.op("dve", lambda e, rs=rs, p2=p2, ms=ms: e.scalar_tensor_tensor(out=rs[:], in0=p2[:], scalar=1.0 / 512, in1=ms[:], op0=ALU.mult, op1=ALU.subtract),
                      reads=[Bp2, Bms], writes=[Brs])
                ph.op("dve", lambda e, rs=rs: e.tensor_scalar(out=rs[:], in0=rs[:], scalar1=EPS, scalar2=None, op0=ALU.add), reads=[Brs], writes=[Brs])
                ph.op("act", lambda e, rs=rs: e.activation(out=rs[:], in_=rs[:], func=AF.Sqrt), reads=[Brs], writes=[Brs])
                ph.op("dve", lambda e, rs=rs: e.reciprocal(out=rs[:], in_=rs[:]), reads=[Brs], writes=[Brs])
                for c in range(4):
                    z_t, Bz = zt.next()
                    ph.op("dve", lambda e, z_t=z_t, yf_t=yf_t, mn=mn, c=c: e.tensor_tensor(out=z_t[:], in0=yf_t[:, c, :], in1=mn[:], op=ALU.subtract), reads=[Byf, Bmn], writes=[Bz])
                    ph.op("dve", lambda e, z_t=z_t, rs=rs: e.tensor_tensor(out=z_t[:], in0=z_t[:], in1=rs[:], op=ALU.mult), reads=[Bz, Brs], writes=[Bz])
                    ph.op("act", lambda e, z_t=z_t, c=c, tc=tc: e.activation(out=self.convT[:, c, tc * 512:(tc + 1) * 512], in_=z_t[:], func=AF.Silu,
                                                                           scale=cv[:, 1, c:c + 1], bias=cv[:, 2, c:c + 1]), reads=[Bz, Bcv], writes=[])
            if "convT" in self.dbg:
                ph.dma("sp", lambda e: e.dma_start(out=self.dbg["convT"], in_=self.convT[:]), reads=[Bout])
            ph.finish()

    def tail_alloc(self, es, deep=False):
        T = {}
        T["hm"] = self.rb(es, "hm", [128, D], F32, 2)
        T["tmp"] = self.rb(es, "tmp", [128, D], F32, 2 if deep else 1)
        T["junk"] = self.sb(es, "junk2", [128, D], BF16)
        T["ss"] = self.rb(es, "ss2", [128, 4], F32, 2)
        T["aug"] = self.rb(es, "aug", [128, RW], BF16, 2)
        T["h2T"] = self.rb(es, "h2T", [128, 8, 128], BF16, 2)
        T["ex"] = self.rb(es, "ex", [128, NE], F32, 2)
        T["pT2"] = self.rps(es, "pT2", [128, 8, 128], BF16, 2 if deep else 1)
        T["pl"] = self.rps(es, "pl", [128, 512], F32, 2 if deep else 1)
        T["Bjunk"] = Buf("junk2")
        T["Baff"] = Buf("aff")
        return T

    def tail_tile(self, ph, T, i, po_list, x_t, Bx, hdst):
        hm, Bhm = T["hm"].next()
        tmp, Btmp = T["tmp"].next()
        ss, Bss = T["ss"].next()
        aug, Baug = T["aug"].next()
        h2T, Bh2T = T["h2T"].next()
        ex, Bex = T["ex"].next()
        pT2, BpT2 = T["pT2"].next()
        pl, Bpl = T["pl"].next()
        for n, (po, Bpo) in enumerate(po_list):
            sl = slice(n * 512, (n + 1) * 512)
            ph.op("dve", lambda e, hm=hm, po=po, sl=sl: e.tensor_tensor(out=hm[:, sl], in0=po, in1=self.G1bc[:, sl], op=ALU.mult), reads=[Bpo], writes=[Bhm])
            ph.op("dve", lambda e, hm=hm, x_t=x_t, sl=sl: e.tensor_tensor(out=hm[:, sl], in0=hm[:, sl], in1=x_t[:, sl], op=ALU.add), reads=[Bhm, Bx], writes=[Bhm])
        ph.dma("sp", lambda e, hm=hm, i=i: e.dma_start(out=hdst[i * 128:(i + 1) * 128, :], in_=hm[:]), reads=[Bhm])
        ph.op("act", lambda e, hm=hm, ss=ss: e.activation(out=T["junk"][:], in_=hm[:], func=AF.Square, accum_out=ss[:, 0:1]), reads=[Bhm], writes=[T["Bjunk"], Bss])
        ph.op("dve", lambda e, ss=ss: e.tensor_scalar(out=ss[:, 0:1], in0=ss[:, 0:1], scalar1=1.0 / D, scalar2=EPS, op0=ALU.mult, op1=ALU.add), reads=[Bss], writes=[Bss])
        ph.op("pool", lambda e, ss=ss: e.tensor_tensor(out=ss[:, 0:1], in0=ss[:, 0:1], in1=self.mhalf[:, 0:1], op=ALU.pow), reads=[Bss], writes=[Bss])
        ph.op("dve", lambda e, tmp=tmp, hm=hm, ss=ss: e.scalar_tensor_tensor(out=tmp[:], in0=hm[:], scalar=ss[:, 0:1], in1=self.A2bc[:], op0=ALU.mult, op1=ALU.mult),
              reads=[Bhm, Bss], writes=[Btmp])
        ph.op("dve", lambda e, tmp=tmp, aug=aug: e.tensor_tensor(out=aug[:, 0:D], in0=tmp[:], in1=self.B2bc[:], op=ALU.add), reads=[Btmp], writes=[Baug])
        for k in range(8):
            ph.op("pe", lambda e, pT2=pT2, aug=aug, k=k: e.transpose(out=pT2[:, k, :], in_=aug[:, k * 128:(k + 1) * 128], identity=self.ident_b[:]),
                  reads=[Baug], writes=[BpT2], signal=(k == 7))
        ph.op("act", lambda e, h2T=h2T, pT2=pT2: e.activation(out=h2T[:], in_=pT2[:], func=AF.Copy), reads=[BpT2], writes=[Bh2T])
        for k in range(8):
            ph.op("pe", lambda e, pl=pl, h2T=h2T, k=k: e.matmul(pl[:, 0:NE], lhsT=h2T[:, k, :], rhs=self.wr_b[:, k, :], start=(k == 0), stop=(k == 7)),
                  reads=[Bh2T], writes=[Bpl], signal=(k == 7))
        ph.op("dve", lambda e, ss=ss, pl=pl: e.tensor_reduce(out=ss[:, 1:2], in_=pl[:, 0:NE], axis=AX.X, op=ALU.max), reads=[Bpl], writes=[Bss])
        ph.op("dve", lambda e, ss=ss: e.tensor_scalar(out=ss[:, 1:2], in0=ss[:, 1:2], scalar1=-1.0, scalar2=None, op0=ALU.mult), reads=[Bss], writes=[Bss])
        ph.op("act", lambda e, ex=ex, pl=pl, ss=ss: e.activation(out=ex[:], in_=pl[:, 0:NE], func=AF.Exp, bias=ss[:, 1:2], accum_out=ss[:, 2:3]),
              reads=[Bpl, Bss], writes=[Bex, Bss])
        ph.op("dve", lambda e, ss=ss: e.reciprocal(out=ss[:, 3:4], in_=ss[:, 2:3]), reads=[Bss], writes=[Bss])
        ph.op("dve", lambda e, ex=ex, ss=ss, i=i: e.tensor_scalar(out=self.affTM[:, i, :], in0=ex[:], scalar1=ss[:, 3:4], scalar2=None, op0=ALU.mult),
              reads=[Bex, Bss], writes=[T["Baff"]])
        ph.op("dve", lambda e, aug=aug, i=i: e.tensor_copy(out=aug[:, D:D + 16], in_=self.affTM[:, i, :]), reads=[T["Baff"]], writes=[Baug])
        ph.op("dve", lambda e, aug=aug, ex=ex, i=i: e.tensor_tensor(out=ex[:], in0=self.affTM[:, i, :], in1=aug[:, D:D + 16], op=ALU.subtract), reads=[T["Baff"], Baug], writes=[Bex])
        ph.op("dve", lambda e, aug=aug, ex=ex: e.tensor_copy(out=aug[:, D + 16:D + 32], in_=ex[:]), reads=[Bex], writes=[Baug])
        ph.op("dve", lambda e, aug=aug, ex=ex: e.tensor_tensor(out=ex[:], in0=ex[:], in1=aug[:, D + 16:D + 32], op=ALU.subtract), reads=[Bex, Baug], writes=[Bex])
        ph.op("dve", lambda e, aug=aug, ex=ex: e.tensor_copy(out=aug[:, D + 32:D + 48], in_=ex[:]), reads=[Bex], writes=[Baug])
        ph.dma("sp", lambda e, aug=aug, i=i: e.dma_start(out=self.h2aug[i * 128:(i + 1) * 128, :], in_=aug[:]), reads=[Baug])

    def outproj0(self):
        nc = self.nc
        with contextlib.ExitStack() as es:
            w_out_b = self.sb(es, "w_out_b", [128, 8, D], BF16)
            xt = self.rb(es, "xt", [128, D], F32, 3)
            T = self.tail_alloc(es, deep=True)
            po = self.rps(es, "po", [128, 512], F32, 4)
            ph = Phase(nc, "outproj0")
            Bw = Buf("w")
            ph.dma("pool", lambda e: e.dma_start(out=w_out_b[:], in_=self.ev_w_out.rearrange("(k p) n -> p k n", p=128)), writes=[Bw])
            loads = {}

            def issue_load(i):
                x_t, Bx = xt.next()
                ph.dma("sp", lambda e, x_t=x_t, i=i: e.dma_start(out=x_t[:], in_=self.x[i * 128:(i + 1) * 128, :]), writes=[Bx])
                loads[i] = (x_t, Bx)
            issue_load(0)
            issue_load(1)
            for i in range(NT):
                if i + 2 < NT:
                    issue_load(i + 2)
                x_t, Bx = loads.pop(i)
                pol = []
                for n in range(2):
                    po_t, Bpo = po.next()
                    for fc in range(8):
                        src = self.attnT[:, fc, i * 128:(i + 1) * 128] if fc < 4 else self.convT[:, fc - 4, i * 128:(i + 1) * 128]
                        ph.op("pe", lambda e, po_t=po_t, src=src, fc=fc, n=n: e.matmul(po_t[:], lhsT=src, rhs=w_out_b[:, fc, n * 512:(n + 1) * 512],
                                                                                     start=(fc == 0), stop=(fc == 7)), reads=[Bw], writes=[Bpo], signal=(fc == 7))
                    pol.append((po_t[:], Bpo))
                self.tail_tile(ph, T, i, pol, x_t, Bx, self.hbuf)
            if "aff" in self.dbg:
                ph.dma("sp", lambda e: e.dma_start(out=self.dbg["aff"], in_=self.affTM[:]), reads=[T["Baff"]])
            ph.finish()
            self.dump_tail()

    def dump_h2(self):
        if "h1" in self.dbg:
            ph = Phase(self.nc, "dumph1")
            B = Buf("d")
            ph.dma("sp", lambda e: e.dma_start(out=self.dbg["h1"], in_=self.hbuf2), reads=[B], writes=[B])
            ph.op("dve", lambda e: e.memset(self.zeros[:, 0:1], 0.0), reads=[B], writes=[B])
            ph.finish()

    def dump_tail(self):
        ph = Phase(self.nc, "dump")
        B = Buf("dump")
        for nm, src in (("hmid", self.hbuf), ("h2aug", self.h2aug)):
            if nm in self.dbg:
                ph.dma("sp", lambda e, nm=nm, src=src: e.dma_start(out=self.dbg[nm], in_=src), reads=[B], writes=[B])
        ph.op("dve", lambda e: e.memset(self.zeros[:, 0:1], 0.0), reads=[B], writes=[B])
        ph.finish()

    def route(self, es_out, l):
        nc = self.nc
        self.Wg = self.rb(es_out, "Wg", [128, 8, D], BF16, 2)
        self.Wu = self.rb(es_out, "Wu", [128, 8, D], BF16, 2)
        self.Wd = self.rb(es_out, "Wd", [128, 8, D], BF16, 2)
        self.slotm = self.sb(es_out, "slotm", [128, NT * NE], F32)
        with contextlib.ExitStack() as es:
            affT = self.sb(es, "affT", [16, S], F32)
            junk = self.sb(es, "junkr", [16, S], BF16)
            sc = self.sb(es, "sc", [16, 8], F32)
            dth = self.sb(es, "dth", [16, 16], F32)
            thr_bc = self.sb(es, "thr_bc", [128, NE], F32)
            mask_f = self.sb(es, "mask_f", [128, NT * NE], F32)
            mask_b = self.sb(es, "mask_b", [128, NT * NE], BF16)
            within = self.sb(es, "within", [128, NT * NE], F32)
            cA = self.sb(es, "cA", [128, NT * NE], F32)
            cB = self.sb(es, "cB", [128, NT * NE], F32)
            tot = self.sb(es, "tot", [128, NT * NE], F32)
            pa = self.rps(es, "pa", [128, 512], F32, 2)
            ph = Phase(nc, f"route{l}")
            self.pre_w = []
            for ring, src in ((self.Wg, self.w_gate), (self.Wu, self.w_up), (self.Wd, self.w_down)):
                wt, Bw = ring.next()
                ph.dma("pool", lambda e, wt=wt, src=src: e.dma_start(out=wt[:], in_=src[l, 0].rearrange("(k p) n -> p k n", p=128)), writes=[Bw])
                self.pre_w.append((wt, Bw))
            B = Buf("r")
            W = dict(reads=[B], writes=[B])
            BaT = Buf("affT")
            for grp in range(8):
                pt, Bp = pa.next()
                for t in range(4):
                    ph.op("pe", lambda e, pt=pt, grp=grp, t=t: e.transpose(out=pt[0:16, t * 128:(t + 1) * 128], in_=self.affTM[:, grp * 4 + t, :], identity=self.ident_f[:]),
                          writes=[Bp], signal=(t == 3))
                ph.op("act", lambda e, pt=pt, grp=grp: e.activation(out=affT[:, grp * 512:(grp + 1) * 512], in_=pt[0:16, :], func=AF.Copy), reads=[Bp], writes=[BaT])
            lo, hi, mid, cnt, pred, dd = [sc[:, i:i + 1] for i in range(6)]
            ph.op("dve", lambda e: e.memset(sc[:], 0.0), reads=[BaT], writes=[B])
            ph.op("dve", lambda e: e.memset(hi, 1.5), **W)
            for it in range(30):
                ph.op("dve", lambda e: e.tensor_scalar(out=mid, in0=lo, scalar1=hi, scalar2=0.5, op0=ALU.add, op1=ALU.mult), **W)
                ph.op("dve", lambda e: e.tensor_scalar(out=junk[:], in0=affT[:], scalar1=mid, scalar2=0.0, op0=ALU.is_ge, op1=ALU.add, accum_out=cnt), **W)
                ph.op("dve", lambda e: e.tensor_scalar(out=pred, in0=cnt, scalar1=float(CAP), scalar2=None, op0=ALU.is_ge), **W)
                ph.op("dve", lambda e: e.tensor_tensor(out=dd, in0=mid, in1=lo, op=ALU.subtract), **W)
                ph.op("dve", lambda e: e.scalar_tensor_tensor(out=lo, in0=dd, scalar=pred, in1=lo, op0=ALU.mult, op1=ALU.add), **W)
                ph.op("dve", lambda e: e.tensor_tensor(out=dd, in0=hi, in1=mid, op=ALU.subtract), **W)
                ph.op("dve", lambda e: e.scalar_tensor_tensor(out=hi, in0=dd, scalar=pred, in1=mid, op0=ALU.mult, op1=ALU.add), **W)
            ph.op("dve", lambda e: e.tensor_scalar(out=dth[:], in0=self.ident_f[0:16, 0:16], scalar1=lo, scalar2=None, op0=ALU.mult), **W)
            pt, Bp = pa.next()
            ph.op("pe", lambda e, pt=pt: e.matmul(pt[:, 0:NE], lhsT=self.ones_f[0:16, :], rhs=dth[:], start=True, stop=True), reads=[B], writes=[Bp])
            ph.op("dve", lambda e, pt=pt: e.tensor_copy(out=thr_bc[:], in_=pt[:, 0:NE]), reads=[Bp], writes=[B])
            ph.op("dve", lambda e: e.tensor_tensor(out=v3(mask_f[:], NE), in0=self.affTM[:], in1=thr_bc[:].unsqueeze(1).broadcast_to([128, NT, NE]), op=ALU.is_ge), **W)
            ph.op("dve", lambda e: e.tensor_copy(out=mask_b[:], in_=mask_f[:]), **W)
            p1, Bp1 = pa.next()
            ph.op("pe", lambda e, p1=p1: e.matmul(p1[:], lhsT=self.ustr_b[:], rhs=mask_b[:], start=True, stop=True), reads=[B], writes=[Bp1])
            ph.op("dve", lambda e, p1=p1: e.tensor_copy(out=within[:], in_=p1[:]), reads=[Bp1], writes=[B])
            p2, Bp2 = pa.next()
            ph.op("pe", lambda e, p2=p2: e.matmul(p2[:], lhsT=self.ones_b[:], rhs=mask_b[:], start=True, stop=True), reads=[B], writes=[Bp2])
            ph.op("dve", lambda e, p2=p2: e.tensor_copy(out=tot[:], in_=p2[:]), reads=[Bp2], writes=[B])
            ph.op("dve", lambda e: e.tensor_copy(out=cA[:], in_=tot[:]), **W)
            src, dst = cA, cB
            for sft in (1, 2, 4, 8, 16):
                w = sft * NE
                ph.op("dve", lambda e, src=src, dst=dst, w=w: e.tensor_copy(out=dst[:, 0:w], in_=src[:, 0:w]), **W)
                ph.op("dve", lambda e, src=src, dst=dst, w=w: e.tensor_tensor(out=dst[:, w:], in0=src[:, w:], in1=src[:, 0:NT * NE - w], op=ALU.add), **W)
                src, dst = dst, src
            ph.op("dve", lambda e, src=src: e.tensor_tensor(out=src[:], in0=src[:], in1=tot[:], op=ALU.subtract), **W)
            ph.op("dve", lambda e, src=src: e.tensor_tensor(out=within[:], in0=within[:], in1=src[:], op=ALU.add), **W)
            ph.op("dve", lambda e: e.scalar_tensor_tensor(out=within[:], in0=within[:], scalar=1.0, in1=mask_f[:], op0=ALU.add, op1=ALU.mult), **W)
            ph.op("dve", lambda e: e.tensor_scalar(out=self.slotm[:], in0=within[:], scalar1=-1.0, scalar2=None, op0=ALU.add), **W)
            if "slotm" in self.dbg:
                ph.dma("sp", lambda e: e.dma_start(out=self.dbg["slotm"], in_=self.slotm[:]), **W)
            if "thr" in self.dbg:
                ph.dma("sp", lambda e: e.dma_start(out=self.dbg["thr"], in_=sc[:]), **W)
            ph.finish()

    def moe(self, l):
        nc = self.nc
        with contextlib.ExitStack() as es:
            Wg, Wu, Wd = self.Wg, self.Wu, self.Wd
            oh = self.rb(es, "oh", [128, 512], BF16, 3)
            idxf = self.rb(es, "idxf", [128, 4], F32, 2)
            idx8 = self.rb(es, "idx8", [128, 8], F32, 2)
            idxi = self.rb(es, "idxi", [128, 4], I32, 2)
            xs = self.rb(es, "xs", [128, 4, RW], BF16, 2)
            gs = self.rb(es, "gs", [128, 4], F32, 2)
            xsT = self.rb(es, "xsT", [128, 8, 512], BF16, 2)
            hidT = self.rb(es, "hidT", [128, 8, 512], BF16, 2)
            sg = self.rb(es, "sg", [128, 512], F32, 2)
            ysb = self.rb(es, "ysb", [128, D], F32, 3)
            pG = self.rps(es, "pG", [128, 512], F32, 2)
            pU = self.rps(es, "pU", [128, 512], F32, 2)
            pY = self.rps(es, "pY", [128, 512], F32, 2)
            pTP = self.rps(es, "pTP", [128, 8, 128], BF16, 1)
            pI = self.rps(es, "pI", [128, 512], F32, 1)
            ph = Phase(nc, f"moe{l}")
            Bacc = Buf("acc")
            Bz = [Buf(f"z{i}") for i in range(NT)]
            for i in range(NT):
                ph.dma("sp", lambda e, i=i: e.dma_start(out=self.acc[i * 128:(i + 1) * 128, :], in_=self.zeros[:, 0:D]), writes=[Bz[i]])
            first_scatter = [True]

            def load_w(e_):
                r = []
                for ring, src in ((Wg, self.w_gate), (Wu, self.w_up), (Wd, self.w_down)):
                    wt, Bw = ring.next()
                    ph.dma("pool", lambda e, wt=wt, src=src, e_=e_: e.dma_start(out=wt[:], in_=src[l, e_].rearrange("(k p) n -> p k n", p=128)), writes=[Bw])
                    r.append((wt, Bw))
                return r

            def build_idx(e_, res):
                pI_t, BpI = pI.next()
                pIv = pI_t[:, 0:8].rearrange("p (c t) -> p c t", t=2)
                for i in range(NT):
                    oh_t, Boh = oh.next()
                    ph.op("dve", lambda e, oh_t=oh_t, i=i, e_=e_: e.tensor_scalar(out=oh_t[:], in0=self.iota512[:], scalar1=self.slotm[:, i * NE + e_:i * NE + e_ + 1],
                                                                                 scalar2=None, op0=ALU.is_equal), writes=[Boh])
                    for c in range(4):
                        ph.op("pe", lambda e, pIv=pIv, oh_t=oh_t, i=i, c=c: e.matmul(pIv[:, c, :], lhsT=oh_t[:, c * 128:(c + 1) * 128], rhs=self.tokval[:, i, :],
                                                                                   start=(i == 0 and c == 0), stop=(i == NT - 1), skip_group_check=True),
                              reads=[Boh], writes=[BpI], signal=(c == 3))
                    yield
                xf, Bxf = idxf.next()
                xi, Bxi = idxi.next()
                x8, Bx8 = idx8.next()
                ph.op("dve", lambda e, x8=x8, pI_t=pI_t: e.tensor_copy(out=x8[:], in_=pI_t[:, 0:8]), reads=[BpI], writes=[Bx8])
                x8v = x8[:].rearrange("p (c t) -> p c t", t=2)
                ph.op("dve", lambda e, xf=xf, x8v=x8v: e.scalar_tensor_tensor(out=xf[:], in0=x8v[:, :, 0], scalar=128.0, in1=x8v[:, :, 1], op0=ALU.mult, op1=ALU.add),
                      reads=[Bx8], writes=[Bxf])
                ph.op("dve", lambda e, xf=xf, xi=xi: e.tensor_copy(out=xi[:], in_=xf[:]), reads=[Bxf], writes=[Bxi])
                xs_t, Bxs = xs.next()
                for c in range(4):
                    ph.dma("pool", lambda e, xs_t=xs_t, xi=xi, c=c: e.indirect_dma_start(
                        out=xs_t[:, c, :], out_offset=None, in_=self.h2aug, in_offset=bass.IndirectOffsetOnAxis(ap=xi[:, c:c + 1], axis=0)),
                        reads=[Bxi], writes=[Bxs])
                res.append((xs_t, Bxs, xi, Bxi))

            def ffn(e_, wts, gath):
                (wg, Bwg), (wu, Bwu), (wd, Bwd) = wts
                xs_t, Bxs, xi, Bxi = gath
                g_t, Bg = gs.next()
                ph.op("dve", lambda e, g_t=g_t, xs_t=xs_t: e.tensor_tensor(out=g_t[:], in0=xs_t[:, :, D + e_], in1=xs_t[:, :, D + 16 + e_], op=ALU.add), reads=[Bxs], writes=[Bg])
                ph.op("dve", lambda e, g_t=g_t, xs_t=xs_t: e.tensor_tensor(out=g_t[:], in0=g_t[:], in1=xs_t[:, :, D + 32 + e_], op=ALU.add), reads=[Bxs, Bg], writes=[Bg])
                xT, BxT = xsT.next()
                for k2 in range(4):
                    tp, Btp = pTP.next()
                    for kk in range(2):
                        k = k2 * 2 + kk
                        for c in range(4):
                            ph.op("pe", lambda e, tp=tp, xs_t=xs_t, kk=kk, c=c, k=k: e.transpose(out=tp[:, kk * 4 + c, :], in_=xs_t[:, c, k * 128:(k + 1) * 128], identity=self.ident_b[:]),
                                  reads=[Bxs], writes=[Btp], signal=(kk == 1 and c == 3))
                    eng = "act" if k2 % 2 == 0 else "dve"
                    if eng == "act":
                        ph.op("act", lambda e, tp=tp, xT=xT, k2=k2: e.activation(out=xT[:, k2 * 2:k2 * 2 + 2, :], in_=tp[:].rearrange("p (k c) t -> p k (c t)", k=2), func=AF.Copy),
                              reads=[Btp], writes=[BxT])
                    else:
                        ph.op("dve", lambda e, tp=tp, xT=xT, k2=k2: e.tensor_copy(out=xT[:, k2 * 2:k2 * 2 + 2, :], in_=tp[:].rearrange("p (k c) t -> p k (c t)", k=2)),
                              reads=[Btp], writes=[BxT])
                    yield
                hT, BhT = hidT.next()
                for f in range(8):
                    pg_t, Bpg = pG.next()
                    pu_t, Bpu = pU.next()
                    for k in range(8):
                        ph.op("pe", lambda e, pg_t=pg_t, wg=wg, xT=xT, k=k, f=f: e.matmul(pg_t[:], lhsT=wg[:, k, f * 128:(f + 1) * 128], rhs=xT[:, k, :], start=(k == 0), stop=(k == 7)),
                              reads=[Bwg, BxT], writes=[Bpg], signal=(k == 7))
                    for k in range(8):
                        ph.op("pe", lambda e, pu_t=pu_t, wu=wu, xT=xT, k=k, f=f: e.matmul(pu_t[:], lhsT=wu[:, k, f * 128:(f + 1) * 128], rhs=xT[:, k, :], start=(k == 0), stop=(k == 7)),
                              reads=[Bwu, BxT], writes=[Bpu], signal=(k == 7))
                    sg_t, Bsg = sg.next()
                    ph.op("act", lambda e, sg_t=sg_t, pg_t=pg_t: e.activation(out=sg_t[:], in_=pg_t[:], func=AF.Silu), reads=[Bpg], writes=[Bsg])
                    ph.op("dve", lambda e, sg_t=sg_t, pu_t=pu_t, hT=hT, f=f: e.tensor_tensor(out=hT[:, f, :], in0=pu_t[:], in1=sg_t[:], op=ALU.mult), reads=[Bpu, Bsg], writes=[BhT])
                    yield
                for c in range(4):
                    y_t, By = ysb.next()
                    for n in range(2):
                        py_t, Bpy = pY.next()
                        for f in range(8):
                            ph.op("pe", lambda e, py_t=py_t, hT=hT, wd=wd, f=f, c=c, n=n: e.matmul(py_t[:], lhsT=hT[:, f, c * 128:(c + 1) * 128], rhs=wd[:, f, n * 512:(n + 1) * 512],
                                                                                               start=(f == 0), stop=(f == 7)), reads=[BhT, Bwd], writes=[Bpy], signal=(f == 7))
                        ph.op("dve", lambda e, y_t=y_t, py_t=py_t, g_t=g_t, c=c, n=n: e.scalar_tensor_tensor(
                            out=y_t[:, n * 512:(n + 1) * 512], in0=py_t[:], scalar=g_t[:, c:c + 1], in1=self.G2bc[:, n * 512:(n + 1) * 512], op0=ALU.mult, op1=ALU.mult),
                            reads=[Bpy, Bg], writes=[By])
                    ph.dma("pool", lambda e, y_t=y_t, xi=xi, c=c: e.indirect_dma_start(
                        out=self.acc, out_offset=bass.IndirectOffsetOnAxis(ap=xi[:, c:c + 1], axis=0), in_=y_t[:], in_offset=None, compute_op=ALU.add),
                        reads=[By, Bxi] + (Bz if first_scatter[0] else []), writes=[Bacc])
                    first_scatter[0] = False
                    yield

            wts = self.pre_w
            r0 = []
            for _ in build_idx(0, r0):
                pass
            gath = r0[0]
            for e_ in range(NE):
                nwts = load_w(e_ + 1) if e_ + 1 < NE else None
                rn = []
                gi = build_idx(e_ + 1, rn) if e_ + 1 < NE else iter(())
                gf = ffn(e_, wts, gath)
                for _ in gi:
                    pass
                for _ in gf:
                    pass
                wts, gath = nwts, (rn[0] if rn else None)
            ph.finish()
            if "acc" in self.dbg:
                ph = Phase(nc, "dumpacc")
                B = Buf("d")
                ph.dma("sp", lambda e: e.dma_start(out=self.dbg["acc"], in_=self.acc), reads=[B], writes=[B])
                ph.op("dve", lambda e: e.memset(self.zeros[:, 0:1], 0.0), reads=[B], writes=[B])
                ph.finish()

    def resid(self, l, hsrc, hdst):
        nc = self.nc
        last = l == 1
        with contextlib.ExitStack() as es:
            hm = self.rb(es, "hmr", [128, D], F32, 3)
            ac = self.rb(es, "acr", [128, D], F32, 3)
            ot = self.rb(es, "otr", [128, D], F32, 2)
            ss = self.rb(es, "ssr", [128, 1], F32, 2)
            junk = self.sb(es, "junkf", [128, D], BF16)
            fg = self.sb(es, "fg", [128, D], F32)
            ph = Phase(nc, f"resid{l}")
            Bfg, Bj = Buf("fg"), Buf("j")
            if last:
                ph.dma("sp", lambda e: e.dma_start(out=fg[:], in_=self.final_g.partition_broadcast(128)), writes=[Bfg])
            loads = {}

            def issue_load(i):
                h_t, Bh = hm.next()
                a_t, Ba = ac.next()
                ph.dma("sp", lambda e, h_t=h_t, i=i: e.dma_start(out=h_t[:], in_=hsrc[i * 128:(i + 1) * 128, :]), writes=[Bh])
                ph.dma("sp", lambda e, a_t=a_t, i=i: e.dma_start(out=a_t[:], in_=self.acc[i * 128:(i + 1) * 128, :]), writes=[Ba])
                loads[i] = (h_t, Bh, a_t, Ba)
            issue_load(0)
            issue_load(1)
            for i in range(NT):
                if i + 2 < NT:
                    issue_load(i + 2)
                h_t, Bh, a_t, Ba = loads.pop(i)
                ph.op("dve", lambda e, a_t=a_t, h_t=h_t: e.tensor_tensor(out=h_t[:], in0=h_t[:], in1=a_t[:], op=ALU.add), reads=[Ba, Bh], writes=[Bh])
                if not last:
                    ph.dma("sp", lambda e, h_t=h_t, i=i: e.dma_start(out=hdst[i * 128:(i + 1) * 128, :], in_=h_t[:]), reads=[Bh])
                else:
                    s_t, Bs = ss.next()
                    o_t, Bo = ot.next()
                    ph.op("act", lambda e, h_t=h_t, s_t=s_t: e.activation(out=junk[:], in_=h_t[:], func=AF.Square, accum_out=s_t[:]), reads=[Bh], writes=[Bj, Bs])
                    ph.op("dve", lambda e, s_t=s_t: e.tensor_scalar(out=s_t[:], in0=s_t[:], scalar1=1.0 / D, scalar2=EPS, op0=ALU.mult, op1=ALU.add), reads=[Bs], writes=[Bs])
                    ph.op("pool", lambda e, s_t=s_t: e.tensor_tensor(out=s_t[:], in0=s_t[:], in1=self.mhalf[:, 0:1], op=ALU.pow), reads=[Bs], writes=[Bs])
                    ph.op("dve", lambda e, o_t=o_t, h_t=h_t, s_t=s_t: e.scalar_tensor_tensor(out=o_t[:], in0=h_t[:], scalar=s_t[:, 0:1], in1=fg[:], op0=ALU.mult, op1=ALU.mult),
                          reads=[Bh, Bs, Bfg], writes=[Bo])
                    ph.dma("sp", lambda e, o_t=o_t, i=i: e.dma_start(out=hdst[i * 128:(i + 1) * 128, :], in_=o_t[:]), reads=[Bo])
            ph.finish()

    def norm1_tile(self, ph, R, src, t, hT, BhT, x_t, Bx):
        BhTa = BhT.__dict__.setdefault("twin", Buf(BhT.name + "a"))
        ss, Bss = R["ssq"].next()
        xn_t, Bxn = R["xn"].next()
        p_t, BpT = R["pT"].next()
        lq = R.get("lq", "sp")
        ph.dma(lq, lambda e: e.dma_start(out=x_t[:], in_=src), writes=[Bx])
        if "fuse" in R:
            asrc, dst = R["fuse"](t)
            a_t, Ba = R["acr"].next()
            ph.dma(lq, lambda e: e.dma_start(out=a_t[:], in_=asrc), writes=[Ba])
            ph.op("dve", lambda e: e.tensor_tensor(out=x_t[:], in0=x_t[:], in1=a_t[:], op=ALU.add), reads=[Ba, Bx], writes=[Bx])
            ph.dma("sp", lambda e: e.dma_start(out=dst, in_=x_t[:]), reads=[Bx])
        ph.op("act", lambda e: e.activation(out=R["junk"][:], in_=x_t[:], func=AF.Square, accum_out=ss[:]), reads=[Bx], writes=[R["Bjunk"], Bss])
        ph.op("dve", lambda e: e.tensor_scalar(out=ss[:], in0=ss[:], scalar1=1.0 / D, scalar2=EPS, op0=ALU.mult, op1=ALU.add), reads=[Bss], writes=[Bss])
        ph.op("pool", lambda e: e.tensor_tensor(out=ss[:], in0=ss[:], in1=self.mhalf[:, 0:1], op=ALU.pow), reads=[Bss], writes=[Bss])
        ph.op("dve", lambda e: e.tensor_scalar(out=xn_t[:], in0=x_t[:], scalar1=ss[:, 0:1], scalar2=None, op0=ALU.mult), reads=[Bx, Bss], writes=[Bxn])
        for k in range(8):
            ph.op("pe", lambda e, k=k: e.transpose(out=p_t[:, k, :], in_=xn_t[:, k * 128:(k + 1) * 128], identity=self.ident_b[:]),
                  reads=[Bxn], writes=[BpT], signal=(k == 7))
        for k in range(8):
            wr = [BhT if t % 2 == 0 else BhTa] if k in (0, 7) else []
            if t % 2 == 0:
                ph.op("dve", lambda e, k=k: e.tensor_scalar(out=hT[:, k, t * 128:(t + 1) * 128], in0=p_t[:, k, :], scalar1=self.A1T[:, k, 0:1],
                                                            scalar2=self.mT[:, k, 0:1], op0=ALU.mult, op1=ALU.add), reads=[BpT], writes=wr)
            else:
                ph.op("act", lambda e, k=k: e.activation(out=hT[:, k, t * 128:(t + 1) * 128], in_=p_t[:, k, :], func=AF.Identity,
                                                         scale=self.A1T[:, k, 0:1], bias=self.mT[:, k, 0:1]), reads=[BpT], writes=wr)

    def l1p1(self, es_out):
        nc = self.nc
        self.cxT = self.sb(es_out, "cxT", [128, 8, S + 2], BF16)
        with contextlib.ExitStack() as es:
            w_cx = self.sb(es, "w_cx", [128, 8, 2 * D], BF16)
            R = {"ssq": self.rb(es, "ssq", [128, 1], F32, 3), "xn": self.rb(es, "xn", [128, D], BF16, 2),
                 "pT": self.rps(es, "pT", [128, 8, 128], BF16, 2), "junk": self.sb(es, "junk", [128, D], BF16), "Bjunk": Buf("junk")}
            xt = self.rb(es, "xt", [128, D], F32, 3)
            hnT = self.rb(es, "hnT", [128, 8, 512], BF16, 2)
            xvs = self.rb(es, "xvs", [128, 512], F32, 2)
            acr = self.rb(es, "acr1", [128, D], F32, 2)
            pC = self.rps(es, "pC", [128, 512], F32, 2)
            pX = self.rps(es, "pX", [128, 512], F32, 2)
            ph = Phase(nc, "l1p1")
            Bw, Bout = Buf("w"), Buf("out")
            ph.dma("pool", lambda e: e.dma_start(out=w_cx[:], in_=self.sc_w_in[:, D:3 * D].rearrange("(k p) n -> p k n", p=128)), writes=[Bw])
            ph.op("pool", lambda e: e.memset(self.cxT[:, :, 0:1], 0.0), writes=[])
            ph.op("pool", lambda e: e.memset(self.cxT[:, :, S + 1:S + 2], 0.0), writes=[])
            for st in range(8):
                hT, BhT = hnT.next()
                R["acr"] = acr
                R["lq"] = "act"
                R["fuse"] = lambda t, st=st: (self.acc[(st * 4 + t) * 128:(st * 4 + t + 1) * 128, :], self.hbuf2[(st * 4 + t) * 128:(st * 4 + t + 1) * 128, :])
                for t in range(4):
                    i = st * 4 + t
                    x_t, Bx = xt.next()
                    self.norm1_tile(ph, R, self.hbuf[i * 128:(i + 1) * 128, :], t, hT, BhT, x_t, Bx)
                for ch in range(8):
                    pc_t, Bpc = pC.next()
                    px_t, Bpx = pX.next()
                    for k in range(8):
                        ph.op("pe", lambda e, pc_t=pc_t, k=k, ch=ch, hT=hT: e.matmul(pc_t[:], lhsT=w_cx[:, k, ch * 128:(ch + 1) * 128], rhs=hT[:, k, :], start=(k == 0), stop=(k == 7)),
                              reads=[Bw, BhT, BhT.twin], writes=[Bpc], signal=(k == 7))
                    for k in range(8):
                        ph.op("pe", lambda e, px_t=px_t, k=k, ch=ch, hT=hT: e.matmul(px_t[:], lhsT=w_cx[:, k, D + ch * 128:D + (ch + 1) * 128], rhs=hT[:, k, :], start=(k == 0), stop=(k == 7)),
                              reads=[Bw, BhT, BhT.twin], writes=[Bpx], signal=(k == 7))
                    xv_t, Bxv = xvs.next()
                    ph.op("act", lambda e, xv_t=xv_t, px_t=px_t: e.activation(out=xv_t[:], in_=px_t[:], func=AF.Copy), reads=[Bpx], writes=[Bxv])
                    ph.op("dve", lambda e, xv_t=xv_t, pc_t=pc_t, ch=ch, st=st: e.tensor_tensor(out=self.cxT[:, ch, 1 + st * 512:1 + (st + 1) * 512], in0=pc_t[:], in1=xv_t[:], op=ALU.mult),
                          reads=[Bpc, Bxv], writes=[])
            ph.finish()

    def l1p2(self):
        nc = self.nc
        with contextlib.ExitStack() as es:
            w_b = self.sb(es, "w_b", [128, 8, D], BF16)
            w_o = self.sb(es, "w_o", [128, 8, D], BF16)
            dg3 = self.sb(es, "dg3", [128, 3, 8, 128], BF16)
            cw3 = self.sb(es, "cw3", [128, 8, 3], F32)
            R = {"ssq": self.rb(es, "ssq", [128, 1], F32, 3), "xn": self.rb(es, "xn", [128, D], BF16, 2),
                 "pT": self.rps(es, "pT", [128, 8, 128], BF16, 1), "junk": self.sb(es, "junk", [128, D], BF16), "Bjunk": Buf("junk")}
            xt = self.rb(es, "xt", [128, D], F32, 5)
            hnT = self.rb(es, "hnT", [128, 8, 512], BF16, 1)
            zT = self.rb(es, "zT", [128, 8, 512], BF16, 1)
            ycs = self.rb(es, "ycs", [128, 512], F32, 2)
            T = self.tail_alloc(es)
            pB = self.rps(es, "pB", [128, 512], F32, 2)
            pYc = self.rps(es, "pYc", [128, 512], F32, 1)
            po = self.rps(es, "po", [128, 512], F32, 2)
            ph = Phase(nc, "l1p2")
            Bw, Bcw, Bdg = Buf("w"), Buf("cw"), Buf("dg")
            ph.dma("pool", lambda e: e.dma_start(out=w_b[:], in_=self.sc_w_in[:, 0:D].rearrange("(k p) n -> p k n", p=128)), writes=[Bw])
            ph.dma("pool", lambda e: e.dma_start(out=w_o[:], in_=self.sc_w_out.rearrange("(k p) n -> p k n", p=128)), writes=[Bw])
            ph.dma("sp", lambda e: e.dma_start(out=cw3[:], in_=self.sc_conv_wT), writes=[Bcw])
            for j in range(3):
                for ch in range(8):
                    ph.op("dve", lambda e, j=j, ch=ch: e.tensor_scalar(out=dg3[:, j, ch, :], in0=self.ident_b[:], scalar1=cw3[:, ch, j:j + 1], scalar2=None, op0=ALU.mult),
                          reads=[Bcw], writes=[Bdg])
            for st in range(8):
                hT, BhT = hnT.next()
                z_t, Bz = zT.next()
                tiles = []
                R["lq"] = "act"
                for t in range(4):
                    i = st * 4 + t
                    x_t, Bx = xt.next()
                    self.norm1_tile(ph, R, self.hbuf2[i * 128:(i + 1) * 128, :], t, hT, BhT, x_t, Bx)
                    tiles.append((x_t, Bx))
                for ch in range(8):
                    pb_t, Bpb = pB.next()
                    py_t, Bpy = pYc.next()
                    for k in range(8):
                        ph.op("pe", lambda e, pb_t=pb_t, k=k, ch=ch, hT=hT: e.matmul(pb_t[:], lhsT=w_b[:, k, ch * 128:(ch + 1) * 128], rhs=hT[:, k, :], start=(k == 0), stop=(k == 7)),
                              reads=[Bw, BhT, BhT.twin], writes=[Bpb], signal=(k == 7))
                    for j in range(3):
                        ph.op("pe", lambda e, py_t=py_t, j=j, ch=ch, st=st: e.matmul(py_t[:], lhsT=dg3[:, j, ch, :], rhs=self.cxT[:, ch, st * 512 + j:st * 512 + j + 512], start=(j == 0), stop=(j == 2)),
                              reads=[Bdg], writes=[Bpy], signal=(j == 2))
                    yc_t, Byc = ycs.next()
                    ph.op("act", lambda e, yc_t=yc_t, py_t=py_t: e.activation(out=yc_t[:], in_=py_t[:], func=AF.Copy), reads=[Bpy], writes=[Byc])
                    ph.op("dve", lambda e, yc_t=yc_t, pb_t=pb_t, z_t=z_t, ch=ch: e.tensor_tensor(out=z_t[:, ch, :], in0=pb_t[:], in1=yc_t[:], op=ALU.mult), reads=[Bpb, Byc], writes=[Bz])
                for t in range(4):
                    i = st * 4 + t
                    pol = []
                    for n in range(2):
                        po_t, Bpo = po.next()
                        for ch in range(8):
                            ph.op("pe", lambda e, po_t=po_t, z_t=z_t, ch=ch, n=n, t=t: e.matmul(po_t[:], lhsT=z_t[:, ch, t * 128:(t + 1) * 128], rhs=w_o[:, ch, n * 512:(n + 1) * 512],
                                                                                              start=(ch == 0), stop=(ch == 7)), reads=[Bz, Bw], writes=[Bpo], signal=(ch == 7))
                        pol.append((po_t[:], Bpo))
                    x_t, Bx = tiles[t]
                    self.tail_tile(ph, T, i, pol, x_t, Bx, self.hbuf)
            ph.finish()

    def run(self):
        nc = self.nc
        self.declare()
        for nm, shape, dt in (("mrow", [2, 6 * D], F32), ("qT", [128, 4, S], BF16), ("kT2", [128, 2, NKT * 128], BF16),
                              ("Vaug", [128, NKT, 2, 65], BF16), ("gT", [128, 4, S + 30], BF16), ("attnT", [128, 4, S], BF16),
                              ("convT", [128, 4, S], BF16), ("aff", [128, NT, NE], F32), ("hmid", [S, D], F32), ("h2aug", [S, RW], BF16),
                              ("slotm", [128, NT * NE], F32), ("thr", [16, 8], F32), ("acc", [S, D], F32), ("h1", [S, D], F32)):
            self.dbg_out(nm, shape, dt)
        with contextlib.ExitStack() as es0:
            _SEMS[0] = Sems(nc, es0)
            self.consts(es0)
            self.ada(0)
            with contextlib.ExitStack() as es_l0:
                self.l0a(es_l0)
                if self.stop == "l0a":
                    self.es_a.close()
                    self.finish_dummy()
                    return
                self.attn(es_l0)
                self.es_a.close()
                if self.stop == "attn":
                    self.finish_dummy()
                    return
                self.conv(es_l0)
                self.outproj0()
                if self.stop == "outproj0":
                    self.finish_dummy()
                    return
            with contextlib.ExitStack() as es_m:
                self.route(es_m, 0)
                if self.stop == "route0":
                    self.finish_dummy()
                    return
                self.moe(0)
            if self.stop == "moe0":
                self.dump_h2()
                self.finish_dummy()
                return
            self.ada(1)
            with contextlib.ExitStack() as es_l1:
                self.l1p1(es_l1)
                self.l1p2()
            if self.stop == "l1":
                self.dump_tail()
                self.finish_dummy()
                return
            with contextlib.ExitStack() as es_m:
                self.route(es_m, 1)
                self.moe(1)
            self.resid(1, self.hbuf, self.out)

    def finish_dummy(self):
        ph = Phase(self.nc, "fin")
        B = Buf("z")
        ph.dma("sp", lambda e: e.dma_start(out=self.out[0:128, :], in_=self.zeros[:, 0:D]), reads=[B])
        ph.finish()


def build(debug=None, stop=None):
    nc = bass.Bass("TRN2", target_bir_lowering=False)
    k = Kern(nc, debug, stop)
    k.run()
    return nc, k


def rope_tables():
    rows = S // 64
    row = np.repeat(np.arange(rows, dtype=np.float32), 64)
    col = np.tile(np.arange(64, dtype=np.float32), rows)
    inv = (10000.0 ** (-np.arange(0, 32, 2, dtype=np.float32) / 32)).astype(np.float32)
    ang = np.concatenate([row[:, None] * inv, col[:, None] * inv], axis=-1).astype(np.float32)
    cos = np.cos(ang).astype(np.float32).reshape(NT, 128, 32).transpose(1, 0, 2)
    sin = np.sin(ang).astype(np.float32).reshape(NT, 128, 32).transpose(1, 0, 2)
    return np.ascontiguousarray(cos), np.ascontiguousarray(sin)


def prep_inputs(inp):
    f = lambda a: np.ascontiguousarray(np.asarray(a, dtype=np.float32))
    cos, sin = rope_tables()
    shared = {
        "ada_w": f(inp["ada_w"]),
        "ada_b": f(inp["ada_b"]).reshape(2, 1, 6 * D),
        "g1T": f(np.asarray(inp["norm1_g"]).reshape(2, 8, 128).transpose(0, 2, 1)),
        "g2row": f(inp["norm2_g"]).reshape(2, 1, D),
        "ev_w_in": f(inp["ev_w_in"][0]),
        "ev_w_out": f(inp["ev_w_out"][0]),
        "qkg": f(np.concatenate([np.tile(np.asarray(inp["ev_q_g"][0]), 8), np.tile(np.asarray(inp["ev_k_g"][0]), 2)])).reshape(1, 640),
        "qg": f(inp["ev_q_g"][0]).reshape(1, 64),
        "kg": f(inp["ev_k_g"][0]).reshape(1, 64),
        "conv_wT": f(np.asarray(inp["ev_conv_w"][0]).reshape(31, 4, 128).transpose(2, 1, 0)),
        "conv_vec": f(np.stack([np.asarray(inp["ev_conv_b"][0]).reshape(4, 128).T,
                                np.asarray(inp["ev_ln_g"][0]).reshape(4, 128).T,
                                np.asarray(inp["ev_ln_b"][0]).reshape(4, 128).T], axis=1)),
        "rope_cos": cos,
        "rope_sin": sin,
        "sc_w_in": f(inp["sc_w_in"][0]),
        "sc_conv_wT": f(np.asarray(inp["sc_conv_w"][0]).reshape(3, 8, 128).transpose(2, 1, 0)),
        "sc_w_out": f(inp["sc_w_out"][0]),
        "w_r": f(np.asarray(inp["moe_w_r"]).reshape(2, 8, 128, NE).transpose(0, 2, 1, 3)),
        "w_gate": f(inp["moe_w_gate"]),
        "w_up": f(inp["moe_w_up"]),
        "w_down": f(inp["moe_w_down"]),
        "final_g": f(inp["final_g"]).reshape(1, D),
    }
    maps = []
    c = np.asarray(inp["c"], dtype=np.float32)
    cc = np.asarray(inp["c_ctx"], dtype=np.float32)
    for core in range(8):
        b = core % 4
        m = dict(shared)
        m["x"] = f(inp["x"][b])
        m["ctx"] = f(inp["ctx"][b])
        m["cT"] = f(np.stack([c[b].reshape(8, 128).T, cc.reshape(8, 128).T], axis=-1))
        maps.append(m)
    return maps


_CACHE = {}


def kernel(**inputs):
    if "nc" not in _CACHE:
        _CACHE["nc"] = build()[0]
    nc = _CACHE["nc"]
    maps = prep_inputs(inputs)
    res = run_bass_kernel_spmd(nc, maps, core_ids=list(range(8)))
    out = np.stack([np.asarray(res.results[b]["out"], dtype=np.float32) for b in range(4)], axis=0)
    return out
```

```python
import contextlib
import numpy as np
import concourse.bass as bass
import concourse.mybir as mybir
from concourse.bass_utils import run_bass_kernel_spmd

F32 = mybir.dt.float32
BF16 = mybir.dt.bfloat16
U32 = mybir.dt.uint32
I32 = mybir.dt.int32
ALU = mybir.AluOpType
AF = mybir.ActivationFunctionType
AX = mybir.AxisListType

ENGS = ["pe", "dve", "act", "pool", "sp"]
CENGS = ["pe", "dve", "act", "pool"]
NDS = 24
NDS_HW = 16
SAME_ENGINE_SYNC = True

D = 1024
S = 4096
CTX = 256
NT = S // 128
NKT = NT + 2
EPS = 1e-6
NE = 16
CAP = 512
RW = 1072


class Buf:
    def __init__(self, name):
        self.name = name
        self.w = None
        self.r = {}


class Sems:
    def __init__(self, nc, es):
        self.esem = {e: es.enter_context(nc.semaphore(f"s_{e}")) for e in CENGS}
        self.ecnt = {e: 0 for e in CENGS}
        self.dsem = [es.enter_context(nc.semaphore(f"s_d{i}")) for i in range(NDS)]
        self.dcnt = [0] * NDS
        self.dnext = 0
        self.dnext_sw = 0
        self.waited = {e: {} for e in ENGS}


_SEMS = [None]


class Phase:
    def __init__(self, nc, name):
        self.nc = nc
        self.name = name
        sm = _SEMS[0]
        self.sm = sm
        self.q = {e: [] for e in ENGS}
        self.esem = sm.esem
        self.ecnt = sm.ecnt
        self.pending = {e: False for e in CENGS}
        self.dsem = sm.dsem
        self.dcnt = sm.dcnt
        self.waited = sm.waited

    def _sem(self, key):
        return self.esem[key[1]] if key[0] == "e" else self.dsem[key[1]]

    def _wait(self, engine, tok, force=False):
        if tok is None:
            return
        key, val = tok
        if not force and key[0] == "e" and key[1] == engine and (engine == "pe" or not SAME_ENGINE_SYNC):
            return
        if self.waited[engine].get(key, 0) >= val:
            return
        self.waited[engine][key] = val
        sem = self._sem(key)
        self.q[engine].append(lambda eng, sem=sem, val=val: eng.wait_ge(sem, val))

    def _deps(self, engine, reads, writes):
        for b in reads:
            self._wait(engine, b.w)
        for b in writes:
            self._wait(engine, b.w)
            for t in b.r.values():
                self._wait(engine, t)

    def _update(self, tok, reads, writes):
        for b in reads:
            b.r[tok[0]] = tok
        for b in writes:
            b.w = tok
            b.r = {}

    def op(self, engine, fn, reads=(), writes=(), signal=True):
        self._deps(engine, reads, writes)
        if signal:
            self.ecnt[engine] += 1
            tok = (("e", engine), self.ecnt[engine])
            sem = self.esem[engine]
            self.q[engine].append(lambda eng, fn=fn, sem=sem: fn(eng).then_inc(sem, 1))
            self.pending[engine] = False
        else:
            tok = (("e", engine), self.ecnt[engine] + 1)
            self.q[engine].append(lambda eng, fn=fn: fn(eng))
            self.pending[engine] = True
        self._update(tok, reads, writes)
        return tok

    def dma(self, queue, fn, reads=(), writes=()):
        self._deps(queue, reads, writes)
        sm = self.sm
        if queue == "pool":
            i = NDS_HW + sm.dnext_sw
            sm.dnext_sw = (sm.dnext_sw + 1) % (NDS - NDS_HW)
        else:
            i = sm.dnext
            sm.dnext = (sm.dnext + 1) % NDS_HW
        if self.dcnt[i] > 0:
            self._wait(queue, (("d", i), self.dcnt[i]))
        self.dcnt[i] += 16
        tok = (("d", i), self.dcnt[i])
        sem = self.dsem[i]
        self.q[queue].append(lambda eng, fn=fn, sem=sem: fn(eng).then_inc(sem, 16))
        self._update(tok, reads, writes)
        return tok

    def finish(self):
        nc = self.nc
        for e in CENGS:
            assert not self.pending[e], f"engine {e} has pending unsignaled ops in {self.name}"
        for e in ENGS:
            for e2 in CENGS:
                if self.ecnt[e2] > 0:
                    self._wait(e, (("e", e2), self.ecnt[e2]), force=True)
            for i in range(NDS):
                if self.dcnt[i] > 0:
                    self._wait(e, (("d", i), self.dcnt[i]), force=True)
        q = self.q
        with nc.Block() as block:
            @block.tensor
            def _(eng):
                for f in q["pe"]:
                    f(eng)

            @block.vector
            def _(eng):
                for f in q["dve"]:
                    f(eng)

            @block.scalar
            def _(eng):
                for f in q["act"]:
                    f(eng)

            @block.gpsimd
            def _(eng):
                for f in q["pool"]:
                    f(eng)

            @block.sync
            def _(eng):
                for f in q["sp"]:
                    f(eng)


class Ring:
    def __init__(self, items):
        self.items = items
        self.i = 0

    def next(self):
        it = self.items[self.i % len(self.items)]
        self.i += 1
        return it


def v3(ap, d):
    return ap.rearrange("p (h d) -> p h d", d=d)


class Kern:
    def __init__(self, nc, debug=None, stop=None):
        self.nc = nc
        self.debug = debug or []
        self.stop = stop
        self.dbg = {}
        self.uid = 0

    def sb(self, es, name, shape, dt):
        self.uid += 1
        return es.enter_context(self.nc.sbuf_tensor(f"{name}_{self.uid}", shape, dt))

    def ps(self, es, name, shape, dt):
        self.uid += 1
        return es.enter_context(self.nc.psum_tensor(f"{name}_{self.uid}", shape, dt))

    def rb(self, es, name, shape, dt, n):
        return Ring([(self.sb(es, f"{name}{i}", shape, dt), Buf(f"{name}{i}")) for i in range(n)])

    def rps(self, es, name, shape, dt, n):
        return Ring([(self.ps(es, f"{name}{i}", shape, dt), Buf(f"{name}{i}")) for i in range(n)])

    def dram_in(self, name, shape, dt=F32):
        return self.nc.dram_tensor(name, list(shape), dt, kind="ExternalInput").ap()

    def dram_out(self, name, shape, dt=F32):
        return self.nc.dram_tensor(name, list(shape), dt, kind="ExternalOutput").ap()

    def dram_tmp(self, name, shape, dt=F32):
        return self.nc.dram_tensor(name, list(shape), dt, kind="Internal").ap()

    def dbg_out(self, name, shape, dt=F32):
        if name in self.debug:
            self.dbg[name] = self.dram_out("dbg_" + name, shape, dt)
            return self.dbg[name]
        return None

    def declare(self):
        self.x = self.dram_in("x", [S, D])
        self.ctx = self.dram_in("ctx", [CTX, D])
        self.cT = self.dram_in("cT", [128, 8, 2])
        self.ada_w = self.dram_in("ada_w", [2, D, 6 * D])
        self.ada_b = self.dram_in("ada_b", [2, 1, 6 * D])
        self.g1T = self.dram_in("g1T", [2, 128, 8])
        self.g2row = self.dram_in("g2row", [2, 1, D])
        self.ev_w_in = self.dram_in("ev_w_in", [D, 1792])
        self.ev_w_out = self.dram_in("ev_w_out", [D, D])
        self.qkg = self.dram_in("qkg", [1, 640])
        self.qg = self.dram_in("qg", [1, 64])
        self.kg = self.dram_in("kg", [1, 64])
        self.conv_wT = self.dram_in("conv_wT", [128, 4, 31])
        self.conv_vec = self.dram_in("conv_vec", [128, 3, 4])
        self.rope_cos = self.dram_in("rope_cos", [128, NT, 32])
        self.rope_sin = self.dram_in("rope_sin", [128, NT, 32])
        self.sc_w_in = self.dram_in("sc_w_in", [D, 3 * D])
        self.sc_conv_wT = self.dram_in("sc_conv_wT", [128, 8, 3])
        self.sc_w_out = self.dram_in("sc_w_out", [D, D])
        self.w_r = self.dram_in("w_r", [2, 128, 8, NE])
        self.w_gate = self.dram_in("w_gate", [2, NE, D, D])
        self.w_up = self.dram_in("w_up", [2, NE, D, D])
        self.w_down = self.dram_in("w_down", [2, NE, D, D])
        self.final_g = self.dram_in("final_g", [1, D])
        self.Gm = self.dram_in("Gm", [128, 128])
        self.out = self.dram_out("out", [S, D])
        self.hbuf = self.dram_tmp("hbuf", [S, D])
        self.hbuf2 = self.dram_tmp("hbuf2", [S, D])
        self.acc = self.dram_tmp("acc", [S, D])
        self.h2aug = self.dram_tmp("h2aug", [S, RW], BF16)

    def consts(self, es):
        nc = self.nc
        self.ident_f = self.sb(es, "ident_f", [128, 128], F32)
        self.ident_b = self.sb(es, "ident_b", [128, 128], BF16)
        self.ones_b = self.sb(es, "ones_b", [128, 128], BF16)
        self.ones_f = self.sb(es, "ones_f", [128, 128], F32)
        self.ustr_b = self.sb(es, "ustr_b", [128, 128], BF16)
        self.sel2 = self.sb(es, "sel2", [2, 128], F32)
        self.iota512 = self.sb(es, "iota512", [128, 512], mybir.dt.float16)
        self.tokval = self.sb(es, "tokval", [128, NT, 2], BF16)
        self.mhalf = self.sb(es, "mhalf", [128, 16], F32)
        self.negC = self.sb(es, "negC", [128, 1], F32)
        self.zeros = self.sb(es, "zeros", [128, 1024], F32)
        self.mT = self.sb(es, "mT", [128, 48, 2], F32)
        self.A1T = self.sb(es, "A1T", [128, 8, 2], F32)
        self.G1bc = self.sb(es, "G1bc", [128, D], F32)
        self.A2bc = self.sb(es, "A2bc", [128, D], F32)
        self.B2bc = self.sb(es, "B2bc", [128, D], F32)
        self.G2bc = self.sb(es, "G2bc", [128, D], F32)
        self.wr_b = self.sb(es, "wr_b", [128, 8, NE], BF16)
        self.affTM = self.sb(es, "affTM", [128, NT, NE], F32)
        with contextlib.ExitStack() as es2:
            ii = self.sb(es2, "ii", [128, 512], I32)
            i2 = self.sb(es2, "i2", [128, NT], I32)
            i3 = self.sb(es2, "i3", [128, 1], I32)
            uf = self.sb(es2, "uf", [128, 128], F32)
            gq = self.sb(es2, "gq", [128, 128], F32)
            mx = self.sb(es2, "mx", [128, 2], F32)
            ph = Phase(nc, "const")
            B = Buf("c")
            W = dict(reads=[B], writes=[B])
            ph.op("pool", lambda e: e.memset(self.ident_f[:], 0.0), **W)
            ph.op("pool", lambda e: e.affine_select(out=self.ident_f[:], in_=self.ident_f[:], pattern=[[-1, 128]],
                                                    compare_op=ALU.not_equal, fill=1.0, base=0, channel_multiplier=1), **W)
            ph.op("dve", lambda e: e.tensor_copy(out=self.ident_b[:], in_=self.ident_f[:]), **W)
            ph.op("dve", lambda e: e.memset(self.ones_b[:], 1.0), **W)
            ph.op("dve", lambda e: e.memset(self.ones_f[:], 1.0), **W)
            ph.op("pool", lambda e: e.memset(uf[:], 0.0), **W)
            ph.op("pool", lambda e: e.affine_select(out=uf[:], in_=uf[:], pattern=[[-1, 128]],
                                                    compare_op=ALU.is_ge, fill=1.0, base=0, channel_multiplier=1), **W)
            ph.op("dve", lambda e: e.tensor_copy(out=self.ustr_b[:], in_=uf[:]), **W)
            ph.op("dve", lambda e: e.memset(self.sel2[:], 0.0), **W)
            ph.op("dve", lambda e: e.memset(self.sel2[0:1, :], 1.0), **W)
            ph.op("pool", lambda e: e.iota(ii[:], pattern=[[1, 512]], base=0, channel_multiplier=0), **W)
            ph.op("dve", lambda e: e.tensor_copy(out=self.iota512[:], in_=ii[:]), **W)
            ph.op("pool", lambda e: e.iota(i2[:], pattern=[[1, NT]], base=0, channel_multiplier=0), **W)
            ph.op("pool", lambda e: e.iota(i3[:], pattern=[[0, 1]], base=0, channel_multiplier=1), **W)
            ph.op("dve", lambda e: e.tensor_copy(out=self.tokval[:, :, 0], in_=i2[:]), **W)
            ph.op("dve", lambda e: e.tensor_copy(out=self.tokval[:, :, 1], in_=i3[:].broadcast_to([128, NT])), **W)
            ph.op("dve", lambda e: e.memset(self.mhalf[:], -0.5), **W)
            ph.op("dve", lambda e: e.memset(self.zeros[:], 0.0), **W)
            ph.dma("sp", lambda e: e.dma_start(out=gq[:, 0:64], in_=self.qg.partition_broadcast(128)), **W)
            ph.dma("sp", lambda e: e.dma_start(out=gq[:, 64:128], in_=self.kg.partition_broadcast(128)), **W)
            ph.op("dve", lambda e: e.tensor_reduce(out=mx[:], in_=v3(gq[:], 64), axis=AX.X, op=ALU.max,
                                                   apply_absolute_value=True), **W)
            ph.op("dve", lambda e: e.scalar_tensor_tensor(out=self.negC[:], in0=mx[:, 0:1], scalar=-8.0, in1=mx[:, 1:2],
                                                          op0=ALU.mult, op1=ALU.mult), **W)
            ph.finish()

    def ada(self, l):
        nc = self.nc
        with contextlib.ExitStack() as es:
            cT_s = self.sb(es, "cT_s", [128, 8, 2], F32)
            scT = self.sb(es, "scT", [128, 8, 2], F32)
            wblk = self.rb(es, "wblk", [128, 8, 512], F32, 2)
            brow = self.sb(es, "brow", [1, 6 * D], F32)
            mrow = self.sb(es, "mrow", [2, 6 * D], F32)
            g2r = self.sb(es, "g2r", [2, D], F32)
            a2row = self.sb(es, "a2row", [2, D], F32)
            g1T_s = self.sb(es, "g1T_s", [128, 8], F32)
            tmpA = self.sb(es, "tmpA", [128, 8, 2], F32)
            wr_f = self.sb(es, "wr_f", [128, 8, NE], F32)
            pm = self.rps(es, "pm", [128, 512], F32, 2)
            pTm = self.ps(es, "pTm", [128, 512], F32)
            pb = self.rps(es, "pb", [128, 512], F32, 2)
            ph = Phase(nc, f"ada{l}")
            Bc, Bsc, Bbrow, Bmrow, Bg2, Ba2, Bg1, BmT, BpT, Bvec, Bwr = [Buf(n) for n in "c sc brow mrow g2 a2 g1 mT pT vec wr".split()]
            ph.dma("sp", lambda e: e.dma_start(out=cT_s[:], in_=self.cT), writes=[Bc])
            ph.dma("sp", lambda e: e.dma_start(out=brow[:], in_=self.ada_b[l]), writes=[Bbrow])
            ph.dma("sp", lambda e: e.dma_start(out=g2r[:], in_=self.g2row[l].partition_broadcast(2)), writes=[Bg2])
            ph.dma("sp", lambda e: e.dma_start(out=g1T_s[:], in_=self.g1T[l]), writes=[Bg1])
            ph.dma("sp", lambda e: e.dma_start(out=wr_f[:], in_=self.w_r[l]), writes=[Bwr])
            ph.op("dve", lambda e: e.tensor_copy(out=self.wr_b[:], in_=wr_f[:]), reads=[Bwr], writes=[Bvec])
            ph.op("act", lambda e: e.activation(out=scT[:], in_=cT_s[:], func=AF.Silu), reads=[Bc], writes=[Bsc])
            for cb in range(12):
                wt, Bw = wblk.next()
                ph.dma("sp", lambda e, wt=wt, cb=cb: e.dma_start(
                    out=wt[:], in_=self.ada_w[l][:, cb * 512:(cb + 1) * 512].rearrange("(k p) n -> p k n", p=128)),
                    writes=[Bw])
                pt, Bp = pm.next()
                for k in range(8):
                    ph.op("pe", lambda e, pt=pt, wt=wt, k=k: e.matmul(pt[0:2, :], lhsT=scT[:, k, :], rhs=wt[:, k, :],
                                                                      start=(k == 0), stop=False),
                          reads=[Bsc, Bw], writes=[Bp], signal=False)
                ph.op("pe", lambda e, pt=pt, cb=cb: e.matmul(pt[0:2, :], lhsT=self.ones_f[0:1, 0:2],
                                                             rhs=brow[0:1, cb * 512:(cb + 1) * 512], start=False, stop=True),
                      reads=[Bbrow], writes=[Bp])
                ph.op("dve", lambda e, pt=pt, cb=cb: e.tensor_copy(out=mrow[:, cb * 512:(cb + 1) * 512], in_=pt[0:2, :]),
                      reads=[Bp], writes=[Bmrow])
            pTv = pTm[:, 0:96].rearrange("p (c t) -> p c t", t=2)
            for c in range(48):
                ph.op("pe", lambda e, c=c: e.transpose(out=pTv[:, c, :], in_=mrow[0:2, c * 128:(c + 1) * 128],
                                                       identity=self.ident_f[0:2, 0:2]),
                      reads=[Bmrow], writes=[BpT], signal=(c == 47))
            ph.op("dve", lambda e: e.tensor_copy(out=self.mT[:], in_=pTv), reads=[BpT], writes=[BmT])
            ph.op("dve", lambda e: e.tensor_scalar(out=tmpA[:], in0=self.mT[:, 8:16, :], scalar1=1.0, scalar2=None, op0=ALU.add),
                  reads=[BmT], writes=[Bvec])
            ph.op("dve", lambda e: e.tensor_tensor(out=self.A1T[:], in0=tmpA[:], in1=g1T_s[:].unsqueeze(2).broadcast_to([128, 8, 2]),
                                                   op=ALU.mult), reads=[Bvec, Bg1], writes=[Bvec])
            ph.op("dve", lambda e: e.tensor_scalar(out=a2row[:], in0=mrow[:, 4 * D:5 * D], scalar1=1.0, scalar2=None, op0=ALU.add),
                  reads=[Bmrow], writes=[Ba2])
            ph.op("dve", lambda e: e.tensor_tensor(out=a2row[:], in0=a2row[:], in1=g2r[:], op=ALU.mult),
                  reads=[Ba2, Bg2], writes=[Ba2])
            srcs = [(self.G1bc, mrow[:, 2 * D:3 * D], Bmrow), (self.A2bc, a2row[:], Ba2),
                    (self.B2bc, mrow[:, 3 * D:4 * D], Bmrow), (self.G2bc, mrow[:, 5 * D:6 * D], Bmrow)]
            for dst, src, Bs in srcs:
                for n in range(2):
                    pt, Bp = pb.next()
                    ph.op("pe", lambda e, pt=pt, src=src, n=n: e.matmul(pt[:], lhsT=self.sel2[:], rhs=src[0:2, n * 512:(n + 1) * 512],
                                                                         start=True, stop=True), reads=[Bs], writes=[Bp])
                    ph.op("act", lambda e, pt=pt, dst=dst, n=n: e.activation(out=dst[:, n * 512:(n + 1) * 512], in_=pt[:], func=AF.Copy),
                          reads=[Bp], writes=[Bvec])
            if "mrow" in self.dbg:
                ph.dma("sp", lambda e: e.dma_start(out=self.dbg["mrow"], in_=mrow[:]), reads=[Bmrow])
            ph.finish()

    def l0a(self, es_out):
        nc = self.nc
        self.gT = self.sb(es_out, "gT", [128, 4, S + 30], BF16)
        self.qT = self.sb(es_out, "qT", [128, 4, S], BF16)
        es_a = contextlib.ExitStack()
        self.es_a = es_a
        self.kT2 = self.sb(es_a, "kT2", [128, 2, NKT * 128], BF16)
        self.Vaug = self.sb(es_a, "Vaug", [128, NKT, 2, 65], BF16)
        with contextlib.ExitStack() as es:
            w_in_b = self.sb(es, "w_in_b", [128, 8, 1792], BF16)
            cos_t = self.sb(es, "cos_t", [128, NT, 32], F32)
            sin_t = self.sb(es, "sin_t", [128, NT, 32], F32)
            gq_bc = self.sb(es, "gq_bc", [128, 640], F32)
            junk = self.sb(es, "junk", [128, D], BF16)
            xt = self.rb(es, "xt", [128, D], F32, 3)
            ssq = self.rb(es, "ssq", [128, 1], F32, 3)
            xn = self.rb(es, "xn", [128, D], BF16, 2)
            hnT = self.rb(es, "hnT", [128, 8, 512], BF16, 2)
            sq = self.rb(es, "sq", [128, 640], F32, 1)
            s10 = self.rb(es, "s10", [128, 10], F32, 2)
            qk = self.rb(es, "qk", [128, 640], F32, 1)
            rt = self.rb(es, "rt", [128, 4, 320], F32, 1)
            qkr = self.rb(es, "qkr", [128, 640], BF16, 2)
            kd = self.rb(es, "kd", [128, 256], BF16, 2)
            sig = self.rb(es, "sig", [128, 512], F32, 1)
            pT = self.rps(es, "pT", [128, 8, 128], BF16, 2)
            pq = self.rps(es, "pq", [128, 512], F32, 2)
            pkv = self.rps(es, "pkv", [128, 512], F32, 1)
            pqk = self.rps(es, "pqk", [128, 8, 128], BF16, 1)
            pu = self.rps(es, "pu", [128, 512], F32, 1)
            pg = self.rps(es, "pg", [128, 512], F32, 1)
            ph = Phase(nc, "l0a")
            Bw, Bcs, Bgq, Bjunk, Bout = [Buf(n) for n in "w cs gq junk out".split()]
            ph.dma("pool", lambda e: e.dma_start(out=w_in_b[:], in_=self.ev_w_in.rearrange("(k p) n -> p k n", p=128)), writes=[Bw])
            ph.dma("sp", lambda e: e.dma_start(out=cos_t[:], in_=self.rope_cos), writes=[Bcs])
            ph.dma("sp", lambda e: e.dma_start(out=sin_t[:], in_=self.rope_sin), writes=[Bcs])
            ph.dma("sp", lambda e: e.dma_start(out=gq_bc[:], in_=self.qkg.partition_broadcast(128)), writes=[Bgq])
            ph.op("pool", lambda e: e.memset(self.gT[:, :, 0:15], 0.0), writes=[])
            ph.op("pool", lambda e: e.memset(self.gT[:, :, S + 15:S + 30], 0.0), writes=[])
            ph.op("pool", lambda e: e.memset(self.Vaug[:, :, :, 64:65], 1.0), writes=[])
            evq = 0
            for st in range(9):
                is_ctx = st == 0
                ntile = 2 if is_ctx else 4
                col = 1 if is_ctx else 0
                hT, BhT = hnT.next()
                BhTa = BhT.__dict__.setdefault("twin", Buf(BhT.name + "a"))
                for t in range(ntile):
                    if is_ctx:
                        src = self.ctx[t * 128:(t + 1) * 128, :]
                        kt = t
                        lt = None
                    else:
                        lt = (st - 1) * 4 + t
                        src = self.x[lt * 128:(lt + 1) * 128, :]
                        kt = 2 + lt
                    x_t, Bx = xt.next()
                    ss, Bss = ssq.next()
                    xn_t, Bxn = xn.next()
                    ph.dma("sp", lambda e, x_t=x_t, src=src: e.dma_start(out=x_t[:], in_=src), writes=[Bx])
                    ph.op("act", lambda e, x_t=x_t, ss=ss: e.activation(out=junk[:], in_=x_t[:], func=AF.Square, accum_out=ss[:]),
                          reads=[Bx], writes=[Bjunk, Bss])
                    ph.op("dve", lambda e, ss=ss: e.tensor_scalar(out=ss[:], in0=ss[:], scalar1=1.0 / D, scalar2=EPS, op0=ALU.mult, op1=ALU.add),
                          reads=[Bss], writes=[Bss])
                    ph.op("pool", lambda e, ss=ss: e.tensor_tensor(out=ss[:], in0=ss[:], in1=self.mhalf[:, 0:1], op=ALU.pow),
                          reads=[Bss], writes=[Bss])
                    ph.op("dve", lambda e, x_t=x_t, ss=ss, xn_t=xn_t: e.tensor_scalar(out=xn_t[:], in0=x_t[:], scalar1=ss[:, 0:1], scalar2=None, op0=ALU.mult),
                          reads=[Bx, Bss], writes=[Bxn])
                    p_t, BpT = pT.next()
                    for k in range(8):
                        ph.op("pe", lambda e, p_t=p_t, xn_t=xn_t, k=k: e.transpose(out=p_t[:, k, :], in_=xn_t[:, k * 128:(k + 1) * 128], identity=self.ident_b[:]),
                              reads=[Bxn], writes=[BpT], signal=(k == 7))
                    for k in range(8):
                        wr = [BhT if t % 2 == 0 else BhTa] if k in (0, 7) else []
                        if t % 2 == 0:
                            ph.op("dve", lambda e, p_t=p_t, hT=hT, k=k, t=t, col=col: e.tensor_scalar(
                                out=hT[:, k, t * 128:(t + 1) * 128], in0=p_t[:, k, :], scalar1=self.A1T[:, k, col:col + 1],
                                scalar2=self.mT[:, k, col:col + 1], op0=ALU.mult, op1=ALU.add), reads=[BpT], writes=wr)
                        else:
                            ph.op("act", lambda e, p_t=p_t, hT=hT, k=k, t=t, col=col: e.activation(
                                out=hT[:, k, t * 128:(t + 1) * 128], in_=p_t[:, k, :], func=AF.Identity,
                                scale=self.A1T[:, k, col:col + 1], bias=self.mT[:, k, col:col + 1]), reads=[BpT], writes=wr)
                    pkv_t, Bpkv = pkv.next()
                    for k in range(8):
                        ph.op("pe", lambda e, pkv_t=pkv_t, hT=hT, k=k, t=t: e.matmul(pkv_t[:, 0:256], lhsT=hT[:, k, t * 128:(t + 1) * 128], rhs=w_in_b[:, k, 512:768],
                                                                                   start=(k == 0), stop=(k == 7)), reads=[BhT, BhTa, Bw], writes=[Bpkv], signal=(k == 7))
                    sq_t, Bsq = sq.next()
                    s_t, Bs10 = s10.next()
                    qk_t, Bqk = qk.next()
                    qkr_t, Bqkr = qkr.next()
                    if not is_ctx:
                        pq_t, Bpq = pq.next()
                        for k in range(8):
                            ph.op("pe", lambda e, pq_t=pq_t, hT=hT, k=k, t=t: e.matmul(pq_t[:], lhsT=hT[:, k, t * 128:(t + 1) * 128], rhs=w_in_b[:, k, 0:512],
                                                                                     start=(k == 0), stop=(k == 7)), reads=[BhT, BhTa, Bw], writes=[Bpq], signal=(k == 7))
                        ph.op("act", lambda e, sq_t=sq_t, pq_t=pq_t: e.activation(out=sq_t[:, 0:512], in_=pq_t[:], func=AF.Square), reads=[Bpq], writes=[Bsq, Bpq])
                    h0 = 8 if is_ctx else 0
                    ph.op("act", lambda e, sq_t=sq_t, pkv_t=pkv_t: e.activation(out=sq_t[:, 512:640], in_=pkv_t[:, 0:128], func=AF.Square), reads=[Bpkv], writes=[Bsq, Bpkv])
                    ph.op("dve", lambda e, s_t=s_t, sq_t=sq_t, h0=h0: e.tensor_reduce(out=s_t[:, h0:10], in_=v3(sq_t[:, h0 * 64:640], 64), axis=AX.X, op=ALU.add),
                          reads=[Bsq], writes=[Bs10])
                    ph.op("dve", lambda e, s_t=s_t, h0=h0: e.tensor_scalar(out=s_t[:, h0:10], in0=s_t[:, h0:10], scalar1=1.0 / 64, scalar2=EPS, op0=ALU.mult, op1=ALU.add),
                          reads=[Bs10], writes=[Bs10])
                    ph.op("pool", lambda e, s_t=s_t, h0=h0: e.tensor_tensor(out=s_t[:, h0:10], in0=s_t[:, h0:10], in1=self.mhalf[:, h0:10], op=ALU.pow),
                          reads=[Bs10], writes=[Bs10])
                    if not is_ctx:
                        ph.op("dve", lambda e, qk_t=qk_t, pq_t=pq_t, s_t=s_t: e.tensor_tensor(out=v3(qk_t[:, 0:512], 64), in0=v3(pq_t[:], 64),
                                                                                            in1=s_t[:, 0:8].unsqueeze(2).broadcast_to([128, 8, 64]), op=ALU.mult),
                              reads=[Bpq, Bs10], writes=[Bqk, Bpq])
                    ph.op("dve", lambda e, qk_t=qk_t, pkv_t=pkv_t, s_t=s_t: e.tensor_tensor(out=v3(qk_t[:, 512:640], 64), in0=v3(pkv_t[:, 0:128], 64),
                                                                                          in1=s_t[:, 8:10].unsqueeze(2).broadcast_to([128, 2, 64]), op=ALU.mult),
                          reads=[Bpkv, Bs10], writes=[Bqk, Bpkv])
                    ph.op("act", lambda e, pkv_t=pkv_t, kt=kt: e.activation(out=self.Vaug[:, kt, :, 0:64], in_=v3(pkv_t[:, 128:256], 64), func=AF.Copy),
                          reads=[Bpkv], writes=[Bpkv])
                    c0 = h0 * 64
                    ph.op("dve", lambda e, qk_t=qk_t, c0=c0: e.tensor_tensor(out=qk_t[:, c0:640], in0=qk_t[:, c0:640], in1=gq_bc[:, c0:640], op=ALU.mult),
                          reads=[Bqk, Bgq], writes=[Bqk])
                    if is_ctx:
                        ph.op("dve", lambda e, qkr_t=qkr_t, qk_t=qk_t: e.tensor_copy(out=qkr_t[:, 512:640], in_=qk_t[:, 512:640]), reads=[Bqk], writes=[Bqkr])
                    else:
                        r_t, Brt = rt.next()
                        q3 = v3(qk_t[:], 64)
                        o3 = v3(qkr_t[:], 64)
                        cb = cos_t[:, lt, :].unsqueeze(1).broadcast_to([128, 10, 32])
                        sbc = sin_t[:, lt, :].unsqueeze(1).broadcast_to([128, 10, 32])
                        r3 = [v3(r_t[:, i, :], 32) for i in range(4)]
                        ph.op("dve", lambda e, r3=r3, q3=q3, cb=cb: e.tensor_tensor(out=r3[0], in0=q3[:, :, 0:32], in1=cb, op=ALU.mult), reads=[Bqk, Bcs], writes=[Brt])
                        ph.op("pool", lambda e, r3=r3, q3=q3, sbc=sbc: e.tensor_tensor(out=r3[1], in0=q3[:, :, 32:64], in1=sbc, op=ALU.mult), reads=[Bqk, Bcs], writes=[Brt])
                        ph.op("dve", lambda e, r3=r3, q3=q3, cb=cb: e.tensor_tensor(out=r3[2], in0=q3[:, :, 32:64], in1=cb, op=ALU.mult), reads=[Bqk, Bcs], writes=[Brt])
                        ph.op("pool", lambda e, r3=r3, q3=q3, sbc=sbc: e.tensor_tensor(out=r3[3], in0=q3[:, :, 0:32], in1=sbc, op=ALU.mult), reads=[Bqk, Bcs], writes=[Brt])
                        ph.op("dve", lambda e, r3=r3, o3=o3: e.tensor_tensor(out=o3[:, :, 0:32], in0=r3[0], in1=r3[1], op=ALU.subtract), reads=[Brt], writes=[Bqkr])
                        ph.op("dve", lambda e, r3=r3, o3=o3: e.tensor_tensor(out=o3[:, :, 32:64], in0=r3[2], in1=r3[3], op=ALU.add), reads=[Brt], writes=[Bqkr])
                    kd_t, Bkd = kd.next()
                    ph.op("pool", lambda e, kd_t=kd_t, qkr_t=qkr_t: e.tensor_copy(
                        out=kd_t[:].rearrange("p (g r d) -> p g r d", g=2, r=2),
                        in_=v3(qkr_t[:, 512:640], 64).unsqueeze(2).broadcast_to([128, 2, 2, 64])), reads=[Bqkr], writes=[Bkd])
                    pqk_t, Bpqk = pqk.next()
                    if not is_ctx:
                        for p in range(4):
                            ph.op("pe", lambda e, pqk_t=pqk_t, qkr_t=qkr_t, p=p: e.transpose(out=pqk_t[:, p, :], in_=qkr_t[:, p * 128:(p + 1) * 128], identity=self.ident_b[:]),
                                  reads=[Bqkr], writes=[Bpqk], signal=False)
                    for g in range(2):
                        ph.op("pe", lambda e, pqk_t=pqk_t, kd_t=kd_t, g=g: e.transpose(out=pqk_t[:, 4 + g, :], in_=kd_t[:, g * 128:(g + 1) * 128], identity=self.ident_b[:]),
                              reads=[Bkd], writes=[Bpqk], signal=(g == 1))
                    if not is_ctx:
                        ph.op("act", lambda e, pqk_t=pqk_t, lt=lt: e.activation(out=self.qT[:, :, lt * 128:(lt + 1) * 128], in_=pqk_t[:, 0:4, :], func=AF.Copy),
                              reads=[Bpqk], writes=[Bpqk])
                    ph.op("dve", lambda e, pqk_t=pqk_t, kt=kt: e.tensor_copy(out=self.kT2[:, :, kt * 128:(kt + 1) * 128], in_=pqk_t[:, 4:6, :]),
                          reads=[Bpqk], writes=[Bpqk])
                if not is_ctx:
                    tok0 = 15 + (st - 1) * 512
                    for c in range(4):
                        pu_t, Bpu = pu.next()
                        pg_t, Bpg = pg.next()
                        sg_t, Bsg = sig.next()
                        for k in range(8):
                            ph.op("pe", lambda e, pu_t=pu_t, hT=hT, k=k, c=c: e.matmul(pu_t[:], lhsT=w_in_b[:, k, 768 + c * 128:768 + (c + 1) * 128], rhs=hT[:, k, :],
                                                                                     start=(k == 0), stop=(k == 7)), reads=[BhT, BhTa, Bw], writes=[Bpu], signal=(k == 7))
                        for k in range(8):
                            ph.op("pe", lambda e, pg_t=pg_t, hT=hT, k=k, c=c: e.matmul(pg_t[:], lhsT=w_in_b[:, k, 1280 + c * 128:1280 + (c + 1) * 128], rhs=hT[:, k, :],
                                                                                     start=(k == 0), stop=(k == 7)), reads=[BhT, BhTa, Bw], writes=[Bpg], signal=(k == 7))
                        ph.op("act", lambda e, sg_t=sg_t, pg_t=pg_t: e.activation(out=sg_t[:], in_=pg_t[:], func=AF.Sigmoid), reads=[Bpg], writes=[Bsg])
                        ph.op("dve", lambda e, sg_t=sg_t, pu_t=pu_t, c=c, tok0=tok0: e.tensor_tensor(out=self.gT[:, c, tok0:tok0 + 512], in0=pu_t[:], in1=sg_t[:], op=ALU.mult),
                              reads=[Bpu, Bsg], writes=[])
            for nm, t in (("qT", self.qT), ("kT2", self.kT2), ("Vaug", self.Vaug), ("gT", self.gT)):
                if nm in self.dbg:
                    ph.dma("sp", lambda e, nm=nm, t=t: e.dma_start(out=self.dbg[nm], in_=t[:]), reads=[Bout])
            ph.finish()

    def attn(self, es_out):
        nc = self.nc
        self.attnT = self.qT
        NJ2 = NKT // 2
        with contextlib.ExitStack() as es:
            kTz = self.sb(es, "kTz", [128, 2, 2, NKT * 128], BF16)
            PT = self.rb(es, "PT", [128, 1024], BF16, 3)
            at2 = self.rb(es, "at2", [128, 4, 128], BF16, 2)
            rec = self.rb(es, "rec", [128, 4], F32, 2)
            pS = self.rps(es, "pS", [128, 1024], F32, 2)
            pO = self.rps(es, "pO", [128, 512], F32, 2)
            pA = self.rps(es, "pA", [128, 8, 128], BF16, 1)
            ph = Phase(nc, "attn")
            Bkz = Buf("kz")
            ph.op("dve", lambda e: e.memset(kTz[:], 0.0), writes=[Bkz])
            for g in range(2):
                ph.op("dve", lambda e, g=g: e.tensor_copy(out=kTz[0:64, g, 0, :], in_=self.kT2[0:64, g, :]), reads=[Bkz], writes=[Bkz])
                ph.op("dve", lambda e, g=g: e.tensor_copy(out=kTz[64:128, g, 1, :], in_=self.kT2[64:128, g, :]), reads=[Bkz], writes=[Bkz])
            for pair in range(4):
                g = pair // 2
                for Q in range(8):
                    at_t, Bat = at2.next()
                    for hl in range(2):
                        pO_t, BpO = pO.next()
                        pOv = pO_t[:, 0:260].rearrange("p (q d) -> p q d", d=65)

                        def emit_st(jj, hl=hl, pair=pair, Q=Q, g=g):
                            pS_t, BpS = pS.next()
                            for u in range(2):
                                j = 2 * jj + u
                                ph.op("pe", lambda e, pS_t=pS_t, j=j, u=u: e.matmul(
                                    pS_t[:, u * 512:(u + 1) * 512], lhsT=kTz[:, g, hl, j * 128:(j + 1) * 128],
                                    rhs=self.qT[:, pair, Q * 512:(Q + 1) * 512], start=True, stop=True), reads=[Bkz], writes=[BpS], signal=(u == 1))
                            return pS_t, BpS
                        nxt = emit_st(0)
                        for jj in range(NJ2):
                            pS_t, BpS = nxt
                            if jj + 1 < NJ2:
                                nxt = emit_st(jj + 1)
                            P_t, BP = PT.next()
                            ph.op("act", lambda e, P_t=P_t, pS_t=pS_t: e.activation(out=P_t[:], in_=pS_t[:], func=AF.Exp, scale=0.125, bias=self.negC[:, 0:1]),
                                  reads=[BpS], writes=[BP])
                            for u in range(2):
                                j = 2 * jj + u
                                for qt in range(4):
                                    ph.op("pe", lambda e, pOv=pOv, P_t=P_t, qt=qt, j=j, u=u, g=g: e.matmul(
                                        pOv[:, qt, :], lhsT=P_t[:, u * 512 + qt * 128:u * 512 + (qt + 1) * 128], rhs=self.Vaug[:, j, g, :],
                                        start=(j == 0 and qt == 0), stop=(j == NKT - 1), skip_group_check=True),
                                        reads=[BP], writes=[BpO], signal=(qt == 3 and (j == NKT - 1)))
                        rc, Brc = rec.next()
                        ph.op("dve", lambda e, rc=rc, pOv=pOv: e.reciprocal(out=rc[:], in_=pOv[:, :, 64]), reads=[BpO], writes=[Brc])
                        ph.op("dve", lambda e, rc=rc, pOv=pOv, at_t=at_t, hl=hl: e.tensor_tensor(
                            out=at_t[:, :, hl * 64:(hl + 1) * 64], in0=pOv[:, :, 0:64], in1=rc[:].unsqueeze(2).broadcast_to([128, 4, 64]), op=ALU.mult),
                            reads=[BpO, Brc], writes=[Bat])
                    pA_t, BpA = pA.next()
                    for qt in range(4):
                        ph.op("pe", lambda e, pA_t=pA_t, at_t=at_t, qt=qt: e.transpose(out=pA_t[:, qt, :], in_=at_t[:, qt, :], identity=self.ident_b[:]),
                              reads=[Bat], writes=[BpA], signal=(qt == 3))
                    ph.op("dve", lambda e, pA_t=pA_t, pair=pair, Q=Q: e.tensor_copy(
                        out=self.attnT[:, pair, Q * 512:(Q + 1) * 512].rearrange("p (q t) -> p q t", t=128), in_=pA_t[:, 0:4, :]),
                        reads=[BpA], writes=[])
            ph.finish()

    def conv(self, es_out):
        nc = self.nc
        self.convT = self.sb(es_out, "convT", [128, 4, S], BF16)
        with contextlib.ExitStack() as es:
            dg = self.sb(es, "dg", [128, 31, 4, 128], BF16)
            cw = self.sb(es, "cw", [128, 4, 31], F32)
            cv = self.sb(es, "cv", [128, 3, 4], F32)
            yf = self.rb(es, "yf", [128, 4, 512], F32, 2)
            ybf = self.rb(es, "ybf", [128, 4, 512], BF16, 2)
            ysq = self.rb(es, "ysq", [128, 4, 512], BF16, 2)
            mean = self.rb(es, "mean", [128, 512], F32, 2)
            msq = self.rb(es, "msq", [128, 512], F32, 1)
            rstd = self.rb(es, "rstd", [128, 512], F32, 2)
            zt = self.rb(es, "zt", [128, 512], F32, 2)
            pc = self.rps(es, "pc", [128, 512], F32, 3)
            pS1 = self.rps(es, "pS1", [128, 512], F32, 1)
            pS2 = self.rps(es, "pS2", [128, 512], F32, 1)
            ph = Phase(nc, "conv")
            Bcw, Bcv, Bdg, Bout = [Buf(n) for n in "cw cv dg out".split()]
            ph.dma("sp", lambda e: e.dma_start(out=cw[:], in_=self.conv_wT), writes=[Bcw])
            ph.dma("sp", lambda e: e.dma_start(out=cv[:], in_=self.conv_vec), writes=[Bcv])
            n = 0
            for j in range(31):
                for c in range(4):
                    eng = "dve"
                    ph.op(eng, lambda e, j=j, c=c: e.tensor_scalar(out=dg[:, j, c, :], in0=self.ident_b[:], scalar1=cw[:, c, j:j + 1], scalar2=None, op0=ALU.mult),
                          reads=[Bcw], writes=[Bdg])
            for tc in range(8):
                yf_t, Byf = yf.next()
                yb_t, Byb = ybf.next()
                ys_t, Bys = ysq.next()
                for c in range(4):
                    pc_t, Bpc = pc.next()
                    for j in range(31):
                        ph.op("pe", lambda e, pc_t=pc_t, j=j, c=c, tc=tc: e.matmul(pc_t[:], lhsT=dg[:, j, c, :], rhs=self.gT[:, c, tc * 512 + j:tc * 512 + j + 512],
                                                                                  start=(j == 0), stop=(j == 30)), reads=[Bdg], writes=[Bpc], signal=(j == 30))
                    ph.op("act", lambda e, pc_t=pc_t, yf_t=yf_t, c=c: e.activation(out=yf_t[:, c, :], in_=pc_t[:], func=AF.Identity, bias=cv[:, 0, c:c + 1]),
                          reads=[Bpc, Bcv], writes=[Byf])
                    ph.op("act", lambda e, pc_t=pc_t, ys_t=ys_t, c=c: e.activation(out=ys_t[:, c, :], in_=pc_t[:], func=AF.Square, bias=cv[:, 0, c:c + 1]),
                          reads=[Bpc, Bcv], writes=[Bys])
                    ph.op("dve", lambda e, yf_t=yf_t, yb_t=yb_t, c=c: e.tensor_copy(out=yb_t[:, c, :], in_=yf_t[:, c, :]), reads=[Byf], writes=[Byb])
                p1, Bp1 = pS1.next()
                p2, Bp2 = pS2.next()
                for c in range(4):
                    ph.op("pe", lambda e, p1=p1, yb_t=yb_t, c=c: e.matmul(p1[:], lhsT=self.ones_b[:], rhs=yb_t[:, c, :], start=(c == 0), stop=(c == 3)),
                          reads=[Byb], writes=[Bp1], signal=(c == 3))
                for c in range(4):
                    ph.op("pe", lambda e, p2=p2, ys_t=ys_t, c=c: e.matmul(p2[:], lhsT=self.ones_b[:], rhs=ys_t[:, c, :], start=(c == 0), stop=(c == 3)),
                          reads=[Bys], writes=[Bp2], signal=(c == 3))
                mn, Bmn = mean.next()
                ms, Bms = msq.next()
                rs, Brs = rstd.next()
                ph.op("act", lambda e, mn=mn, p1=p1: e.activation(out=mn[:], in_=p1[:], func=AF.Copy, scale=1.0 / 512), reads=[Bp1], writes=[Bmn])
                ph.op("dve", lambda e, mn=mn, ms=ms: e.tensor_tensor(out=ms[:], in0=mn[:], in1=mn[:], op=ALU.mult), reads=[Bmn], writes=[Bms])
                ph.op("dve", lambda e, rs=rs, p2=p2, ms=ms: e.scalar_tensor_tensor(out=rs[:], in0=p2[:], scalar=1.0 / 512, in1=ms[:], op0=ALU.mult, op1=ALU.subtract),
                      reads=[Bp2, Bms], writes=[Brs])
                ph.op("dve", lambda e, rs=rs: e.tensor_scalar(out=rs[:], in0=rs[:], scalar1=EPS, scalar2=None, op0=ALU.add), reads=[Brs], writes=[Brs])
                ph.op("act", lambda e, rs=rs: e.activation(out=rs[:], in_=rs[:], func=AF.Sqrt), reads=[Brs], writes=[Brs])
                ph.op("dve", lambda e, rs=rs: e.reciprocal(out=rs[:], in_=rs[:]), reads=[Brs], writes=[Brs])
                for c in range(4):
                    z_t, Bz = zt.next()
                    ph.op("dve", lambda e, z_t=z_t, yf_t=yf_t, mn=mn, c=c: e.tensor_tensor(out=z_t[:], in0=yf_t[:, c, :], in1=mn[:], op=ALU.subtract), reads=[Byf, Bmn], writes=[Bz])
                    ph.op("dve", lambda e, z_t=z_t, rs=rs: e.tensor_tensor(out=z_t[:], in0=z_t[:], in1=rs[:], op=ALU.mult), reads=[Bz, Brs], writes=[Bz])
                    ph.op("act", lambda e, z_t=z_t, c=c, tc=tc: e.activation(out=self.convT[:, c, tc * 512:(tc + 1) * 512], in_=z_t[:], func=AF.Silu,
                                                                           scale=cv[:, 1, c:c + 1], bias=cv[:, 2, c:c + 1]), reads=[Bz, Bcv], writes=[])
            if "convT" in self.dbg:
                ph.dma("sp", lambda e: e.dma_start(out=self.dbg["convT"], in_=self.convT[:]), reads=[Bout])
            ph.finish()

    def tail_alloc(self, es, deep=False):
        T = {}
        T["hm"] = self.rb(es, "hm", [128, D], F32, 2)
        T["tmp"] = self.rb(es, "tmp", [128, D], F32, 2 if deep else 1)
        T["junk"] = self.sb(es, "junk2", [128, D], BF16)
        T["ss"] = self.rb(es, "ss2", [128, 4], F32, 2)
        T["aug"] = self.rb(es, "aug", [128, RW], BF16, 2)
        T["h2T"] = self.rb(es, "h2T", [128, 8, 128], BF16, 2)
        T["ex"] = self.rb(es, "ex", [128, NE], F32, 2)
        T["pT2"] = self.rps(es, "pT2", [128, 8, 128], BF16, 2 if deep else 1)
        T["pl"] = self.rps(es, "pl", [128, 512], F32, 2 if deep else 1)
        T["Bjunk"] = Buf("junk2")
        T["Baff"] = Buf("aff")
        return T

    def tail_tile(self, ph, T, i, po_list, x_t, Bx, hdst):
        hm, Bhm = T["hm"].next()
        tmp, Btmp = T["tmp"].next()
        ss, Bss = T["ss"].next()
        aug, Baug = T["aug"].next()
        h2T, Bh2T = T["h2T"].next()
        ex, Bex = T["ex"].next()
        pT2, BpT2 = T["pT2"].next()
        pl, Bpl = T["pl"].next()
        for n, (po, Bpo) in enumerate(po_list):
            sl = slice(n * 512, (n + 1) * 512)
            ph.op("dve", lambda e, hm=hm, po=po, sl=sl: e.tensor_tensor(out=hm[:, sl], in0=po, in1=self.G1bc[:, sl], op=ALU.mult), reads=[Bpo], writes=[Bhm])
            ph.op("dve", lambda e, hm=hm, x_t=x_t, sl=sl: e.tensor_tensor(out=hm[:, sl], in0=hm[:, sl], in1=x_t[:, sl], op=ALU.add), reads=[Bhm, Bx], writes=[Bhm])
        ph.dma("sp", lambda e, hm=hm, i=i: e.dma_start(out=hdst[i * 128:(i + 1) * 128, :], in_=hm[:]), reads=[Bhm])
        ph.op("act", lambda e, hm=hm, ss=ss: e.activation(out=T["junk"][:], in_=hm[:], func=AF.Square, accum_out=ss[:, 0:1]), reads=[Bhm], writes=[T["Bjunk"], Bss])
        ph.op("dve", lambda e, ss=ss: e.tensor_scalar(out=ss[:, 0:1], in0=ss[:, 0:1], scalar1=1.0 / D, scalar2=EPS, op0=ALU.mult, op1=ALU.add), reads=[Bss], writes=[Bss])
        ph.op("pool", lambda e, ss=ss: e.tensor_tensor(out=ss[:, 0:1], in0=ss[:, 0:1], in1=self.mhalf[:, 0:1], op=ALU.pow), reads=[Bss], writes=[Bss])
        ph.op("dve", lambda e, tmp=tmp, hm=hm, ss=ss: e.scalar_tensor_tensor(out=tmp[:], in0=hm[:], scalar=ss[:, 0:1], in1=self.A2bc[:], op0=ALU.mult, op1=ALU.mult),
              reads=[Bhm, Bss], writes=[Btmp])
        ph.op("dve", lambda e, tmp=tmp, aug=aug: e.tensor_tensor(out=aug[:, 0:D], in0=tmp[:], in1=self.B2bc[:], op=ALU.add), reads=[Btmp], writes=[Baug])
        for k in range(8):
            ph.op("pe", lambda e, pT2=pT2, aug=aug, k=k: e.transpose(out=pT2[:, k, :], in_=aug[:, k * 128:(k + 1) * 128], identity=self.ident_b[:]),
                  reads=[Baug], writes=[BpT2], signal=(k == 7))
        ph.op("act", lambda e, h2T=h2T, pT2=pT2: e.activation(out=h2T[:], in_=pT2[:], func=AF.Copy), reads=[BpT2], writes=[Bh2T])
        for k in range(8):
            ph.op("pe", lambda e, pl=pl, h2T=h2T, k=k: e.matmul(pl[:, 0:NE], lhsT=h2T[:, k, :], rhs=self.wr_b[:, k, :], start=(k == 0), stop=(k == 7)),
                  reads=[Bh2T], writes=[Bpl], signal=(k == 7))
        ph.op("dve", lambda e, ss=ss, pl=pl: e.tensor_reduce(out=ss[:, 1:2], in_=pl[:, 0:NE], axis=AX.X, op=ALU.max), reads=[Bpl], writes=[Bss])
        ph.op("dve", lambda e, ss=ss: e.tensor_scalar(out=ss[:, 1:2], in0=ss[:, 1:2], scalar1=-1.0, scalar2=None, op0=ALU.mult), reads=[Bss], writes=[Bss])
        ph.op("act", lambda e, ex=ex, pl=pl, ss=ss: e.activation(out=ex[:], in_=pl[:, 0:NE], func=AF.Exp, bias=ss[:, 1:2], accum_out=ss[:, 2:3]),
              reads=[Bpl, Bss], writes=[Bex, Bss])
        ph.op("dve", lambda e, ss=ss: e.reciprocal(out=ss[:, 3:4], in_=ss[:, 2:3]), reads=[Bss], writes=[Bss])
        ph.op("dve", lambda e, ex=ex, ss=ss, i=i: e.tensor_scalar(out=self.affTM[:, i, :], in0=ex[:], scalar1=ss[:, 3:4], scalar2=None, op0=ALU.mult),
              reads=[Bex, Bss], writes=[T["Baff"]])
        ph.op("dve", lambda e, aug=aug, i=i: e.tensor_copy(out=aug[:, D:D + 16], in_=self.affTM[:, i, :]), reads=[T["Baff"]], writes=[Baug])
        ph.op("dve", lambda e, aug=aug, ex=ex, i=i: e.tensor_tensor(out=ex[:], in0=self.affTM[:, i, :], in1=aug[:, D:D + 16], op=ALU.subtract), reads=[T["Baff"], Baug], writes=[Bex])
        ph.op("dve", lambda e, aug=aug, ex=ex: e.tensor_copy(out=aug[:, D + 16:D + 32], in_=ex[:]), reads=[Bex], writes=[Baug])
        ph.op("dve", lambda e, aug=aug, ex=ex: e.tensor_tensor(out=ex[:], in0=ex[:], in1=aug[:, D + 16:D + 32], op=ALU.subtract), reads=[Bex, Baug], writes=[Bex])
        ph.op("dve", lambda e, aug=aug, ex=ex: e.tensor_copy(out=aug[:, D + 32:D + 48], in_=ex[:]), reads=[Bex], writes=[Baug])
        ph.dma("sp", lambda e, aug=aug, i=i: e.dma_start(out=self.h2aug[i * 128:(i + 1) * 128, :], in_=aug[:]), reads=[Baug])

    def outproj0(self):
        nc = self.nc
        with contextlib.ExitStack() as es:
            w_out_b = self.sb(es, "w_out_b", [128, 8, D], BF16)
            xt = self.rb(es, "xt", [128, D], F32, 3)
            T = self.tail_alloc(es, deep=True)
            po = self.rps(es, "po", [128, 512], F32, 4)
            ph = Phase(nc, "outproj0")
            Bw = Buf("w")
            ph.dma("pool", lambda e: e.dma_start(out=w_out_b[:], in_=self.ev_w_out.rearrange("(k p) n -> p k n", p=128)), writes=[Bw])
            loads = {}

            def issue_load(i):
                x_t, Bx = xt.next()
                ph.dma("sp", lambda e, x_t=x_t, i=i: e.dma_start(out=x_t[:], in_=self.x[i * 128:(i + 1) * 128, :]), writes=[Bx])
                loads[i] = (x_t, Bx)
            issue_load(0)
            issue_load(1)
            for i in range(NT):
                if i + 2 < NT:
                    issue_load(i + 2)
                x_t, Bx = loads.pop(i)
                pol = []
                for n in range(2):
                    po_t, Bpo = po.next()
                    for fc in range(8):
                        src = self.attnT[:, fc, i * 128:(i + 1) * 128] if fc < 4 else self.convT[:, fc - 4, i * 128:(i + 1) * 128]
                        ph.op("pe", lambda e, po_t=po_t, src=src, fc=fc, n=n: e.matmul(po_t[:], lhsT=src, rhs=w_out_b[:, fc, n * 512:(n + 1) * 512],
                                                                                     start=(fc == 0), stop=(fc == 7)), reads=[Bw], writes=[Bpo], signal=(fc == 7))
                    pol.append((po_t[:], Bpo))
                self.tail_tile(ph, T, i, pol, x_t, Bx, self.hbuf)
            if "aff" in self.dbg:
                ph.dma("sp", lambda e: e.dma_start(out=self.dbg["aff"], in_=self.affTM[:]), reads=[T["Baff"]])
            ph.finish()
            self.dump_tail()

    def dump_h2(self):
        if "h1" in self.dbg:
            ph = Phase(self.nc, "dumph1")
            B = Buf("d")
            ph.dma("sp", lambda e: e.dma_start(out=self.dbg["h1"], in_=self.hbuf2), reads=[B], writes=[B])
            ph.op("dve", lambda e: e.memset(self.zeros[:, 0:1], 0.0), reads=[B], writes=[B])
            ph.finish()

    def dump_tail(self):
        ph = Phase(self.nc, "dump")
        B = Buf("dump")
        for nm, src in (("hmid", self.hbuf), ("h2aug", self.h2aug)):
            if nm in self.dbg:
                ph.dma("sp", lambda e, nm=nm, src=src: e.dma_start(out=self.dbg[nm], in_=src), reads=[B], writes=[B])
        ph.op("dve", lambda e: e.memset(self.zeros[:, 0:1], 0.0), reads=[B], writes=[B])
        ph.finish()

    def route(self, es_out, l):
        nc = self.nc
        self.Wg = self.rb(es_out, "Wg", [128, 8, D], BF16, 2)
        self.Wu = self.rb(es_out, "Wu", [128, 8, D], BF16, 2)
        self.Wd = self.rb(es_out, "Wd", [128, 8, D], BF16, 2)
        self.slotm = self.sb(es_out, "slotm", [128, NT * NE], F32)
        with contextlib.ExitStack() as es:
            affP = self.sb(es, "affP", [128, 512], F32)
            junk = self.sb(es, "junkr", [128, 512], BF16)
            Gm_s = self.sb(es, "Gm_s", [128, 128], F32)
            sc = self.sb(es, "sc", [128, 8], F32)
            dth = self.sb(es, "dth", [16, 16], F32)
            thr_bc = self.sb(es, "thr_bc", [128, NE], F32)
            mask_f = self.sb(es, "mask_f", [128, NT * NE], F32)
            mask_b = self.sb(es, "mask_b", [128, NT * NE], BF16)
            within = self.sb(es, "within", [128, NT * NE], F32)
            cA = self.sb(es, "cA", [128, NT * NE], F32)
            cB = self.sb(es, "cB", [128, NT * NE], F32)
            tot = self.sb(es, "tot", [128, NT * NE], F32)
            pa = self.rps(es, "pa", [128, 512], F32, 2)
            ph = Phase(nc, f"route{l}")
            self.pre_w = []
            for ring, src in ((self.Wg, self.w_gate), (self.Wu, self.w_up), (self.Wd, self.w_down)):
                wt, Bw = ring.next()
                ph.dma("pool", lambda e, wt=wt, src=src: e.dma_start(out=wt[:], in_=src[l, 0].rearrange("(k p) n -> p k n", p=128)), writes=[Bw])
                self.pre_w.append((wt, Bw))
            B = Buf("r")
            W = dict(reads=[B], writes=[B])
            BaT = Buf("affT")
            BG = Buf("Gm")
            ph.dma("sp", lambda e: e.dma_start(out=Gm_s[:], in_=self.Gm), writes=[BG])
            pt, Bp = pa.next()
            for g in range(4):
                ph.op("pe", lambda e, pt=pt, g=g: e.transpose(out=pt[:, g * 128:(g + 1) * 128], in_=self.affTM[:, g * 8:(g + 1) * 8, :].rearrange("p a b -> p (a b)"),
                                                              identity=self.ident_f[:]), writes=[Bp], signal=(g == 3))
            ph.op("act", lambda e, pt=pt: e.activation(out=affP[:], in_=pt[:], func=AF.Copy), reads=[Bp], writes=[BaT])
            lo, hi, mid, cnt, pred, dd = [sc[:, i:i + 1] for i in range(6)]
            ph.op("dve", lambda e: e.memset(sc[:], 0.0), reads=[BaT], writes=[B])
            ph.op("dve", lambda e: e.memset(hi, 1.5), **W)
            for it in range(30):
                ph.op("dve", lambda e: e.tensor_scalar(out=mid, in0=lo, scalar1=hi, scalar2=0.5, op0=ALU.add, op1=ALU.mult), **W)
                ph.op("dve", lambda e: e.tensor_scalar(out=junk[:], in0=affP[:], scalar1=mid, scalar2=0.0, op0=ALU.is_ge, op1=ALU.add, accum_out=cnt), **W)
                pc_t, Bpc = pa.next()
                ph.op("pe", lambda e, pc_t=pc_t: e.matmul(pc_t[:, 0:1], lhsT=Gm_s[:], rhs=cnt, start=True, stop=True), reads=[B, BG], writes=[Bpc])
                ph.op("dve", lambda e, pc_t=pc_t: e.tensor_scalar(out=pred, in0=pc_t[:, 0:1], scalar1=float(CAP), scalar2=None, op0=ALU.is_ge), reads=[Bpc, B], writes=[B, Bpc])
                ph.op("dve", lambda e: e.tensor_tensor(out=dd, in0=mid, in1=lo, op=ALU.subtract), **W)
                ph.op("dve", lambda e: e.scalar_tensor_tensor(out=lo, in0=dd, scalar=pred, in1=lo, op0=ALU.mult, op1=ALU.add), **W)
                ph.op("dve", lambda e: e.tensor_tensor(out=dd, in0=hi, in1=mid, op=ALU.subtract), **W)
                ph.op("dve", lambda e: e.scalar_tensor_tensor(out=hi, in0=dd, scalar=pred, in1=mid, op0=ALU.mult, op1=ALU.add), **W)
            lo16 = sc[0:16, 0:1]
            ph.op("dve", lambda e: e.tensor_scalar(out=dth[:], in0=self.ident_f[0:16, 0:16], scalar1=lo16, scalar2=None, op0=ALU.mult), **W)
            pt, Bp = pa.next()
            ph.op("pe", lambda e, pt=pt: e.matmul(pt[:, 0:NE], lhsT=self.ones_f[0:16, :], rhs=dth[:], start=True, stop=True), reads=[B], writes=[Bp])
            ph.op("dve", lambda e, pt=pt: e.tensor_copy(out=thr_bc[:], in_=pt[:, 0:NE]), reads=[Bp], writes=[B])
            ph.op("dve", lambda e: e.tensor_tensor(out=v3(mask_f[:], NE), in0=self.affTM[:], in1=thr_bc[:].unsqueeze(1).broadcast_to([128, NT, NE]), op=ALU.is_ge), **W)
            ph.op("dve", lambda e: e.tensor_copy(out=mask_b[:], in_=mask_f[:]), **W)
            p1, Bp1 = pa.next()
            ph.op("pe", lambda e, p1=p1: e.matmul(p1[:], lhsT=self.ustr_b[:], rhs=mask_b[:], start=True, stop=True), reads=[B], writes=[Bp1])
            ph.op("dve", lambda e, p1=p1: e.tensor_copy(out=within[:], in_=p1[:]), reads=[Bp1], writes=[B])
            p2, Bp2 = pa.next()
            ph.op("pe", lambda e, p2=p2: e.matmul(p2[:], lhsT=self.ones_b[:], rhs=mask_b[:], start=True, stop=True), reads=[B], writes=[Bp2])
            ph.op("dve", lambda e, p2=p2: e.tensor_copy(out=tot[:], in_=p2[:]), reads=[Bp2], writes=[B])
            ph.op("dve", lambda e: e.tensor_copy(out=cA[:], in_=tot[:]), **W)
            src, dst = cA, cB
            for sft in (1, 2, 4, 8, 16):
                w = sft * NE
                ph.op("dve", lambda e, src=src, dst=dst, w=w: e.tensor_copy(out=dst[:, 0:w], in_=src[:, 0:w]), **W)
                ph.op("dve", lambda e, src=src, dst=dst, w=w: e.tensor_tensor(out=dst[:, w:], in0=src[:, w:], in1=src[:, 0:NT * NE - w], op=ALU.add), **W)
                src, dst = dst, src
            ph.op("dve", lambda e, src=src: e.tensor_tensor(out=src[:], in0=src[:], in1=tot[:], op=ALU.subtract), **W)
            ph.op("dve", lambda e, src=src: e.tensor_tensor(out=within[:], in0=within[:], in1=src[:], op=ALU.add), **W)
            ph.op("dve", lambda e: e.scalar_tensor_tensor(out=within[:], in0=within[:], scalar=1.0, in1=mask_f[:], op0=ALU.add, op1=ALU.mult), **W)
            ph.op("dve", lambda e: e.tensor_scalar(out=self.slotm[:], in0=within[:], scalar1=-1.0, scalar2=None, op0=ALU.add), **W)
            if "slotm" in self.dbg:
                ph.dma("sp", lambda e: e.dma_start(out=self.dbg["slotm"], in_=self.slotm[:]), **W)
            if "thr" in self.dbg:
                ph.dma("sp", lambda e: e.dma_start(out=self.dbg["thr"], in_=sc[:]), **W)
            ph.finish()

    def moe(self, l):
        nc = self.nc
        with contextlib.ExitStack() as es:
            Wg, Wu, Wd = self.Wg, self.Wu, self.Wd
            oh = self.rb(es, "oh", [128, 512], BF16, 3)
            idxf = self.rb(es, "idxf", [128, 4], F32, 2)
            idx8 = self.rb(es, "idx8", [128, 8], F32, 2)
            idxi = self.rb(es, "idxi", [128, 4], I32, 2)
            xs = self.rb(es, "xs", [128, 4, RW], BF16, 2)
            gs = self.rb(es, "gs", [128, 4], F32, 2)
            xsT = self.rb(es, "xsT", [128, 8, 512], BF16, 2)
            hidT = self.rb(es, "hidT", [128, 8, 512], BF16, 2)
            sg = self.rb(es, "sg", [128, 512], F32, 2)
            ysb = self.rb(es, "ysb", [128, D], F32, 3)
            pG = self.rps(es, "pG", [128, 512], F32, 2)
            pU = self.rps(es, "pU", [128, 512], F32, 2)
            pY = self.rps(es, "pY", [128, 512], F32, 2)
            pTP = self.rps(es, "pTP", [128, 8, 128], BF16, 1)
            pI = self.rps(es, "pI", [128, 512], F32, 1)
            ph = Phase(nc, f"moe{l}")
            Bacc = Buf("acc")
            Bz = [Buf(f"z{i}") for i in range(NT)]
            for i in range(NT):
                ph.dma("sp", lambda e, i=i: e.dma_start(out=self.acc[i * 128:(i + 1) * 128, :], in_=self.zeros[:, 0:D]), writes=[Bz[i]])
            first_scatter = [True]

            def load_w(e_):
                r = []
                for ring, src in ((Wg, self.w_gate), (Wu, self.w_up), (Wd, self.w_down)):
                    wt, Bw = ring.next()
                    ph.dma("pool", lambda e, wt=wt, src=src, e_=e_: e.dma_start(out=wt[:], in_=src[l, e_].rearrange("(k p) n -> p k n", p=128)), writes=[Bw])
                    r.append((wt, Bw))
                return r

            def build_idx(e_, res):
                pI_t, BpI = pI.next()
                pIv = pI_t[:, 0:8].rearrange("p (c t) -> p c t", t=2)
                for i in range(NT):
                    oh_t, Boh = oh.next()
                    ph.op("dve", lambda e, oh_t=oh_t, i=i, e_=e_: e.tensor_scalar(out=oh_t[:], in0=self.iota512[:], scalar1=self.slotm[:, i * NE + e_:i * NE + e_ + 1],
                                                                                 scalar2=None, op0=ALU.is_equal), writes=[Boh])
                    for c in range(4):
                        ph.op("pe", lambda e, pIv=pIv, oh_t=oh_t, i=i, c=c: e.matmul(pIv[:, c, :], lhsT=oh_t[:, c * 128:(c + 1) * 128], rhs=self.tokval[:, i, :],
                                                                                   start=(i == 0 and c == 0), stop=(i == NT - 1), skip_group_check=True),
                              reads=[Boh], writes=[BpI], signal=(c == 3))
                    yield
                xf, Bxf = idxf.next()
                xi, Bxi = idxi.next()
                x8, Bx8 = idx8.next()
                ph.op("dve", lambda e, x8=x8, pI_t=pI_t: e.tensor_copy(out=x8[:], in_=pI_t[:, 0:8]), reads=[BpI], writes=[Bx8])
                x8v = x8[:].rearrange("p (c t) -> p c t", t=2)
                ph.op("dve", lambda e, xf=xf, x8v=x8v: e.scalar_tensor_tensor(out=xf[:], in0=x8v[:, :, 0], scalar=128.0, in1=x8v[:, :, 1], op0=ALU.mult, op1=ALU.add),
                      reads=[Bx8], writes=[Bxf])
                ph.op("dve", lambda e, xf=xf, xi=xi: e.tensor_copy(out=xi[:], in_=xf[:]), reads=[Bxf], writes=[Bxi])
                xs_t, Bxs = xs.next()
                for c in range(4):
                    ph.dma("pool", lambda e, xs_t=xs_t, xi=xi, c=c: e.indirect_dma_start(
                        out=xs_t[:, c, :], out_offset=None, in_=self.h2aug, in_offset=bass.IndirectOffsetOnAxis(ap=xi[:, c:c + 1], axis=0)),
                        reads=[Bxi], writes=[Bxs])
                res.append((xs_t, Bxs, xi, Bxi))

            def ffn(e_, wts, gath):
                (wg, Bwg), (wu, Bwu), (wd, Bwd) = wts
                xs_t, Bxs, xi, Bxi = gath
                g_t, Bg = gs.next()
                ph.op("dve", lambda e, g_t=g_t, xs_t=xs_t: e.tensor_tensor(out=g_t[:], in0=xs_t[:, :, D + e_], in1=xs_t[:, :, D + 16 + e_], op=ALU.add), reads=[Bxs], writes=[Bg])
                ph.op("dve", lambda e, g_t=g_t, xs_t=xs_t: e.tensor_tensor(out=g_t[:], in0=g_t[:], in1=xs_t[:, :, D + 32 + e_], op=ALU.add), reads=[Bxs, Bg], writes=[Bg])
                xT, BxT = xsT.next()
                for k2 in range(4):
                    tp, Btp = pTP.next()
                    for kk in range(2):
                        k = k2 * 2 + kk
                        for c in range(4):
                            ph.op("pe", lambda e, tp=tp, xs_t=xs_t, kk=kk, c=c, k=k: e.transpose(out=tp[:, kk * 4 + c, :], in_=xs_t[:, c, k * 128:(k + 1) * 128], identity=self.ident_b[:]),
                                  reads=[Bxs], writes=[Btp], signal=(kk == 1 and c == 3))
                    eng = "act" if k2 % 2 == 0 else "dve"
                    if eng == "act":
                        ph.op("act", lambda e, tp=tp, xT=xT, k2=k2: e.activation(out=xT[:, k2 * 2:k2 * 2 + 2, :], in_=tp[:].rearrange("p (k c) t -> p k (c t)", k=2), func=AF.Copy),
                              reads=[Btp], writes=[BxT])
                    else:
                        ph.op("dve", lambda e, tp=tp, xT=xT, k2=k2: e.tensor_copy(out=xT[:, k2 * 2:k2 * 2 + 2, :], in_=tp[:].rearrange("p (k c) t -> p k (c t)", k=2)),
                              reads=[Btp], writes=[BxT])
                    yield
                hT, BhT = hidT.next()
                for f in range(8):
                    pg_t, Bpg = pG.next()
                    pu_t, Bpu = pU.next()
                    for k in range(8):
                        ph.op("pe", lambda e, pg_t=pg_t, wg=wg, xT=xT, k=k, f=f: e.matmul(pg_t[:], lhsT=wg[:, k, f * 128:(f + 1) * 128], rhs=xT[:, k, :], start=(k == 0), stop=(k == 7)),
                              reads=[Bwg, BxT], writes=[Bpg], signal=(k == 7))
                    for k in range(8):
                        ph.op("pe", lambda e, pu_t=pu_t, wu=wu, xT=xT, k=k, f=f: e.matmul(pu_t[:], lhsT=wu[:, k, f * 128:(f + 1) * 128], rhs=xT[:, k, :], start=(k == 0), stop=(k == 7)),
                              reads=[Bwu, BxT], writes=[Bpu], signal=(k == 7))
                    sg_t, Bsg = sg.next()
                    ph.op("act", lambda e, sg_t=sg_t, pg_t=pg_t: e.activation(out=sg_t[:], in_=pg_t[:], func=AF.Silu), reads=[Bpg], writes=[Bsg])
                    ph.op("dve", lambda e, sg_t=sg_t, pu_t=pu_t, hT=hT, f=f: e.tensor_tensor(out=hT[:, f, :], in0=pu_t[:], in1=sg_t[:], op=ALU.mult), reads=[Bpu, Bsg], writes=[BhT])
                    yield
                for c in range(4):
                    y_t, By = ysb.next()
                    for n in range(2):
                        py_t, Bpy = pY.next()
                        for f in range(8):
                            ph.op("pe", lambda e, py_t=py_t, hT=hT, wd=wd, f=f, c=c, n=n: e.matmul(py_t[:], lhsT=hT[:, f, c * 128:(c + 1) * 128], rhs=wd[:, f, n * 512:(n + 1) * 512],
                                                                                               start=(f == 0), stop=(f == 7)), reads=[BhT, Bwd], writes=[Bpy], signal=(f == 7))
                        ph.op("dve", lambda e, y_t=y_t, py_t=py_t, g_t=g_t, c=c, n=n: e.scalar_tensor_tensor(
                            out=y_t[:, n * 512:(n + 1) * 512], in0=py_t[:], scalar=g_t[:, c:c + 1], in1=self.G2bc[:, n * 512:(n + 1) * 512], op0=ALU.mult, op1=ALU.mult),
                            reads=[Bpy, Bg], writes=[By])
                    ph.dma("pool", lambda e, y_t=y_t, xi=xi, c=c: e.indirect_dma_start(
                        out=self.acc, out_offset=bass.IndirectOffsetOnAxis(ap=xi[:, c:c + 1], axis=0), in_=y_t[:], in_offset=None, compute_op=ALU.add),
                        reads=[By, Bxi] + (Bz if first_scatter[0] else []), writes=[Bacc])
                    first_scatter[0] = False
                    yield

            wts = self.pre_w
            r0 = []
            for _ in build_idx(0, r0):
                pass
            gath = r0[0]
            for e_ in range(NE):
                nwts = load_w(e_ + 1) if e_ + 1 < NE else None
                rn = []
                gi = build_idx(e_ + 1, rn) if e_ + 1 < NE else iter(())
                gf = ffn(e_, wts, gath)
                for _ in gi:
                    pass
                for _ in gf:
                    pass
                wts, gath = nwts, (rn[0] if rn else None)
            ph.finish()
            if "acc" in self.dbg:
                ph = Phase(nc, "dumpacc")
                B = Buf("d")
                ph.dma("sp", lambda e: e.dma_start(out=self.dbg["acc"], in_=self.acc), reads=[B], writes=[B])
                ph.op("dve", lambda e: e.memset(self.zeros[:, 0:1], 0.0), reads=[B], writes=[B])
                ph.finish()

    def resid(self, l, hsrc, hdst):
        nc = self.nc
        last = l == 1
        with contextlib.ExitStack() as es:
            hm = self.rb(es, "hmr", [128, D], F32, 3)
            ac = self.rb(es, "acr", [128, D], F32, 3)
            ot = self.rb(es, "otr", [128, D], F32, 2)
            ss = self.rb(es, "ssr", [128, 1], F32, 2)
            junk = self.sb(es, "junkf", [128, D], BF16)
            fg = self.sb(es, "fg", [128, D], F32)
            ph = Phase(nc, f"resid{l}")
            Bfg, Bj = Buf("fg"), Buf("j")
            if last:
                ph.dma("sp", lambda e: e.dma_start(out=fg[:], in_=self.final_g.partition_broadcast(128)), writes=[Bfg])
            loads = {}

            def issue_load(i):
                h_t, Bh = hm.next()
                a_t, Ba = ac.next()
                ph.dma("sp", lambda e, h_t=h_t, i=i: e.dma_start(out=h_t[:], in_=hsrc[i * 128:(i + 1) * 128, :]), writes=[Bh])
                ph.dma("sp", lambda e, a_t=a_t, i=i: e.dma_start(out=a_t[:], in_=self.acc[i * 128:(i + 1) * 128, :]), writes=[Ba])
                loads[i] = (h_t, Bh, a_t, Ba)
            issue_load(0)
            issue_load(1)
            for i in range(NT):
                if i + 2 < NT:
                    issue_load(i + 2)
                h_t, Bh, a_t, Ba = loads.pop(i)
                ph.op("dve", lambda e, a_t=a_t, h_t=h_t: e.tensor_tensor(out=h_t[:], in0=h_t[:], in1=a_t[:], op=ALU.add), reads=[Ba, Bh], writes=[Bh])
                if not last:
                    ph.dma("sp", lambda e, h_t=h_t, i=i: e.dma_start(out=hdst[i * 128:(i + 1) * 128, :], in_=h_t[:]), reads=[Bh])
                else:
                    s_t, Bs = ss.next()
                    o_t, Bo = ot.next()
                    ph.op("act", lambda e, h_t=h_t, s_t=s_t: e.activation(out=junk[:], in_=h_t[:], func=AF.Square, accum_out=s_t[:]), reads=[Bh], writes=[Bj, Bs])
                    ph.op("dve", lambda e, s_t=s_t: e.tensor_scalar(out=s_t[:], in0=s_t[:], scalar1=1.0 / D, scalar2=EPS, op0=ALU.mult, op1=ALU.add), reads=[Bs], writes=[Bs])
                    ph.op("pool", lambda e, s_t=s_t: e.tensor_tensor(out=s_t[:], in0=s_t[:], in1=self.mhalf[:, 0:1], op=ALU.pow), reads=[Bs], writes=[Bs])
                    ph.op("dve", lambda e, o_t=o_t, h_t=h_t, s_t=s_t: e.scalar_tensor_tensor(out=o_t[:], in0=h_t[:], scalar=s_t[:, 0:1], in1=fg[:], op0=ALU.mult, op1=ALU.mult),
                          reads=[Bh, Bs, Bfg], writes=[Bo])
                    ph.dma("sp", lambda e, o_t=o_t, i=i: e.dma_start(out=hdst[i * 128:(i + 1) * 128, :], in_=o_t[:]), reads=[Bo])
            ph.finish()

    def norm1_tile(self, ph, R, src, t, hT, BhT, x_t, Bx):
        BhTa = BhT.__dict__.setdefault("twin", Buf(BhT.name + "a"))
        ss, Bss = R["ssq"].next()
        xn_t, Bxn = R["xn"].next()
        p_t, BpT = R["pT"].next()
        lq = R.get("lq", "sp")
        ph.dma(lq, lambda e: e.dma_start(out=x_t[:], in_=src), writes=[Bx])
        if "fuse" in R:
            asrc, dst = R["fuse"](t)
            a_t, Ba = R["acr"].next()
            ph.dma(lq, lambda e: e.dma_start(out=a_t[:], in_=asrc), writes=[Ba])
            ph.op("dve", lambda e: e.tensor_tensor(out=x_t[:], in0=x_t[:], in1=a_t[:], op=ALU.add), reads=[Ba, Bx], writes=[Bx])
            ph.dma("sp", lambda e: e.dma_start(out=dst, in_=x_t[:]), reads=[Bx])
        ph.op("act", lambda e: e.activation(out=R["junk"][:], in_=x_t[:], func=AF.Square, accum_out=ss[:]), reads=[Bx], writes=[R["Bjunk"], Bss])
        ph.op("dve", lambda e: e.tensor_scalar(out=ss[:], in0=ss[:], scalar1=1.0 / D, scalar2=EPS, op0=ALU.mult, op1=ALU.add), reads=[Bss], writes=[Bss])
        ph.op("pool", lambda e: e.tensor_tensor(out=ss[:], in0=ss[:], in1=self.mhalf[:, 0:1], op=ALU.pow), reads=[Bss], writes=[Bss])
        ph.op("dve", lambda e: e.tensor_scalar(out=xn_t[:], in0=x_t[:], scalar1=ss[:, 0:1], scalar2=None, op0=ALU.mult), reads=[Bx, Bss], writes=[Bxn])
        for k in range(8):
            ph.op("pe", lambda e, k=k: e.transpose(out=p_t[:, k, :], in_=xn_t[:, k * 128:(k + 1) * 128], identity=self.ident_b[:]),
                  reads=[Bxn], writes=[BpT], signal=(k == 7))
        for k in range(8):
            wr = [BhT if t % 2 == 0 else BhTa] if k in (0, 7) else []
            if t % 2 == 0:
                ph.op("dve", lambda e, k=k: e.tensor_scalar(out=hT[:, k, t * 128:(t + 1) * 128], in0=p_t[:, k, :], scalar1=self.A1T[:, k, 0:1],
                                                            scalar2=self.mT[:, k, 0:1], op0=ALU.mult, op1=ALU.add), reads=[BpT], writes=wr)
            else:
                ph.op("act", lambda e, k=k: e.activation(out=hT[:, k, t * 128:(t + 1) * 128], in_=p_t[:, k, :], func=AF.Identity,
                                                         scale=self.A1T[:, k, 0:1], bias=self.mT[:, k, 0:1]), reads=[BpT], writes=wr)

    def l1p1(self, es_out):
        nc = self.nc
        self.cxT = self.sb(es_out, "cxT", [128, 8, S + 2], BF16)
        with contextlib.ExitStack() as es:
            w_cx = self.sb(es, "w_cx", [128, 8, 2 * D], BF16)
            R = {"ssq": self.rb(es, "ssq", [128, 1], F32, 3), "xn": self.rb(es, "xn", [128, D], BF16, 2),
                 "pT": self.rps(es, "pT", [128, 8, 128], BF16, 2), "junk": self.sb(es, "junk", [128, D], BF16), "Bjunk": Buf("junk")}
            xt = self.rb(es, "xt", [128, D], F32, 3)
            hnT = self.rb(es, "hnT", [128, 8, 512], BF16, 2)
            xvs = self.rb(es, "xvs", [128, 512], F32, 2)
            acr = self.rb(es, "acr1", [128, D], F32, 2)
            pC = self.rps(es, "pC", [128, 512], F32, 2)
            pX = self.rps(es, "pX", [128, 512], F32, 2)
            ph = Phase(nc, "l1p1")
            Bw, Bout = Buf("w"), Buf("out")
            ph.dma("pool", lambda e: e.dma_start(out=w_cx[:], in_=self.sc_w_in[:, D:3 * D].rearrange("(k p) n -> p k n", p=128)), writes=[Bw])
            ph.op("pool", lambda e: e.memset(self.cxT[:, :, 0:1], 0.0), writes=[])
            ph.op("pool", lambda e: e.memset(self.cxT[:, :, S + 1:S + 2], 0.0), writes=[])
            for st in range(8):
                hT, BhT = hnT.next()
                R["acr"] = acr
                R["lq"] = "act"
                R["fuse"] = lambda t, st=st: (self.acc[(st * 4 + t) * 128:(st * 4 + t + 1) * 128, :], self.hbuf2[(st * 4 + t) * 128:(st * 4 + t + 1) * 128, :])
                for t in range(4):
                    i = st * 4 + t
                    x_t, Bx = xt.next()
                    self.norm1_tile(ph, R, self.hbuf[i * 128:(i + 1) * 128, :], t, hT, BhT, x_t, Bx)
                for ch in range(8):
                    pc_t, Bpc = pC.next()
                    px_t, Bpx = pX.next()
                    for k in range(8):
                        ph.op("pe", lambda e, pc_t=pc_t, k=k, ch=ch, hT=hT: e.matmul(pc_t[:], lhsT=w_cx[:, k, ch * 128:(ch + 1) * 128], rhs=hT[:, k, :], start=(k == 0), stop=(k == 7)),
                              reads=[Bw, BhT, BhT.twin], writes=[Bpc], signal=(k == 7))
                    for k in range(8):
                        ph.op("pe", lambda e, px_t=px_t, k=k, ch=ch, hT=hT: e.matmul(px_t[:], lhsT=w_cx[:, k, D + ch * 128:D + (ch + 1) * 128], rhs=hT[:, k, :], start=(k == 0), stop=(k == 7)),
                              reads=[Bw, BhT, BhT.twin], writes=[Bpx], signal=(k == 7))
                    xv_t, Bxv = xvs.next()
                    ph.op("act", lambda e, xv_t=xv_t, px_t=px_t: e.activation(out=xv_t[:], in_=px_t[:], func=AF.Copy), reads=[Bpx], writes=[Bxv])
                    ph.op("dve", lambda e, xv_t=xv_t, pc_t=pc_t, ch=ch, st=st: e.tensor_tensor(out=self.cxT[:, ch, 1 + st * 512:1 + (st + 1) * 512], in0=pc_t[:], in1=xv_t[:], op=ALU.mult),
                          reads=[Bpc, Bxv], writes=[])
            ph.finish()

    def l1p2(self):
        nc = self.nc
        with contextlib.ExitStack() as es:
            w_b = self.sb(es, "w_b", [128, 8, D], BF16)
            w_o = self.sb(es, "w_o", [128, 8, D], BF16)
            dg3 = self.sb(es, "dg3", [128, 3, 8, 128], BF16)
            cw3 = self.sb(es, "cw3", [128, 8, 3], F32)
            R = {"ssq": self.rb(es, "ssq", [128, 1], F32, 3), "xn": self.rb(es, "xn", [128, D], BF16, 2),
                 "pT": self.rps(es, "pT", [128, 8, 128], BF16, 1), "junk": self.sb(es, "junk", [128, D], BF16), "Bjunk": Buf("junk")}
            xt = self.rb(es, "xt", [128, D], F32, 5)
            hnT = self.rb(es, "hnT", [128, 8, 512], BF16, 1)
            zT = self.rb(es, "zT", [128, 8, 512], BF16, 1)
            ycs = self.rb(es, "ycs", [128, 512], F32, 2)
            T = self.tail_alloc(es)
            pB = self.rps(es, "pB", [128, 512], F32, 2)
            pYc = self.rps(es, "pYc", [128, 512], F32, 1)
            po = self.rps(es, "po", [128, 512], F32, 2)
            ph = Phase(nc, "l1p2")
            Bw, Bcw, Bdg = Buf("w"), Buf("cw"), Buf("dg")
            ph.dma("pool", lambda e: e.dma_start(out=w_b[:], in_=self.sc_w_in[:, 0:D].rearrange("(k p) n -> p k n", p=128)), writes=[Bw])
            ph.dma("pool", lambda e: e.dma_start(out=w_o[:], in_=self.sc_w_out.rearrange("(k p) n -> p k n", p=128)), writes=[Bw])
            ph.dma("sp", lambda e: e.dma_start(out=cw3[:], in_=self.sc_conv_wT), writes=[Bcw])
            for j in range(3):
                for ch in range(8):
                    ph.op("dve", lambda e, j=j, ch=ch: e.tensor_scalar(out=dg3[:, j, ch, :], in0=self.ident_b[:], scalar1=cw3[:, ch, j:j + 1], scalar2=None, op0=ALU.mult),
                          reads=[Bcw], writes=[Bdg])
            for st in range(8):
                hT, BhT = hnT.next()
                z_t, Bz = zT.next()
                tiles = []
                R["lq"] = "act"
                for t in range(4):
                    i = st * 4 + t
                    x_t, Bx = xt.next()
                    self.norm1_tile(ph, R, self.hbuf2[i * 128:(i + 1) * 128, :], t, hT, BhT, x_t, Bx)
                    tiles.append((x_t, Bx))
                for ch in range(8):
                    pb_t, Bpb = pB.next()
                    py_t, Bpy = pYc.next()
                    for k in range(8):
                        ph.op("pe", lambda e, pb_t=pb_t, k=k, ch=ch, hT=hT: e.matmul(pb_t[:], lhsT=w_b[:, k, ch * 128:(ch + 1) * 128], rhs=hT[:, k, :], start=(k == 0), stop=(k == 7)),
                              reads=[Bw, BhT, BhT.twin], writes=[Bpb], signal=(k == 7))
                    for j in range(3):
                        ph.op("pe", lambda e, py_t=py_t, j=j, ch=ch, st=st: e.matmul(py_t[:], lhsT=dg3[:, j, ch, :], rhs=self.cxT[:, ch, st * 512 + j:st * 512 + j + 512], start=(j == 0), stop=(j == 2)),
                              reads=[Bdg], writes=[Bpy], signal=(j == 2))
                    yc_t, Byc = ycs.next()
                    ph.op("act", lambda e, yc_t=yc_t, py_t=py_t: e.activation(out=yc_t[:], in_=py_t[:], func=AF.Copy), reads=[Bpy], writes=[Byc])
                    ph.op("dve", lambda e, yc_t=yc_t, pb_t=pb_t, z_t=z_t, ch=ch: e.tensor_tensor(out=z_t[:, ch, :], in0=pb_t[:], in1=yc_t[:], op=ALU.mult), reads=[Bpb, Byc], writes=[Bz])
                for t in range(4):
                    i = st * 4 + t
                    pol = []
                    for n in range(2):
                        po_t, Bpo = po.next()
                        for ch in range(8):
                            ph.op("pe", lambda e, po_t=po_t, z_t=z_t, ch=ch, n=n, t=t: e.matmul(po_t[:], lhsT=z_t[:, ch, t * 128:(t + 1) * 128], rhs=w_o[:, ch, n * 512:(n + 1) * 512],
                                                                                              start=(ch == 0), stop=(ch == 7)), reads=[Bz, Bw], writes=[Bpo], signal=(ch == 7))
                        pol.append((po_t[:], Bpo))
                    x_t, Bx = tiles[t]
                    self.tail_tile(ph, T, i, pol, x_t, Bx, self.hbuf)
            ph.finish()

    def run(self):
        nc = self.nc
        self.declare()
        for nm, shape, dt in (("mrow", [2, 6 * D], F32), ("qT", [128, 4, S], BF16), ("kT2", [128, 2, NKT * 128], BF16),
                              ("Vaug", [128, NKT, 2, 65], BF16), ("gT", [128, 4, S + 30], BF16), ("attnT", [128, 4, S], BF16),
                              ("convT", [128, 4, S], BF16), ("aff", [128, NT, NE], F32), ("hmid", [S, D], F32), ("h2aug", [S, RW], BF16),
                              ("slotm", [128, NT * NE], F32), ("thr", [16, 8], F32), ("acc", [S, D], F32), ("h1", [S, D], F32)):
            self.dbg_out(nm, shape, dt)
        with contextlib.ExitStack() as es0:
            _SEMS[0] = Sems(nc, es0)
            self.consts(es0)
            self.ada(0)
            with contextlib.ExitStack() as es_l0:
                self.l0a(es_l0)
                if self.stop == "l0a":
                    self.es_a.close()
                    self.finish_dummy()
                    return
                self.attn(es_l0)
                self.es_a.close()
                if self.stop == "attn":
                    self.finish_dummy()
                    return
                self.conv(es_l0)
                self.outproj0()
                if self.stop == "outproj0":
                    self.finish_dummy()
                    return
            with contextlib.ExitStack() as es_m:
                self.route(es_m, 0)
                if self.stop == "route0":
                    self.finish_dummy()
                    return
                self.moe(0)
            if self.stop == "moe0":
                self.dump_h2()
                self.finish_dummy()
                return
            self.ada(1)
            with contextlib.ExitStack() as es_l1:
                self.l1p1(es_l1)
                self.l1p2()
            if self.stop == "l1":
                self.dump_tail()
                self.finish_dummy()
                return
            with contextlib.ExitStack() as es_m:
                self.route(es_m, 1)
                self.moe(1)
            self.resid(1, self.hbuf, self.out)

    def finish_dummy(self):
        ph = Phase(self.nc, "fin")
        B = Buf("z")
        ph.dma("sp", lambda e: e.dma_start(out=self.out[0:128, :], in_=self.zeros[:, 0:D]), reads=[B])
        ph.finish()


def build(debug=None, stop=None):
    nc = bass.Bass("TRN2", target_bir_lowering=False)
    k = Kern(nc, debug, stop)
    k.run()
    return nc, k


def rope_tables():
    rows = S // 64
    row = np.repeat(np.arange(rows, dtype=np.float32), 64)
    col = np.tile(np.arange(64, dtype=np.float32), rows)
    inv = (10000.0 ** (-np.arange(0, 32, 2, dtype=np.float32) / 32)).astype(np.float32)
    ang = np.concatenate([row[:, None] * inv, col[:, None] * inv], axis=-1).astype(np.float32)
    cos = np.cos(ang).astype(np.float32).reshape(NT, 128, 32).transpose(1, 0, 2)
    sin = np.sin(ang).astype(np.float32).reshape(NT, 128, 32).transpose(1, 0, 2)
    return np.ascontiguousarray(cos), np.ascontiguousarray(sin)


def prep_inputs(inp):
    f = lambda a: np.ascontiguousarray(np.asarray(a, dtype=np.float32))
    cos, sin = rope_tables()
    shared = {
        "ada_w": f(inp["ada_w"]),
        "ada_b": f(inp["ada_b"]).reshape(2, 1, 6 * D),
        "g1T": f(np.asarray(inp["norm1_g"]).reshape(2, 8, 128).transpose(0, 2, 1)),
        "g2row": f(inp["norm2_g"]).reshape(2, 1, D),
        "ev_w_in": f(inp["ev_w_in"][0]),
        "ev_w_out": f(inp["ev_w_out"][0]),
        "qkg": f(np.concatenate([np.tile(np.asarray(inp["ev_q_g"][0]), 8), np.tile(np.asarray(inp["ev_k_g"][0]), 2)])).reshape(1, 640),
        "qg": f(inp["ev_q_g"][0]).reshape(1, 64),
        "kg": f(inp["ev_k_g"][0]).reshape(1, 64),
        "conv_wT": f(np.asarray(inp["ev_conv_w"][0]).reshape(31, 4, 128).transpose(2, 1, 0)),
        "conv_vec": f(np.stack([np.asarray(inp["ev_conv_b"][0]).reshape(4, 128).T,
                                np.asarray(inp["ev_ln_g"][0]).reshape(4, 128).T,
                                np.asarray(inp["ev_ln_b"][0]).reshape(4, 128).T], axis=1)),
        "rope_cos": cos,
        "rope_sin": sin,
        "sc_w_in": f(inp["sc_w_in"][0]),
        "sc_conv_wT": f(np.asarray(inp["sc_conv_w"][0]).reshape(3, 8, 128).transpose(2, 1, 0)),
        "sc_w_out": f(inp["sc_w_out"][0]),
        "w_r": f(np.asarray(inp["moe_w_r"]).reshape(2, 8, 128, NE).transpose(0, 2, 1, 3)),
        "w_gate": f(inp["moe_w_gate"]),
        "w_up": f(inp["moe_w_up"]),
        "w_down": f(inp["moe_w_down"]),
        "final_g": f(inp["final_g"]).reshape(1, D),
        "Gm": f((np.arange(128)[:, None] % 16) == (np.arange(128)[None, :] % 16)),
    }
    maps = []
    c = np.asarray(inp["c"], dtype=np.float32)
    cc = np.asarray(inp["c_ctx"], dtype=np.float32)
    for core in range(8):
        b = core % 4
        m = dict(shared)
        m["x"] = f(inp["x"][b])
        m["ctx"] = f(inp["ctx"][b])
        m["cT"] = f(np.stack([c[b].reshape(8, 128).T, cc.reshape(8, 128).T], axis=-1))
        maps.append(m)
    return maps


_CACHE = {}


def kernel(**inputs):
    if "nc" not in _CACHE:
        _CACHE["nc"] = build()[0]
    nc = _CACHE["nc"]
    maps = prep_inputs(inputs)
    res = run_bass_kernel_spmd(nc, maps, core_ids=list(range(8)))
    out = np.stack([np.asarray(res.results[b]["out"], dtype=np.float32) for b in range(4)], axis=0)
    return out
```

```python
import contextlib
import numpy as np
import concourse.bass as bass
import concourse.mybir as mybir
from concourse.bass_utils import run_bass_kernel_spmd

F32 = mybir.dt.float32
BF16 = mybir.dt.bfloat16
U32 = mybir.dt.uint32
I32 = mybir.dt.int32
ALU = mybir.AluOpType
AF = mybir.ActivationFunctionType
AX = mybir.AxisListType

ENGS = ["pe", "dve", "act", "pool", "sp"]
CENGS = ["pe", "dve", "act", "pool"]
NDS = 24
NDS_HW = 16
SAME_ENGINE_SYNC = True

D = 1024
S = 4096
CTX = 256
NT = S // 128
NKT = NT + 2
EPS = 1e-6
NE = 16
CAP = 512
RW = 1072


class Buf:
    def __init__(self, name):
        self.name = name
        self.w = None
        self.r = {}


class Sems:
    def __init__(self, nc, es):
        self.esem = {e: es.enter_context(nc.semaphore(f"s_{e}")) for e in CENGS}
        self.ecnt = {e: 0 for e in CENGS}
        self.dsem = [es.enter_context(nc.semaphore(f"s_d{i}")) for i in range(NDS)]
        self.dcnt = [0] * NDS
        self.dnext = 0
        self.dnext_sw = 0
        self.waited = {e: {} for e in ENGS}


_SEMS = [None]


class Phase:
    def __init__(self, nc, name):
        self.nc = nc
        self.name = name
        sm = _SEMS[0]
        self.sm = sm
        self.q = {e: [] for e in ENGS}
        self.esem = sm.esem
        self.ecnt = sm.ecnt
        self.pending = {e: False for e in CENGS}
        self.dsem = sm.dsem
        self.dcnt = sm.dcnt
        self.waited = sm.waited

    def _sem(self, key):
        return self.esem[key[1]] if key[0] == "e" else self.dsem[key[1]]

    def _wait(self, engine, tok, force=False):
        if tok is None:
            return
        key, val = tok
        if not force and key[0] == "e" and key[1] == engine and (engine == "pe" or not SAME_ENGINE_SYNC):
            return
        if self.waited[engine].get(key, 0) >= val:
            return
        self.waited[engine][key] = val
        sem = self._sem(key)
        self.q[engine].append(lambda eng, sem=sem, val=val: eng.wait_ge(sem, val))

    def _deps(self, engine, reads, writes):
        for b in reads:
            self._wait(engine, b.w)
        for b in writes:
            self._wait(engine, b.w)
            for t in b.r.values():
                self._wait(engine, t)

    def _update(self, tok, reads, writes):
        for b in reads:
            b.r[tok[0]] = tok
        for b in writes:
            b.w = tok
            b.r = {}

    def op(self, engine, fn, reads=(), writes=(), signal=True):
        self._deps(engine, reads, writes)
        if signal:
            self.ecnt[engine] += 1
            tok = (("e", engine), self.ecnt[engine])
            sem = self.esem[engine]
            self.q[engine].append(lambda eng, fn=fn, sem=sem: fn(eng).then_inc(sem, 1))
            self.pending[engine] = False
        else:
            tok = (("e", engine), self.ecnt[engine] + 1)
            self.q[engine].append(lambda eng, fn=fn: fn(eng))
            self.pending[engine] = True
        self._update(tok, reads, writes)
        return tok

    def dma(self, queue, fn, reads=(), writes=()):
        self._deps(queue, reads, writes)
        sm = self.sm
        if queue == "pool":
            i = NDS_HW + sm.dnext_sw
            sm.dnext_sw = (sm.dnext_sw + 1) % (NDS - NDS_HW)
        else:
            i = sm.dnext
            sm.dnext = (sm.dnext + 1) % NDS_HW
        if self.dcnt[i] > 0:
            self._wait(queue, (("d", i), self.dcnt[i]))
        self.dcnt[i] += 16
        tok = (("d", i), self.dcnt[i])
        sem = self.dsem[i]
        self.q[queue].append(lambda eng, fn=fn, sem=sem: fn(eng).then_inc(sem, 16))
        self._update(tok, reads, writes)
        return tok

    def finish(self):
        nc = self.nc
        for e in CENGS:
            assert not self.pending[e], f"engine {e} has pending unsignaled ops in {self.name}"
        for e in ENGS:
            for e2 in CENGS:
                if self.ecnt[e2] > 0:
                    self._wait(e, (("e", e2), self.ecnt[e2]), force=True)
            for i in range(NDS):
                if self.dcnt[i] > 0:
                    self._wait(e, (("d", i), self.dcnt[i]), force=True)
        q = self.q
        with nc.Block() as block:
            @block.tensor
            def _(eng):
                for f in q["pe"]:
                    f(eng)

            @block.vector
            def _(eng):
                for f in q["dve"]:
                    f(eng)

            @block.scalar
            def _(eng):
                for f in q["act"]:
                    f(eng)

            @block.gpsimd
            def _(eng):
                for f in q["pool"]:
                    f(eng)

            @block.sync
            def _(eng):
                for f in q["sp"]:
                    f(eng)


class Ring:
    def __init__(self, items):
        self.items = items
        self.i = 0

    def next(self):
        it = self.items[self.i % len(self.items)]
        self.i += 1
        return it


def v3(ap, d):
    return ap.rearrange("p (h d) -> p h d", d=d)


class Kern:
    def __init__(self, nc, debug=None, stop=None):
        self.nc = nc
        self.debug = debug or []
        self.stop = stop
        self.dbg = {}
        self.uid = 0

    def sb(self, es, name, shape, dt):
        self.uid += 1
        return es.enter_context(self.nc.sbuf_tensor(f"{name}_{self.uid}", shape, dt))

    def ps(self, es, name, shape, dt):
        self.uid += 1
        return es.enter_context(self.nc.psum_tensor(f"{name}_{self.uid}", shape, dt))

    def rb(self, es, name, shape, dt, n):
        return Ring([(self.sb(es, f"{name}{i}", shape, dt), Buf(f"{name}{i}")) for i in range(n)])

    def rps(self, es, name, shape, dt, n):
        return Ring([(self.ps(es, f"{name}{i}", shape, dt), Buf(f"{name}{i}")) for i in range(n)])

    def dram_in(self, name, shape, dt=F32):
        return self.nc.dram_tensor(name, list(shape), dt, kind="ExternalInput").ap()

    def dram_out(self, name, shape, dt=F32):
        return self.nc.dram_tensor(name, list(shape), dt, kind="ExternalOutput").ap()

    def dram_tmp(self, name, shape, dt=F32):
        return self.nc.dram_tensor(name, list(shape), dt, kind="Internal").ap()

    def dbg_out(self, name, shape, dt=F32):
        if name in self.debug:
            self.dbg[name] = self.dram_out("dbg_" + name, shape, dt)
            return self.dbg[name]
        return None

    def declare(self):
        self.x = self.dram_in("x", [S, D])
        self.ctx = self.dram_in("ctx", [CTX, D])
        self.cT = self.dram_in("cT", [128, 8, 2])
        self.ada_w = self.dram_in("ada_w", [2, D, 6 * D])
        self.ada_b = self.dram_in("ada_b", [2, 1, 6 * D])
        self.g1T = self.dram_in("g1T", [2, 128, 8])
        self.g2row = self.dram_in("g2row", [2, 1, D])
        self.ev_w_in = self.dram_in("ev_w_in", [D, 1792])
        self.ev_w_out = self.dram_in("ev_w_out", [D, D])
        self.qkg = self.dram_in("qkg", [1, 640])
        self.qg = self.dram_in("qg", [1, 64])
        self.kg = self.dram_in("kg", [1, 64])
        self.conv_wT = self.dram_in("conv_wT", [128, 4, 31])
        self.conv_vec = self.dram_in("conv_vec", [128, 3, 4])
        self.rope_cos = self.dram_in("rope_cos", [128, NT, 32])
        self.rope_sin = self.dram_in("rope_sin", [128, NT, 32])
        self.sc_w_in = self.dram_in("sc_w_in", [D, 3 * D])
        self.sc_conv_wT = self.dram_in("sc_conv_wT", [128, 8, 3])
        self.sc_w_out = self.dram_in("sc_w_out", [D, D])
        self.w_r = self.dram_in("w_r", [2, 128, 8, NE])
        self.w_gate = self.dram_in("w_gate", [2, NE, D, D])
        self.w_up = self.dram_in("w_up", [2, NE, D, D])
        self.w_down = self.dram_in("w_down", [2, NE, D, D])
        self.final_g = self.dram_in("final_g", [1, D])
        self.Gm = self.dram_in("Gm", [128, 128])
        self.out = self.dram_out("out", [S, D])
        self.hbuf = self.dram_tmp("hbuf", [S, D])
        self.hbuf2 = self.dram_tmp("hbuf2", [S, D])
        self.acc = self.dram_tmp("acc", [S, D])
        self.h2aug = self.dram_tmp("h2aug", [S, RW], BF16)

    def consts(self, es):
        nc = self.nc
        self.ident_f = self.sb(es, "ident_f", [128, 128], F32)
        self.ident_b = self.sb(es, "ident_b", [128, 128], BF16)
        self.ones_b = self.sb(es, "ones_b", [128, 128], BF16)
        self.ones_f = self.sb(es, "ones_f", [128, 128], F32)
        self.ustr_b = self.sb(es, "ustr_b", [128, 128], BF16)
        self.sel2 = self.sb(es, "sel2", [2, 128], F32)
        self.iota512 = self.sb(es, "iota512", [128, 512], mybir.dt.float16)
        self.tokval = self.sb(es, "tokval", [128, NT, 2], BF16)
        self.mhalf = self.sb(es, "mhalf", [128, 16], F32)
        self.negC = self.sb(es, "negC", [128, 1], F32)
        self.zeros = self.sb(es, "zeros", [128, 1024], F32)
        self.mT = self.sb(es, "mT", [128, 48, 2], F32)
        self.A1T = self.sb(es, "A1T", [128, 8, 2], F32)
        self.G1bc = self.sb(es, "G1bc", [128, D], F32)
        self.A2bc = self.sb(es, "A2bc", [128, D], F32)
        self.B2bc = self.sb(es, "B2bc", [128, D], F32)
        self.G2bc = self.sb(es, "G2bc", [128, D], F32)
        self.wr_b = self.sb(es, "wr_b", [128, 8, NE], BF16)
        self.affTM = self.sb(es, "affTM", [128, NT, NE], F32)
        with contextlib.ExitStack() as es2:
            ii = self.sb(es2, "ii", [128, 512], I32)
            i2 = self.sb(es2, "i2", [128, NT], I32)
            i3 = self.sb(es2, "i3", [128, 1], I32)
            uf = self.sb(es2, "uf", [128, 128], F32)
            gq = self.sb(es2, "gq", [128, 128], F32)
            mx = self.sb(es2, "mx", [128, 2], F32)
            ph = Phase(nc, "const")
            B = Buf("c")
            W = dict(reads=[B], writes=[B])
            ph.op("pool", lambda e: e.memset(self.ident_f[:], 0.0), **W)
            ph.op("pool", lambda e: e.affine_select(out=self.ident_f[:], in_=self.ident_f[:], pattern=[[-1, 128]],
                                                    compare_op=ALU.not_equal, fill=1.0, base=0, channel_multiplier=1), **W)
            ph.op("dve", lambda e: e.tensor_copy(out=self.ident_b[:], in_=self.ident_f[:]), **W)
            ph.op("dve", lambda e: e.memset(self.ones_b[:], 1.0), **W)
            ph.op("dve", lambda e: e.memset(self.ones_f[:], 1.0), **W)
            ph.op("pool", lambda e: e.memset(uf[:], 0.0), **W)
            ph.op("pool", lambda e: e.affine_select(out=uf[:], in_=uf[:], pattern=[[-1, 128]],
                                                    compare_op=ALU.is_ge, fill=1.0, base=0, channel_multiplier=1), **W)
            ph.op("dve", lambda e: e.tensor_copy(out=self.ustr_b[:], in_=uf[:]), **W)
            ph.op("dve", lambda e: e.memset(self.sel2[:], 0.0), **W)
            ph.op("dve", lambda e: e.memset(self.sel2[0:1, :], 1.0), **W)
            ph.op("pool", lambda e: e.iota(ii[:], pattern=[[1, 512]], base=0, channel_multiplier=0), **W)
            ph.op("dve", lambda e: e.tensor_copy(out=self.iota512[:], in_=ii[:]), **W)
            ph.op("pool", lambda e: e.iota(i2[:], pattern=[[1, NT]], base=0, channel_multiplier=0), **W)
            ph.op("pool", lambda e: e.iota(i3[:], pattern=[[0, 1]], base=0, channel_multiplier=1), **W)
            ph.op("dve", lambda e: e.tensor_copy(out=self.tokval[:, :, 0], in_=i2[:]), **W)
            ph.op("dve", lambda e: e.tensor_copy(out=self.tokval[:, :, 1], in_=i3[:].broadcast_to([128, NT])), **W)
            ph.op("dve", lambda e: e.memset(self.mhalf[:], -0.5), **W)
            ph.op("dve", lambda e: e.memset(self.zeros[:], 0.0), **W)
            ph.dma("sp", lambda e: e.dma_start(out=gq[:, 0:64], in_=self.qg.partition_broadcast(128)), **W)
            ph.dma("sp", lambda e: e.dma_start(out=gq[:, 64:128], in_=self.kg.partition_broadcast(128)), **W)
            ph.op("dve", lambda e: e.tensor_reduce(out=mx[:], in_=v3(gq[:], 64), axis=AX.X, op=ALU.max,
                                                   apply_absolute_value=True), **W)
            ph.op("dve", lambda e: e.scalar_tensor_tensor(out=self.negC[:], in0=mx[:, 0:1], scalar=-8.0, in1=mx[:, 1:2],
                                                          op0=ALU.mult, op1=ALU.mult), **W)
            ph.finish()

    def ada(self, l):
        nc = self.nc
        with contextlib.ExitStack() as es:
            cT_s = self.sb(es, "cT_s", [128, 8, 2], F32)
            scT = self.sb(es, "scT", [128, 8, 2], F32)
            wblk = self.rb(es, "wblk", [128, 8, 512], F32, 2)
            brow = self.sb(es, "brow", [1, 6 * D], F32)
            mrow = self.sb(es, "mrow", [2, 6 * D], F32)
            g2r = self.sb(es, "g2r", [2, D], F32)
            a2row = self.sb(es, "a2row", [2, D], F32)
            g1T_s = self.sb(es, "g1T_s", [128, 8], F32)
            tmpA = self.sb(es, "tmpA", [128, 8, 2], F32)
            wr_f = self.sb(es, "wr_f", [128, 8, NE], F32)
            pm = self.rps(es, "pm", [128, 512], F32, 2)
            pTm = self.ps(es, "pTm", [128, 512], F32)
            pb = self.rps(es, "pb", [128, 512], F32, 2)
            ph = Phase(nc, f"ada{l}")
            Bc, Bsc, Bbrow, Bmrow, Bg2, Ba2, Bg1, BmT, BpT, Bvec, Bwr = [Buf(n) for n in "c sc brow mrow g2 a2 g1 mT pT vec wr".split()]
            ph.dma("sp", lambda e: e.dma_start(out=cT_s[:], in_=self.cT), writes=[Bc])
            ph.dma("sp", lambda e: e.dma_start(out=brow[:], in_=self.ada_b[l]), writes=[Bbrow])
            ph.dma("sp", lambda e: e.dma_start(out=g2r[:], in_=self.g2row[l].partition_broadcast(2)), writes=[Bg2])
            ph.dma("sp", lambda e: e.dma_start(out=g1T_s[:], in_=self.g1T[l]), writes=[Bg1])
            ph.dma("sp", lambda e: e.dma_start(out=wr_f[:], in_=self.w_r[l]), writes=[Bwr])
            ph.op("dve", lambda e: e.tensor_copy(out=self.wr_b[:], in_=wr_f[:]), reads=[Bwr], writes=[Bvec])
            ph.op("act", lambda e: e.activation(out=scT[:], in_=cT_s[:], func=AF.Silu), reads=[Bc], writes=[Bsc])
            for cb in range(12):
                wt, Bw = wblk.next()
                ph.dma("sp", lambda e, wt=wt, cb=cb: e.dma_start(
                    out=wt[:], in_=self.ada_w[l][:, cb * 512:(cb + 1) * 512].rearrange("(k p) n -> p k n", p=128)),
                    writes=[Bw])
                pt, Bp = pm.next()
                for k in range(8):
                    ph.op("pe", lambda e, pt=pt, wt=wt, k=k: e.matmul(pt[0:2, :], lhsT=scT[:, k, :], rhs=wt[:, k, :],
                                                                      start=(k == 0), stop=False),
                          reads=[Bsc, Bw], writes=[Bp], signal=False)
                ph.op("pe", lambda e, pt=pt, cb=cb: e.matmul(pt[0:2, :], lhsT=self.ones_f[0:1, 0:2],
                                                             rhs=brow[0:1, cb * 512:(cb + 1) * 512], start=False, stop=True),
                      reads=[Bbrow], writes=[Bp])
                ph.op("dve", lambda e, pt=pt, cb=cb: e.tensor_copy(out=mrow[:, cb * 512:(cb + 1) * 512], in_=pt[0:2, :]),
                      reads=[Bp], writes=[Bmrow])
            pTv = pTm[:, 0:96].rearrange("p (c t) -> p c t", t=2)
            for c in range(48):
                ph.op("pe", lambda e, c=c: e.transpose(out=pTv[:, c, :], in_=mrow[0:2, c * 128:(c + 1) * 128],
                                                       identity=self.ident_f[0:2, 0:2]),
                      reads=[Bmrow], writes=[BpT], signal=(c == 47))
            ph.op("dve", lambda e: e.tensor_copy(out=self.mT[:], in_=pTv), reads=[BpT], writes=[BmT])
            ph.op("dve", lambda e: e.tensor_scalar(out=tmpA[:], in0=self.mT[:, 8:16, :], scalar1=1.0, scalar2=None, op0=ALU.add),
                  reads=[BmT], writes=[Bvec])
            ph.op("dve", lambda e: e.tensor_tensor(out=self.A1T[:], in0=tmpA[:], in1=g1T_s[:].unsqueeze(2).broadcast_to([128, 8, 2]),
                                                   op=ALU.mult), reads=[Bvec, Bg1], writes=[Bvec])
            ph.op("dve", lambda e: e.tensor_scalar(out=a2row[:], in0=mrow[:, 4 * D:5 * D], scalar1=1.0, scalar2=None, op0=ALU.add),
                  reads=[Bmrow], writes=[Ba2])
            ph.op("dve", lambda e: e.tensor_tensor(out=a2row[:], in0=a2row[:], in1=g2r[:], op=ALU.mult),
                  reads=[Ba2, Bg2], writes=[Ba2])
            srcs = [(self.G1bc, mrow[:, 2 * D:3 * D], Bmrow), (self.A2bc, a2row[:], Ba2),
                    (self.B2bc, mrow[:, 3 * D:4 * D], Bmrow), (self.G2bc, mrow[:, 5 * D:6 * D], Bmrow)]
            for dst, src, Bs in srcs:
                for n in range(2):
                    pt, Bp = pb.next()
                    ph.op("pe", lambda e, pt=pt, src=src, n=n: e.matmul(pt[:], lhsT=self.sel2[:], rhs=src[0:2, n * 512:(n + 1) * 512],
                                                                         start=True, stop=True), reads=[Bs], writes=[Bp])
                    ph.op("act", lambda e, pt=pt, dst=dst, n=n: e.activation(out=dst[:, n * 512:(n + 1) * 512], in_=pt[:], func=AF.Copy),
                          reads=[Bp], writes=[Bvec])
            if "mrow" in self.dbg:
                ph.dma("sp", lambda e: e.dma_start(out=self.dbg["mrow"], in_=mrow[:]), reads=[Bmrow])
            ph.finish()

    def l0a(self, es_out):
        nc = self.nc
        self.gT = self.sb(es_out, "gT", [128, 4, S + 30], BF16)
        self.qT = self.sb(es_out, "qT", [128, 4, S], BF16)
        es_a = contextlib.ExitStack()
        self.es_a = es_a
        self.kT2 = self.sb(es_a, "kT2", [128, 2, NKT * 128], BF16)
        self.Vaug = self.sb(es_a, "Vaug", [128, NKT, 2, 65], BF16)
        with contextlib.ExitStack() as es:
            w_in_b = self.sb(es, "w_in_b", [128, 8, 1792], BF16)
            cos_t = self.sb(es, "cos_t", [128, NT, 32], F32)
            sin_t = self.sb(es, "sin_t", [128, NT, 32], F32)
            gq_bc = self.sb(es, "gq_bc", [128, 640], F32)
            junk = self.sb(es, "junk", [128, D], BF16)
            xt = self.rb(es, "xt", [128, D], F32, 3)
            ssq = self.rb(es, "ssq", [128, 1], F32, 3)
            xn = self.rb(es, "xn", [128, D], BF16, 2)
            hnT = self.rb(es, "hnT", [128, 8, 512], BF16, 2)
            sq = self.rb(es, "sq", [128, 640], F32, 1)
            s10 = self.rb(es, "s10", [128, 10], F32, 2)
            qk = self.rb(es, "qk", [128, 640], F32, 1)
            rt = self.rb(es, "rt", [128, 4, 320], F32, 1)
            qkr = self.rb(es, "qkr", [128, 640], BF16, 2)
            kd = self.rb(es, "kd", [128, 256], BF16, 2)
            sig = self.rb(es, "sig", [128, 512], F32, 1)
            pT = self.rps(es, "pT", [128, 8, 128], BF16, 2)
            pq = self.rps(es, "pq", [128, 512], F32, 2)
            pkv = self.rps(es, "pkv", [128, 512], F32, 1)
            pqk = self.rps(es, "pqk", [128, 8, 128], BF16, 1)
            pu = self.rps(es, "pu", [128, 512], F32, 1)
            pg = self.rps(es, "pg", [128, 512], F32, 1)
            ph = Phase(nc, "l0a")
            Bw, Bcs, Bgq, Bjunk, Bout = [Buf(n) for n in "w cs gq junk out".split()]
            ph.dma("pool", lambda e: e.dma_start(out=w_in_b[:], in_=self.ev_w_in.rearrange("(k p) n -> p k n", p=128)), writes=[Bw])
            ph.dma("sp", lambda e: e.dma_start(out=cos_t[:], in_=self.rope_cos), writes=[Bcs])
            ph.dma("sp", lambda e: e.dma_start(out=sin_t[:], in_=self.rope_sin), writes=[Bcs])
            ph.dma("sp", lambda e: e.dma_start(out=gq_bc[:], in_=self.qkg.partition_broadcast(128)), writes=[Bgq])
            ph.op("pool", lambda e: e.memset(self.gT[:, :, 0:15], 0.0), writes=[])
            ph.op("pool", lambda e: e.memset(self.gT[:, :, S + 15:S + 30], 0.0), writes=[])
            ph.op("pool", lambda e: e.memset(self.Vaug[:, :, :, 64:65], 1.0), writes=[])
            evq = 0
            for st in range(9):
                is_ctx = st == 0
                ntile = 2 if is_ctx else 4
                col = 1 if is_ctx else 0
                hT, BhT = hnT.next()
                BhTa = BhT.__dict__.setdefault("twin", Buf(BhT.name + "a"))
                for t in range(ntile):
                    if is_ctx:
                        src = self.ctx[t * 128:(t + 1) * 128, :]
                        kt = t
                        lt = None
                    else:
                        lt = (st - 1) * 4 + t
                        src = self.x[lt * 128:(lt + 1) * 128, :]
                        kt = 2 + lt
                    x_t, Bx = xt.next()
                    ss, Bss = ssq.next()
                    xn_t, Bxn = xn.next()
                    ph.dma("sp", lambda e, x_t=x_t, src=src: e.dma_start(out=x_t[:], in_=src), writes=[Bx])
                    ph.op("act", lambda e, x_t=x_t, ss=ss: e.activation(out=junk[:], in_=x_t[:], func=AF.Square, accum_out=ss[:]),
                          reads=[Bx], writes=[Bjunk, Bss])
                    ph.op("dve", lambda e, ss=ss: e.tensor_scalar(out=ss[:], in0=ss[:], scalar1=1.0 / D, scalar2=EPS, op0=ALU.mult, op1=ALU.add),
                          reads=[Bss], writes=[Bss])
                    ph.op("pool", lambda e, ss=ss: e.tensor_tensor(out=ss[:], in0=ss[:], in1=self.mhalf[:, 0:1], op=ALU.pow),
                          reads=[Bss], writes=[Bss])
                    ph.op("dve", lambda e, x_t=x_t, ss=ss, xn_t=xn_t: e.tensor_scalar(out=xn_t[:], in0=x_t[:], scalar1=ss[:, 0:1], scalar2=None, op0=ALU.mult),
                          reads=[Bx, Bss], writes=[Bxn])
                    p_t, BpT = pT.next()
                    for k in range(8):
                        ph.op("pe", lambda e, p_t=p_t, xn_t=xn_t, k=k: e.transpose(out=p_t[:, k, :], in_=xn_t[:, k * 128:(k + 1) * 128], identity=self.ident_b[:]),
                              reads=[Bxn], writes=[BpT], signal=(k == 7))
                    for k in range(8):
                        wr = [BhT if t % 2 == 0 else BhTa] if k in (0, 7) else []
                        if t % 2 == 0:
                            ph.op("dve", lambda e, p_t=p_t, hT=hT, k=k, t=t, col=col: e.tensor_scalar(
                                out=hT[:, k, t * 128:(t + 1) * 128], in0=p_t[:, k, :], scalar1=self.A1T[:, k, col:col + 1],
                                scalar2=self.mT[:, k, col:col + 1], op0=ALU.mult, op1=ALU.add), reads=[BpT], writes=wr)
                        else:
                            ph.op("act", lambda e, p_t=p_t, hT=hT, k=k, t=t, col=col: e.activation(
                                out=hT[:, k, t * 128:(t + 1) * 128], in_=p_t[:, k, :], func=AF.Identity,
                                scale=self.A1T[:, k, col:col + 1], bias=self.mT[:, k, col:col + 1]), reads=[BpT], writes=wr)
                    pkv_t, Bpkv = pkv.next()
                    for k in range(8):
                        ph.op("pe", lambda e, pkv_t=pkv_t, hT=hT, k=k, t=t: e.matmul(pkv_t[:, 0:256], lhsT=hT[:, k, t * 128:(t + 1) * 128], rhs=w_in_b[:, k, 512:768],
                                                                                   start=(k == 0), stop=(k == 7)), reads=[BhT, BhTa, Bw], writes=[Bpkv], signal=(k == 7))
                    sq_t, Bsq = sq.next()
                    s_t, Bs10 = s10.next()
                    qk_t, Bqk = qk.next()
                    qkr_t, Bqkr = qkr.next()
                    if not is_ctx:
                        pq_t, Bpq = pq.next()
                        for k in range(8):
                            ph.op("pe", lambda e, pq_t=pq_t, hT=hT, k=k, t=t: e.matmul(pq_t[:], lhsT=hT[:, k, t * 128:(t + 1) * 128], rhs=w_in_b[:, k, 0:512],
                                                                                     start=(k == 0), stop=(k == 7)), reads=[BhT, BhTa, Bw], writes=[Bpq], signal=(k == 7))
                        ph.op("act", lambda e, sq_t=sq_t, pq_t=pq_t: e.activation(out=sq_t[:, 0:512], in_=pq_t[:], func=AF.Square), reads=[Bpq], writes=[Bsq, Bpq])
                    h0 = 8 if is_ctx else 0
                    ph.op("act", lambda e, sq_t=sq_t, pkv_t=pkv_t: e.activation(out=sq_t[:, 512:640], in_=pkv_t[:, 0:128], func=AF.Square), reads=[Bpkv], writes=[Bsq, Bpkv])
                    ph.op("dve", lambda e, s_t=s_t, sq_t=sq_t, h0=h0: e.tensor_reduce(out=s_t[:, h0:10], in_=v3(sq_t[:, h0 * 64:640], 64), axis=AX.X, op=ALU.add),
                          reads=[Bsq], writes=[Bs10])
                    ph.op("dve", lambda e, s_t=s_t, h0=h0: e.tensor_scalar(out=s_t[:, h0:10], in0=s_t[:, h0:10], scalar1=1.0 / 64, scalar2=EPS, op0=ALU.mult, op1=ALU.add),
                          reads=[Bs10], writes=[Bs10])
                    ph.op("pool", lambda e, s_t=s_t, h0=h0: e.tensor_tensor(out=s_t[:, h0:10], in0=s_t[:, h0:10], in1=self.mhalf[:, h0:10], op=ALU.pow),
                          reads=[Bs10], writes=[Bs10])
                    if not is_ctx:
                        ph.op("dve", lambda e, qk_t=qk_t, pq_t=pq_t, s_t=s_t: e.tensor_tensor(out=v3(qk_t[:, 0:512], 64), in0=v3(pq_t[:], 64),
                                                                                            in1=s_t[:, 0:8].unsqueeze(2).broadcast_to([128, 8, 64]), op=ALU.mult),
                              reads=[Bpq, Bs10], writes=[Bqk, Bpq])
                    ph.op("dve", lambda e, qk_t=qk_t, pkv_t=pkv_t, s_t=s_t: e.tensor_tensor(out=v3(qk_t[:, 512:640], 64), in0=v3(pkv_t[:, 0:128], 64),
                                                                                          in1=s_t[:, 8:10].unsqueeze(2).broadcast_to([128, 2, 64]), op=ALU.mult),
                          reads=[Bpkv, Bs10], writes=[Bqk, Bpkv])
                    ph.op("act", lambda e, pkv_t=pkv_t, kt=kt: e.activation(out=self.Vaug[:, kt, :, 0:64], in_=v3(pkv_t[:, 128:256], 64), func=AF.Copy),
                          reads=[Bpkv], writes=[Bpkv])
                    c0 = h0 * 64
                    ph.op("dve", lambda e, qk_t=qk_t, c0=c0: e.tensor_tensor(out=qk_t[:, c0:640], in0=qk_t[:, c0:640], in1=gq_bc[:, c0:640], op=ALU.mult),
                          reads=[Bqk, Bgq], writes=[Bqk])
                    if is_ctx:
                        ph.op("dve", lambda e, qkr_t=qkr_t, qk_t=qk_t: e.tensor_copy(out=qkr_t[:, 512:640], in_=qk_t[:, 512:640]), reads=[Bqk], writes=[Bqkr])
                    else:
                        r_t, Brt = rt.next()
                        q3 = v3(qk_t[:], 64)
                        o3 = v3(qkr_t[:], 64)
                        cb = cos_t[:, lt, :].unsqueeze(1).broadcast_to([128, 10, 32])
                        sbc = sin_t[:, lt, :].unsqueeze(1).broadcast_to([128, 10, 32])
                        r3 = [v3(r_t[:, i, :], 32) for i in range(4)]
                        ph.op("dve", lambda e, r3=r3, q3=q3, cb=cb: e.tensor_tensor(out=r3[0], in0=q3[:, :, 0:32], in1=cb, op=ALU.mult), reads=[Bqk, Bcs], writes=[Brt])
                        ph.op("pool", lambda e, r3=r3, q3=q3, sbc=sbc: e.tensor_tensor(out=r3[1], in0=q3[:, :, 32:64], in1=sbc, op=ALU.mult), reads=[Bqk, Bcs], writes=[Brt])
                        ph.op("dve", lambda e, r3=r3, q3=q3, cb=cb: e.tensor_tensor(out=r3[2], in0=q3[:, :, 32:64], in1=cb, op=ALU.mult), reads=[Bqk, Bcs], writes=[Brt])
                        ph.op("pool", lambda e, r3=r3, q3=q3, sbc=sbc: e.tensor_tensor(out=r3[3], in0=q3[:, :, 0:32], in1=sbc, op=ALU.mult), reads=[Bqk, Bcs], writes=[Brt])
                        ph.op("dve", lambda e, r3=r3, o3=o3: e.tensor_tensor(out=o3[:, :, 0:32], in0=r3[0], in1=r3[1], op=ALU.subtract), reads=[Brt], writes=[Bqkr])
                        ph.op("dve", lambda e, r3=r3, o3=o3: e.tensor_tensor(out=o3[:, :, 32:64], in0=r3[2], in1=r3[3], op=ALU.add), reads=[Brt], writes=[Bqkr])
                    kd_t, Bkd = kd.next()
                    ph.op("pool", lambda e, kd_t=kd_t, qkr_t=qkr_t: e.tensor_copy(
                        out=kd_t[:].rearrange("p (g r d) -> p g r d", g=2, r=2),
                        in_=v3(qkr_t[:, 512:640], 64).unsqueeze(2).broadcast_to([128, 2, 2, 64])), reads=[Bqkr], writes=[Bkd])
                    pqk_t, Bpqk = pqk.next()
                    if not is_ctx:
                        for p in range(4):
                            ph.op("pe", lambda e, pqk_t=pqk_t, qkr_t=qkr_t, p=p: e.transpose(out=pqk_t[:, p, :], in_=qkr_t[:, p * 128:(p + 1) * 128], identity=self.ident_b[:]),
                                  reads=[Bqkr], writes=[Bpqk], signal=False)
                    for g in range(2):
                        ph.op("pe", lambda e, pqk_t=pqk_t, kd_t=kd_t, g=g: e.transpose(out=pqk_t[:, 4 + g, :], in_=kd_t[:, g * 128:(g + 1) * 128], identity=self.ident_b[:]),
                              reads=[Bkd], writes=[Bpqk], signal=(g == 1))
                    if not is_ctx:
                        ph.op("act", lambda e, pqk_t=pqk_t, lt=lt: e.activation(out=self.qT[:, :, lt * 128:(lt + 1) * 128], in_=pqk_t[:, 0:4, :], func=AF.Copy),
                              reads=[Bpqk], writes=[Bpqk])
                    ph.op("dve", lambda e, pqk_t=pqk_t, kt=kt: e.tensor_copy(out=self.kT2[:, :, kt * 128:(kt + 1) * 128], in_=pqk_t[:, 4:6, :]),
                          reads=[Bpqk], writes=[Bpqk])
                if not is_ctx:
                    tok0 = 15 + (st - 1) * 512
                    for c in range(4):
                        pu_t, Bpu = pu.next()
                        pg_t, Bpg = pg.next()
                        sg_t, Bsg = sig.next()
                        for k in range(8):
                            ph.op("pe", lambda e, pu_t=pu_t, hT=hT, k=k, c=c: e.matmul(pu_t[:], lhsT=w_in_b[:, k, 768 + c * 128:768 + (c + 1) * 128], rhs=hT[:, k, :],
                                                                                     start=(k == 0), stop=(k == 7)), reads=[BhT, BhTa, Bw], writes=[Bpu], signal=(k == 7))
                        for k in range(8):
                            ph.op("pe", lambda e, pg_t=pg_t, hT=hT, k=k, c=c: e.matmul(pg_t[:], lhsT=w_in_b[:, k, 1280 + c * 128:1280 + (c + 1) * 128], rhs=hT[:, k, :],
                                                                                     start=(k == 0), stop=(k == 7)), reads=[BhT, BhTa, Bw], writes=[Bpg], signal=(k == 7))
                        ph.op("act", lambda e, sg_t=sg_t, pg_t=pg_t: e.activation(out=sg_t[:], in_=pg_t[:], func=AF.Sigmoid), reads=[Bpg], writes=[Bsg])
                        ph.op("dve", lambda e, sg_t=sg_t, pu_t=pu_t, c=c, tok0=tok0: e.tensor_tensor(out=self.gT[:, c, tok0:tok0 + 512], in0=pu_t[:], in1=sg_t[:], op=ALU.mult),
                              reads=[Bpu, Bsg], writes=[])
            for nm, t in (("qT", self.qT), ("kT2", self.kT2), ("Vaug", self.Vaug), ("gT", self.gT)):
                if nm in self.dbg:
                    ph.dma("sp", lambda e, nm=nm, t=t: e.dma_start(out=self.dbg[nm], in_=t[:]), reads=[Bout])
            ph.finish()

    def attn(self, es_out):
        nc = self.nc
        self.attnT = self.qT
        NJ2 = NKT // 2
        with contextlib.ExitStack() as es:
            kTz = self.sb(es, "kTz", [128, 2, 2, NKT * 128], BF16)
            PT = self.rb(es, "PT", [128, 1024], BF16, 3)
            at2 = self.rb(es, "at2", [128, 4, 128], BF16, 2)
            rec = self.rb(es, "rec", [128, 4], F32, 2)
            pS = self.rps(es, "pS", [128, 1024], F32, 2)
            pO = self.rps(es, "pO", [128, 512], F32, 2)
            pA = self.rps(es, "pA", [128, 8, 128], BF16, 1)
            ph = Phase(nc, "attn")
            Bkz = Buf("kz")
            ph.op("dve", lambda e: e.memset(kTz[:], 0.0), writes=[Bkz])
            for g in range(2):
                ph.op("dve", lambda e, g=g: e.tensor_copy(out=kTz[0:64, g, 0, :], in_=self.kT2[0:64, g, :]), reads=[Bkz], writes=[Bkz])
                ph.op("dve", lambda e, g=g: e.tensor_copy(out=kTz[64:128, g, 1, :], in_=self.kT2[64:128, g, :]), reads=[Bkz], writes=[Bkz])
            for pair in range(4):
                g = pair // 2
                for Q in range(8):
                    at_t, Bat = at2.next()
                    for hl in range(2):
                        pO_t, BpO = pO.next()
                        pOv = pO_t[:, 0:260].rearrange("p (q d) -> p q d", d=65)

                        def emit_st(jj, hl=hl, pair=pair, Q=Q, g=g):
                            pS_t, BpS = pS.next()
                            for u in range(2):
                                j = 2 * jj + u
                                ph.op("pe", lambda e, pS_t=pS_t, j=j, u=u: e.matmul(
                                    pS_t[:, u * 512:(u + 1) * 512], lhsT=kTz[:, g, hl, j * 128:(j + 1) * 128],
                                    rhs=self.qT[:, pair, Q * 512:(Q + 1) * 512], start=True, stop=True), reads=[Bkz], writes=[BpS], signal=(u == 1))
                            return pS_t, BpS
                        nxt = emit_st(0)
                        for jj in range(NJ2):
                            pS_t, BpS = nxt
                            if jj + 1 < NJ2:
                                nxt = emit_st(jj + 1)
                            P_t, BP = PT.next()
                            ph.op("act", lambda e, P_t=P_t, pS_t=pS_t: e.activation(out=P_t[:], in_=pS_t[:], func=AF.Exp, scale=0.125, bias=self.negC[:, 0:1]),
                                  reads=[BpS], writes=[BP])
                            for u in range(2):
                                j = 2 * jj + u
                                for qt in range(4):
                                    ph.op("pe", lambda e, pOv=pOv, P_t=P_t, qt=qt, j=j, u=u, g=g: e.matmul(
                                        pOv[:, qt, :], lhsT=P_t[:, u * 512 + qt * 128:u * 512 + (qt + 1) * 128], rhs=self.Vaug[:, j, g, :],
                                        start=(j == 0 and qt == 0), stop=(j == NKT - 1), skip_group_check=True),
                                        reads=[BP], writes=[BpO], signal=(qt == 3 and (j == NKT - 1)))
                        rc, Brc = rec.next()
                        ph.op("dve", lambda e, rc=rc, pOv=pOv: e.reciprocal(out=rc[:], in_=pOv[:, :, 64]), reads=[BpO], writes=[Brc])
                        ph.op("dve", lambda e, rc=rc, pOv=pOv, at_t=at_t, hl=hl: e.tensor_tensor(
                            out=at_t[:, :, hl * 64:(hl + 1) * 64], in0=pOv[:, :, 0:64], in1=rc[:].unsqueeze(2).broadcast_to([128, 4, 64]), op=ALU.mult),
                            reads=[BpO, Brc], writes=[Bat])
                    pA_t, BpA = pA.next()
                    for qt in range(4):
                        ph.op("pe", lambda e, pA_t=pA_t, at_t=at_t, qt=qt: e.transpose(out=pA_t[:, qt, :], in_=at_t[:, qt, :], identity=self.ident_b[:]),
                              reads=[Bat], writes=[BpA], signal=(qt == 3))
                    ph.op("dve", lambda e, pA_t=pA_t, pair=pair, Q=Q: e.tensor_copy(
                        out=self.attnT[:, pair, Q * 512:(Q + 1) * 512].rearrange("p (q t) -> p q t", t=128), in_=pA_t[:, 0:4, :]),
                        reads=[BpA], writes=[])
            ph.finish()

    def conv(self, es_out):
        nc = self.nc
        self.convT = self.sb(es_out, "convT", [128, 4, S], BF16)
        with contextlib.ExitStack() as es:
            dg = self.sb(es, "dg", [128, 31, 4, 128], BF16)
            cw = self.sb(es, "cw", [128, 4, 31], F32)
            cv = self.sb(es, "cv", [128, 3, 4], F32)
            yf = self.rb(es, "yf", [128, 4, 512], F32, 2)
            ybf = self.rb(es, "ybf", [128, 4, 512], BF16, 2)
            ysq = self.rb(es, "ysq", [128, 4, 512], BF16, 2)
            mean = self.rb(es, "mean", [128, 512], F32, 2)
            msq = self.rb(es, "msq", [128, 512], F32, 1)
            rstd = self.rb(es, "rstd", [128, 512], F32, 2)
            zt = self.rb(es, "zt", [128, 512], F32, 2)
            pc = self.rps(es, "pc", [128, 512], F32, 3)
            pS1 = self.rps(es, "pS1", [128, 512], F32, 1)
            pS2 = self.rps(es, "pS2", [128, 512], F32, 1)
            ph = Phase(nc, "conv")
            Bcw, Bcv, Bdg, Bout = [Buf(n) for n in "cw cv dg out".split()]
            ph.dma("sp", lambda e: e.dma_start(out=cw[:], in_=self.conv_wT), writes=[Bcw])
            ph.dma("sp", lambda e: e.dma_start(out=cv[:], in_=self.conv_vec), writes=[Bcv])
            for j in range(31):
                for c in range(4):
                    last = (j == 30 and c == 3)
                    ph.op("dve", lambda e, j=j, c=c: e.tensor_scalar(out=dg[:, j, c, :], in0=self.ident_b[:], scalar1=cw[:, c, j:j + 1], scalar2=None, op0=ALU.mult),
                          reads=[Bcw], writes=([Bdg] if last else []), signal=last)
            for tc in range(8):
                yf_t, Byf = yf.next()
                yb_t, Byb = ybf.next()
                ys_t, Bys = ysq.next()
                for c in range(4):
                    pc_t, Bpc = pc.next()
                    for j in range(31):
                        ph.op("pe", lambda e, pc_t=pc_t, j=j, c=c, tc=tc: e.matmul(pc_t[:], lhsT=dg[:, j, c, :], rhs=self.gT[:, c, tc * 512 + j:tc * 512 + j + 512],
                                                                                  start=(j == 0), stop=(j == 30)), reads=[Bdg], writes=[Bpc], signal=(j == 30))
                    ph.op("act", lambda e, pc_t=pc_t, yf_t=yf_t, c=c: e.activation(out=yf_t[:, c, :], in_=pc_t[:], func=AF.Identity, bias=cv[:, 0, c:c + 1]),
                          reads=[Bpc, Bcv], writes=[Byf])
                    ph.op("act", lambda e, pc_t=pc_t, ys_t=ys_t, c=c: e.activation(out=ys_t[:, c, :], in_=pc_t[:], func=AF.Square, bias=cv[:, 0, c:c + 1]),
                          reads=[Bpc, Bcv], writes=[Bys])
                    ph.op("dve", lambda e, yf_t=yf_t, yb_t=yb_t, c=c: e.tensor_copy(out=yb_t[:, c, :], in_=yf_t[:, c, :]), reads=[Byf], writes=[Byb])
                p1, Bp1 = pS1.next()
                p2, Bp2 = pS2.next()
                for c in range(4):
                    ph.op("pe", lambda e, p1=p1, yb_t=yb_t, c=c: e.matmul(p1[:], lhsT=self.ones_b[:], rhs=yb_t[:, c, :], start=(c == 0), stop=(c == 3)),
                          reads=[Byb], writes=[Bp1], signal=(c == 3))
                for c in range(4):
                    ph.op("pe", lambda e, p2=p2, ys_t=ys_t, c=c: e.matmul(p2[:], lhsT=self.ones_b[:], rhs=ys_t[:, c, :], start=(c == 0), stop=(c == 3)),
                          reads=[Bys], writes=[Bp2], signal=(c == 3))
                mn, Bmn = mean.next()
                ms, Bms = msq.next()
                rs, Brs = rstd.next()
                ph.op("act", lambda e, mn=mn, p1=p1: e.activation(out=mn[:], in_=p1[:], func=AF.Copy, scale=1.0 / 512), reads=[Bp1], writes=[Bmn])
                ph.op("dve", lambda e, mn=mn, ms=ms: e.tensor_tensor(out=ms[:], in0=mn[:], in1=mn[:], op=ALU.mult), reads=[Bmn], writes=[Bms])
                ph.op("dve", lambda e, rs=rs, p2=p2, ms=ms: e.scalar_tensor_tensor(out=rs[:], in0=p2[:], scalar=1.0 / 512, in1=ms[:], op0=ALU.mult, op1=ALU.subtract),
                      reads=[Bp2, Bms], writes=[Brs])
                ph.op("dve", lambda e, rs=rs: e.tensor_scalar(out=rs[:], in0=rs[:], scalar1=EPS, scalar2=None, op0=ALU.add), reads=[Brs], writes=[Brs])
                ph.op("act", lambda e, rs=rs: e.activation(out=rs[:], in_=rs[:], func=AF.Sqrt), reads=[Brs], writes=[Brs])
                ph.op("dve", lambda e, rs=rs: e.reciprocal(out=rs[:], in_=rs[:]), reads=[Brs], writes=[Brs])
                for c in range(4):
                    z_t, Bz = zt.next()
                    ph.op("dve", lambda e, z_t=z_t, yf_t=yf_t, mn=mn, c=c: e.tensor_tensor(out=z_t[:], in0=yf_t[:, c, :], in1=mn[:], op=ALU.subtract), reads=[Byf, Bmn], writes=[Bz])
                    ph.op("dve", lambda e, z_t=z_t, rs=rs: e.tensor_tensor(out=z_t[:], in0=z_t[:], in1=rs[:], op=ALU.mult), reads=[Bz, Brs], writes=[Bz])
                    ph.op("act", lambda e, z_t=z_t, c=c, tc=tc: e.activation(out=self.convT[:, c, tc * 512:(tc + 1) * 512], in_=z_t[:], func=AF.Silu,
                                                                           scale=cv[:, 1, c:c + 1], bias=cv[:, 2, c:c + 1]), reads=[Bz, Bcv], writes=[])
            if "convT" in self.dbg:
                ph.dma("sp", lambda e: e.dma_start(out=self.dbg["convT"], in_=self.convT[:]), reads=[Bout])
            ph.finish()

    def tail_alloc(self, es, deep=False):
        T = {}
        T["hm"] = self.rb(es, "hm", [128, D], F32, 2)
        T["tmp"] = self.rb(es, "tmp", [128, D], F32, 2 if deep else 1)
        T["junk"] = self.sb(es, "junk2", [128, D], BF16)
        T["ss"] = self.rb(es, "ss2", [128, 4], F32, 2)
        T["aug"] = self.rb(es, "aug", [128, RW], BF16, 2)
        T["h2T"] = self.rb(es, "h2T", [128, 8, 128], BF16, 2)
        T["ex"] = self.rb(es, "ex", [128, NE], F32, 2)
        T["pT2"] = self.rps(es, "pT2", [128, 8, 128], BF16, 2 if deep else 1)
        T["pl"] = self.rps(es, "pl", [128, 512], F32, 2 if deep else 1)
        T["Bjunk"] = Buf("junk2")
        T["Baff"] = Buf("aff")
        return T

    def tail_tile(self, ph, T, i, po_list, x_t, Bx, hdst):
        hm, Bhm = T["hm"].next()
        tmp, Btmp = T["tmp"].next()
        ss, Bss = T["ss"].next()
        aug, Baug = T["aug"].next()
        h2T, Bh2T = T["h2T"].next()
        ex, Bex = T["ex"].next()
        pT2, BpT2 = T["pT2"].next()
        pl, Bpl = T["pl"].next()
        for n, (po, Bpo) in enumerate(po_list):
            sl = slice(n * 512, (n + 1) * 512)
            ph.op("dve", lambda e, hm=hm, po=po, sl=sl: e.tensor_tensor(out=hm[:, sl], in0=po, in1=self.G1bc[:, sl], op=ALU.mult), reads=[Bpo], writes=[Bhm])
            ph.op("dve", lambda e, hm=hm, x_t=x_t, sl=sl: e.tensor_tensor(out=hm[:, sl], in0=hm[:, sl], in1=x_t[:, sl], op=ALU.add), reads=[Bhm, Bx], writes=[Bhm])
        ph.dma("sp", lambda e, hm=hm, i=i: e.dma_start(out=hdst[i * 128:(i + 1) * 128, :], in_=hm[:]), reads=[Bhm])
        ph.op("act", lambda e, hm=hm, ss=ss: e.activation(out=T["junk"][:], in_=hm[:], func=AF.Square, accum_out=ss[:, 0:1]), reads=[Bhm], writes=[T["Bjunk"], Bss])
        ph.op("dve", lambda e, ss=ss: e.tensor_scalar(out=ss[:, 0:1], in0=ss[:, 0:1], scalar1=1.0 / D, scalar2=EPS, op0=ALU.mult, op1=ALU.add), reads=[Bss], writes=[Bss])
        ph.op("pool", lambda e, ss=ss: e.tensor_tensor(out=ss[:, 0:1], in0=ss[:, 0:1], in1=self.mhalf[:, 0:1], op=ALU.pow), reads=[Bss], writes=[Bss])
        ph.op("dve", lambda e, tmp=tmp, hm=hm, ss=ss: e.scalar_tensor_tensor(out=tmp[:], in0=hm[:], scalar=ss[:, 0:1], in1=self.A2bc[:], op0=ALU.mult, op1=ALU.mult),
              reads=[Bhm, Bss], writes=[Btmp])
        ph.op("dve", lambda e, tmp=tmp, aug=aug: e.tensor_tensor(out=aug[:, 0:D], in0=tmp[:], in1=self.B2bc[:], op=ALU.add), reads=[Btmp], writes=[Baug])
        for k in range(8):
            ph.op("pe", lambda e, pT2=pT2, aug=aug, k=k: e.transpose(out=pT2[:, k, :], in_=aug[:, k * 128:(k + 1) * 128], identity=self.ident_b[:]),
                  reads=[Baug], writes=[BpT2], signal=(k == 7))
        ph.op("act", lambda e, h2T=h2T, pT2=pT2: e.activation(out=h2T[:], in_=pT2[:], func=AF.Copy), reads=[BpT2], writes=[Bh2T])
        for k in range(8):
            ph.op("pe", lambda e, pl=pl, h2T=h2T, k=k: e.matmul(pl[:, 0:NE], lhsT=h2T[:, k, :], rhs=self.wr_b[:, k, :], start=(k == 0), stop=(k == 7)),
                  reads=[Bh2T], writes=[Bpl], signal=(k == 7))
        ph.op("dve", lambda e, ss=ss, pl=pl: e.tensor_reduce(out=ss[:, 1:2], in_=pl[:, 0:NE], axis=AX.X, op=ALU.max), reads=[Bpl], writes=[Bss])
        ph.op("dve", lambda e, ss=ss: e.tensor_scalar(out=ss[:, 1:2], in0=ss[:, 1:2], scalar1=-1.0, scalar2=None, op0=ALU.mult), reads=[Bss], writes=[Bss])
        ph.op("act", lambda e, ex=ex, pl=pl, ss=ss: e.activation(out=ex[:], in_=pl[:, 0:NE], func=AF.Exp, bias=ss[:, 1:2], accum_out=ss[:, 2:3]),
              reads=[Bpl, Bss], writes=[Bex, Bss])
        ph.op("dve", lambda e, ss=ss: e.reciprocal(out=ss[:, 3:4], in_=ss[:, 2:3]), reads=[Bss], writes=[Bss])
        ph.op("dve", lambda e, ex=ex, ss=ss, i=i: e.tensor_scalar(out=self.affTM[:, i, :], in0=ex[:], scalar1=ss[:, 3:4], scalar2=None, op0=ALU.mult),
              reads=[Bex, Bss], writes=[T["Baff"]])
        ph.op("dve", lambda e, aug=aug, i=i: e.tensor_copy(out=aug[:, D:D + 16], in_=self.affTM[:, i, :]), reads=[T["Baff"]], writes=[Baug])
        ph.op("dve", lambda e, aug=aug, ex=ex, i=i: e.tensor_tensor(out=ex[:], in0=self.affTM[:, i, :], in1=aug[:, D:D + 16], op=ALU.subtract), reads=[T["Baff"], Baug], writes=[Bex])
        ph.op("dve", lambda e, aug=aug, ex=ex: e.tensor_copy(out=aug[:, D + 16:D + 32], in_=ex[:]), reads=[Bex], writes=[Baug])
        ph.op("dve", lambda e, aug=aug, ex=ex: e.tensor_tensor(out=ex[:], in0=ex[:], in1=aug[:, D + 16:D + 32], op=ALU.subtract), reads=[Bex, Baug], writes=[Bex])
        ph.op("dve", lambda e, aug=aug, ex=ex: e.tensor_copy(out=aug[:, D + 32:D + 48], in_=ex[:]), reads=[Bex], writes=[Baug])
        ph.dma("sp", lambda e, aug=aug, i=i: e.dma_start(out=self.h2aug[i * 128:(i + 1) * 128, :], in_=aug[:]), reads=[Baug])

    def outproj0(self):
        nc = self.nc
        with contextlib.ExitStack() as es:
            w_out_b = self.sb(es, "w_out_b", [128, 8, D], BF16)
            xt = self.rb(es, "xt", [128, D], F32, 3)
            T = self.tail_alloc(es, deep=True)
            po = self.rps(es, "po", [128, 512], F32, 4)
            ph = Phase(nc, "outproj0")
            Bw = Buf("w")
            ph.dma("pool", lambda e: e.dma_start(out=w_out_b[:], in_=self.ev_w_out.rearrange("(k p) n -> p k n", p=128)), writes=[Bw])
            loads = {}

            def issue_load(i):
                x_t, Bx = xt.next()
                ph.dma("sp", lambda e, x_t=x_t, i=i: e.dma_start(out=x_t[:], in_=self.x[i * 128:(i + 1) * 128, :]), writes=[Bx])
                loads[i] = (x_t, Bx)
            issue_load(0)
            issue_load(1)
            for i in range(NT):
                if i + 2 < NT:
                    issue_load(i + 2)
                x_t, Bx = loads.pop(i)
                pol = []
                for n in range(2):
                    po_t, Bpo = po.next()
                    for fc in range(8):
                        src = self.attnT[:, fc, i * 128:(i + 1) * 128] if fc < 4 else self.convT[:, fc - 4, i * 128:(i + 1) * 128]
                        ph.op("pe", lambda e, po_t=po_t, src=src, fc=fc, n=n: e.matmul(po_t[:], lhsT=src, rhs=w_out_b[:, fc, n * 512:(n + 1) * 512],
                                                                                     start=(fc == 0), stop=(fc == 7)), reads=[Bw], writes=[Bpo], signal=(fc == 7))
                    pol.append((po_t[:], Bpo))
                self.tail_tile(ph, T, i, pol, x_t, Bx, self.hbuf)
            if "aff" in self.dbg:
                ph.dma("sp", lambda e: e.dma_start(out=self.dbg["aff"], in_=self.affTM[:]), reads=[T["Baff"]])
            ph.finish()
            self.dump_tail()

    def dump_h2(self):
        if "h1" in self.dbg:
            ph = Phase(self.nc, "dumph1")
            B = Buf("d")
            ph.dma("sp", lambda e: e.dma_start(out=self.dbg["h1"], in_=self.hbuf2), reads=[B], writes=[B])
            ph.op("dve", lambda e: e.memset(self.zeros[:, 0:1], 0.0), reads=[B], writes=[B])
            ph.finish()

    def dump_tail(self):
        ph = Phase(self.nc, "dump")
        B = Buf("dump")
        for nm, src in (("hmid", self.hbuf), ("h2aug", self.h2aug)):
            if nm in self.dbg:
                ph.dma("sp", lambda e, nm=nm, src=src: e.dma_start(out=self.dbg[nm], in_=src), reads=[B], writes=[B])
        ph.op("dve", lambda e: e.memset(self.zeros[:, 0:1], 0.0), reads=[B], writes=[B])
        ph.finish()

    def route(self, es_out, l):
        nc = self.nc
        self.Wg = self.rb(es_out, "Wg", [128, 8, D], BF16, 2)
        self.Wu = self.rb(es_out, "Wu", [128, 8, D], BF16, 2)
        self.Wd = self.rb(es_out, "Wd", [128, 8, D], BF16, 2)
        self.slotm = self.sb(es_out, "slotm", [128, NT * NE], F32)
        with contextlib.ExitStack() as es:
            affP = self.sb(es, "affP", [128, 512], F32)
            junk = self.sb(es, "junkr", [128, 512], BF16)
            Gm_s = self.sb(es, "Gm_s", [128, 128], F32)
            sc = self.sb(es, "sc", [128, 8], F32)
            dth = self.sb(es, "dth", [16, 16], F32)
            thr_bc = self.sb(es, "thr_bc", [128, NE], F32)
            mask_f = self.sb(es, "mask_f", [128, NT * NE], F32)
            mask_b = self.sb(es, "mask_b", [128, NT * NE], BF16)
            within = self.sb(es, "within", [128, NT * NE], F32)
            cA = self.sb(es, "cA", [128, NT * NE], F32)
            cB = self.sb(es, "cB", [128, NT * NE], F32)
            tot = self.sb(es, "tot", [128, NT * NE], F32)
            pa = self.rps(es, "pa", [128, 512], F32, 2)
            ph = Phase(nc, f"route{l}")
            self.pre_w = []
            for ring, src in ((self.Wg, self.w_gate), (self.Wu, self.w_up), (self.Wd, self.w_down)):
                wt, Bw = ring.next()
                ph.dma("pool", lambda e, wt=wt, src=src: e.dma_start(out=wt[:], in_=src[l, 0].rearrange("(k p) n -> p k n", p=128)), writes=[Bw])
                self.pre_w.append((wt, Bw))
            B = Buf("r")
            W = dict(reads=[B], writes=[B])
            BaT = Buf("affT")
            BG = Buf("Gm")
            ph.dma("sp", lambda e: e.dma_start(out=Gm_s[:], in_=self.Gm), writes=[BG])
            pt, Bp = pa.next()
            for g in range(4):
                ph.op("pe", lambda e, pt=pt, g=g: e.transpose(out=pt[:, g * 128:(g + 1) * 128], in_=self.affTM[:, g * 8:(g + 1) * 8, :].rearrange("p a b -> p (a b)"),
                                                              identity=self.ident_f[:]), writes=[Bp], signal=(g == 3))
            ph.op("act", lambda e, pt=pt: e.activation(out=affP[:], in_=pt[:], func=AF.Copy), reads=[Bp], writes=[BaT])
            lo, hi, mid, cnt, pred, dd = [sc[:, i:i + 1] for i in range(6)]
            ph.op("dve", lambda e: e.memset(sc[:], 0.0), reads=[BaT], writes=[B])
            ph.op("dve", lambda e: e.memset(hi, 1.5), **W)
            for it in range(30):
                ph.op("dve", lambda e: e.tensor_scalar(out=mid, in0=lo, scalar1=hi, scalar2=0.5, op0=ALU.add, op1=ALU.mult), **W)
                ph.op("dve", lambda e: e.tensor_scalar(out=junk[:], in0=affP[:], scalar1=mid, scalar2=0.0, op0=ALU.is_ge, op1=ALU.add, accum_out=cnt), **W)
                pc_t, Bpc = pa.next()
                ph.op("pe", lambda e, pc_t=pc_t: e.matmul(pc_t[:, 0:1], lhsT=Gm_s[:], rhs=cnt, start=True, stop=True), reads=[B, BG], writes=[Bpc])
                ph.op("dve", lambda e, pc_t=pc_t: e.tensor_scalar(out=pred, in0=pc_t[:, 0:1], scalar1=float(CAP), scalar2=None, op0=ALU.is_ge), reads=[Bpc, B], writes=[B, Bpc])
                ph.op("dve", lambda e: e.tensor_tensor(out=dd, in0=mid, in1=lo, op=ALU.subtract), **W)
                ph.op("dve", lambda e: e.scalar_tensor_tensor(out=lo, in0=dd, scalar=pred, in1=lo, op0=ALU.mult, op1=ALU.add), **W)
                ph.op("dve", lambda e: e.tensor_tensor(out=dd, in0=hi, in1=mid, op=ALU.subtract), **W)
                ph.op("dve", lambda e: e.scalar_tensor_tensor(out=hi, in0=dd, scalar=pred, in1=mid, op0=ALU.mult, op1=ALU.add), **W)
            lo16 = sc[0:16, 0:1]
            ph.op("dve", lambda e: e.tensor_scalar(out=dth[:], in0=self.ident_f[0:16, 0:16], scalar1=lo16, scalar2=None, op0=ALU.mult), **W)
            pt, Bp = pa.next()
            ph.op("pe", lambda e, pt=pt: e.matmul(pt[:, 0:NE], lhsT=self.ones_f[0:16, :], rhs=dth[:], start=True, stop=True), reads=[B], writes=[Bp])
            ph.op("dve", lambda e, pt=pt: e.tensor_copy(out=thr_bc[:], in_=pt[:, 0:NE]), reads=[Bp], writes=[B])
            ph.op("dve", lambda e: e.tensor_tensor(out=v3(mask_f[:], NE), in0=self.affTM[:], in1=thr_bc[:].unsqueeze(1).broadcast_to([128, NT, NE]), op=ALU.is_ge), **W)
            ph.op("dve", lambda e: e.tensor_copy(out=mask_b[:], in_=mask_f[:]), **W)
            p1, Bp1 = pa.next()
            ph.op("pe", lambda e, p1=p1: e.matmul(p1[:], lhsT=self.ustr_b[:], rhs=mask_b[:], start=True, stop=True), reads=[B], writes=[Bp1])
            ph.op("dve", lambda e, p1=p1: e.tensor_copy(out=within[:], in_=p1[:]), reads=[Bp1], writes=[B])
            p2, Bp2 = pa.next()
            ph.op("pe", lambda e, p2=p2: e.matmul(p2[:], lhsT=self.ones_b[:], rhs=mask_b[:], start=True, stop=True), reads=[B], writes=[Bp2])
            ph.op("dve", lambda e, p2=p2: e.tensor_copy(out=tot[:], in_=p2[:]), reads=[Bp2], writes=[B])
            ph.op("dve", lambda e: e.tensor_copy(out=cA[:], in_=tot[:]), **W)
            src, dst = cA, cB
            for sft in (1, 2, 4, 8, 16):
                w = sft * NE
                ph.op("dve", lambda e, src=src, dst=dst, w=w: e.tensor_copy(out=dst[:, 0:w], in_=src[:, 0:w]), **W)
                ph.op("dve", lambda e, src=src, dst=dst, w=w: e.tensor_tensor(out=dst[:, w:], in0=src[:, w:], in1=src[:, 0:NT * NE - w], op=ALU.add), **W)
                src, dst = dst, src
            ph.op("dve", lambda e, src=src: e.tensor_tensor(out=src[:], in0=src[:], in1=tot[:], op=ALU.subtract), **W)
            ph.op("dve", lambda e, src=src: e.tensor_tensor(out=within[:], in0=within[:], in1=src[:], op=ALU.add), **W)
            ph.op("dve", lambda e: e.scalar_tensor_tensor(out=within[:], in0=within[:], scalar=1.0, in1=mask_f[:], op0=ALU.add, op1=ALU.mult), **W)
            ph.op("dve", lambda e: e.tensor_scalar(out=self.slotm[:], in0=within[:], scalar1=-1.0, scalar2=None, op0=ALU.add), **W)
            if "slotm" in self.dbg:
                ph.dma("sp", lambda e: e.dma_start(out=self.dbg["slotm"], in_=self.slotm[:]), **W)
            if "thr" in self.dbg:
                ph.dma("sp", lambda e: e.dma_start(out=self.dbg["thr"], in_=sc[:]), **W)
            ph.finish()

    def moe(self, l):
        nc = self.nc
        with contextlib.ExitStack() as es:
            Wg, Wu, Wd = self.Wg, self.Wu, self.Wd
            oh = self.rb(es, "oh", [128, 512], BF16, 3)
            idxf = self.rb(es, "idxf", [128, 4], F32, 2)
            idx8 = self.rb(es, "idx8", [128, 8], F32, 2)
            idxi = self.rb(es, "idxi", [128, 4], I32, 2)
            xs = self.rb(es, "xs", [128, 4, RW], BF16, 2)
            gs = self.rb(es, "gs", [128, 4], F32, 2)
            xsT = self.rb(es, "xsT", [128, 8, 512], BF16, 2)
            hidT = self.rb(es, "hidT", [128, 8, 512], BF16, 2)
            sg = self.rb(es, "sg", [128, 512], F32, 2)
            ysb = self.rb(es, "ysb", [128, D], F32, 3)
            pG = self.rps(es, "pG", [128, 512], F32, 2)
            pU = self.rps(es, "pU", [128, 512], F32, 2)
            pY = self.rps(es, "pY", [128, 512], F32, 2)
            pTP = self.rps(es, "pTP", [128, 8, 128], BF16, 1)
            pI = self.rps(es, "pI", [128, 512], F32, 1)
            ph = Phase(nc, f"moe{l}")
            Bacc = Buf("acc")
            Bz = [Buf(f"z{i}") for i in range(NT)]
            for i in range(NT):
                ph.dma("sp", lambda e, i=i: e.dma_start(out=self.acc[i * 128:(i + 1) * 128, :], in_=self.zeros[:, 0:D]), writes=[Bz[i]])
            first_scatter = [True]

            def load_w(e_):
                r = []
                for ring, src in ((Wg, self.w_gate), (Wu, self.w_up), (Wd, self.w_down)):
                    wt, Bw = ring.next()
                    ph.dma("pool", lambda e, wt=wt, src=src, e_=e_: e.dma_start(out=wt[:], in_=src[l, e_].rearrange("(k p) n -> p k n", p=128)), writes=[Bw])
                    r.append((wt, Bw))
                return r

            def build_idx(e_, res):
                pI_t, BpI = pI.next()
                pIv = pI_t[:, 0:8].rearrange("p (c t) -> p c t", t=2)
                for i in range(NT):
                    oh_t, Boh = oh.next()
                    ph.op("dve", lambda e, oh_t=oh_t, i=i, e_=e_: e.tensor_scalar(out=oh_t[:], in0=self.iota512[:], scalar1=self.slotm[:, i * NE + e_:i * NE + e_ + 1],
                                                                                 scalar2=None, op0=ALU.is_equal), writes=[Boh])
                    for c in range(4):
                        ph.op("pe", lambda e, pIv=pIv, oh_t=oh_t, i=i, c=c: e.matmul(pIv[:, c, :], lhsT=oh_t[:, c * 128:(c + 1) * 128], rhs=self.tokval[:, i, :],
                                                                                   start=(i == 0 and c == 0), stop=(i == NT - 1), skip_group_check=True),
                              reads=[Boh], writes=[BpI], signal=(c == 3))
                    yield
                xf, Bxf = idxf.next()
                xi, Bxi = idxi.next()
                x8, Bx8 = idx8.next()
                ph.op("dve", lambda e, x8=x8, pI_t=pI_t: e.tensor_copy(out=x8[:], in_=pI_t[:, 0:8]), reads=[BpI], writes=[Bx8])
                x8v = x8[:].rearrange("p (c t) -> p c t", t=2)
                ph.op("dve", lambda e, xf=xf, x8v=x8v: e.scalar_tensor_tensor(out=xf[:], in0=x8v[:, :, 0], scalar=128.0, in1=x8v[:, :, 1], op0=ALU.mult, op1=ALU.add),
                      reads=[Bx8], writes=[Bxf])
                ph.op("dve", lambda e, xf=xf, xi=xi: e.tensor_copy(out=xi[:], in_=xf[:]), reads=[Bxf], writes=[Bxi])
                xs_t, Bxs = xs.next()
                for c in range(4):
                    ph.dma("pool", lambda e, xs_t=xs_t, xi=xi, c=c: e.indirect_dma_start(
                        out=xs_t[:, c, :], out_offset=None, in_=self.h2aug, in_offset=bass.IndirectOffsetOnAxis(ap=xi[:, c:c + 1], axis=0)),
                        reads=[Bxi], writes=[Bxs])
                res.append((xs_t, Bxs, xi, Bxi))

            def ffn(e_, wts, gath):
                (wg, Bwg), (wu, Bwu), (wd, Bwd) = wts
                xs_t, Bxs, xi, Bxi = gath
                g_t, Bg = gs.next()
                ph.op("dve", lambda e, g_t=g_t, xs_t=xs_t: e.tensor_tensor(out=g_t[:], in0=xs_t[:, :, D + e_], in1=xs_t[:, :, D + 16 + e_], op=ALU.add), reads=[Bxs], writes=[Bg])
                ph.op("dve", lambda e, g_t=g_t, xs_t=xs_t: e.tensor_tensor(out=g_t[:], in0=g_t[:], in1=xs_t[:, :, D + 32 + e_], op=ALU.add), reads=[Bxs, Bg], writes=[Bg])
                xT, BxT = xsT.next()
                for k2 in range(4):
                    tp, Btp = pTP.next()
                    for kk in range(2):
                        k = k2 * 2 + kk
                        for c in range(4):
                            ph.op("pe", lambda e, tp=tp, xs_t=xs_t, kk=kk, c=c, k=k: e.transpose(out=tp[:, kk * 4 + c, :], in_=xs_t[:, c, k * 128:(k + 1) * 128], identity=self.ident_b[:]),
                                  reads=[Bxs], writes=[Btp], signal=(kk == 1 and c == 3))
                    eng = "act" if k2 % 2 == 0 else "dve"
                    if eng == "act":
                        ph.op("act", lambda e, tp=tp, xT=xT, k2=k2: e.activation(out=xT[:, k2 * 2:k2 * 2 + 2, :], in_=tp[:].rearrange("p (k c) t -> p k (c t)", k=2), func=AF.Copy),
                              reads=[Btp], writes=[BxT])
                    else:
                        ph.op("dve", lambda e, tp=tp, xT=xT, k2=k2: e.tensor_copy(out=xT[:, k2 * 2:k2 * 2 + 2, :], in_=tp[:].rearrange("p (k c) t -> p k (c t)", k=2)),
                              reads=[Btp], writes=[BxT])
                    yield
                hT, BhT = hidT.next()
                for f in range(8):
                    pg_t, Bpg = pG.next()
                    pu_t, Bpu = pU.next()
                    for k in range(8):
                        ph.op("pe", lambda e, pg_t=pg_t, wg=wg, xT=xT, k=k, f=f: e.matmul(pg_t[:], lhsT=wg[:, k, f * 128:(f + 1) * 128], rhs=xT[:, k, :], start=(k == 0), stop=(k == 7)),
                              reads=[Bwg, BxT], writes=[Bpg], signal=(k == 7))
                    for k in range(8):
                        ph.op("pe", lambda e, pu_t=pu_t, wu=wu, xT=xT, k=k, f=f: e.matmul(pu_t[:], lhsT=wu[:, k, f * 128:(f + 1) * 128], rhs=xT[:, k, :], start=(k == 0), stop=(k == 7)),
                              reads=[Bwu, BxT], writes=[Bpu], signal=(k == 7))
                    sg_t, Bsg = sg.next()
                    ph.op("act", lambda e, sg_t=sg_t, pg_t=pg_t: e.activation(out=sg_t[:], in_=pg_t[:], func=AF.Silu), reads=[Bpg], writes=[Bsg])
                    ph.op("dve", lambda e, sg_t=sg_t, pu_t=pu_t, hT=hT, f=f: e.tensor_tensor(out=hT[:, f, :], in0=pu_t[:], in1=sg_t[:], op=ALU.mult), reads=[Bpu, Bsg], writes=[BhT])
                    yield
                for c in range(4):
                    y_t, By = ysb.next()
                    for n in range(2):
                        py_t, Bpy = pY.next()
                        for f in range(8):
                            ph.op("pe", lambda e, py_t=py_t, hT=hT, wd=wd, f=f, c=c, n=n: e.matmul(py_t[:], lhsT=hT[:, f, c * 128:(c + 1) * 128], rhs=wd[:, f, n * 512:(n + 1) * 512],
                                                                                               start=(f == 0), stop=(f == 7)), reads=[BhT, Bwd], writes=[Bpy], signal=(f == 7))
                        ph.op("dve", lambda e, y_t=y_t, py_t=py_t, g_t=g_t, c=c, n=n: e.scalar_tensor_tensor(
                            out=y_t[:, n * 512:(n + 1) * 512], in0=py_t[:], scalar=g_t[:, c:c + 1], in1=self.G2bc[:, n * 512:(n + 1) * 512], op0=ALU.mult, op1=ALU.mult),
                            reads=[Bpy, Bg], writes=[By])
                    ph.dma("pool", lambda e, y_t=y_t, xi=xi, c=c: e.indirect_dma_start(
                        out=self.acc, out_offset=bass.IndirectOffsetOnAxis(ap=xi[:, c:c + 1], axis=0), in_=y_t[:], in_offset=None, compute_op=ALU.add),
                        reads=[By, Bxi] + (Bz if first_scatter[0] else []), writes=[Bacc])
                    first_scatter[0] = False
                    yield

            wts = self.pre_w
            r0 = []
            for _ in build_idx(0, r0):
                pass
            gath = r0[0]
            for e_ in range(NE):
                nwts = load_w(e_ + 1) if e_ + 1 < NE else None
                rn = []
                gi = build_idx(e_ + 1, rn) if e_ + 1 < NE else iter(())
                gf = ffn(e_, wts, gath)
                for _ in gi:
                    pass
                for _ in gf:
                    pass
                wts, gath = nwts, (rn[0] if rn else None)
            ph.finish()
            if "acc" in self.dbg:
                ph = Phase(nc, "dumpacc")
                B = Buf("d")
                ph.dma("sp", lambda e: e.dma_start(out=self.dbg["acc"], in_=self.acc), reads=[B], writes=[B])
                ph.op("dve", lambda e: e.memset(self.zeros[:, 0:1], 0.0), reads=[B], writes=[B])
                ph.finish()

    def resid(self, l, hsrc, hdst):
        nc = self.nc
        last = l == 1
        with contextlib.ExitStack() as es:
            hm = self.rb(es, "hmr", [128, D], F32, 3)
            ac = self.rb(es, "acr", [128, D], F32, 3)
            ot = self.rb(es, "otr", [128, D], F32, 2)
            ss = self.rb(es, "ssr", [128, 1], F32, 2)
            junk = self.sb(es, "junkf", [128, D], BF16)
            fg = self.sb(es, "fg", [128, D], F32)
            ph = Phase(nc, f"resid{l}")
            Bfg, Bj = Buf("fg"), Buf("j")
            if last:
                ph.dma("sp", lambda e: e.dma_start(out=fg[:], in_=self.final_g.partition_broadcast(128)), writes=[Bfg])
            loads = {}

            def issue_load(i):
                h_t, Bh = hm.next()
                a_t, Ba = ac.next()
                ph.dma("sp", lambda e, h_t=h_t, i=i: e.dma_start(out=h_t[:], in_=hsrc[i * 128:(i + 1) * 128, :]), writes=[Bh])
                ph.dma("sp", lambda e, a_t=a_t, i=i: e.dma_start(out=a_t[:], in_=self.acc[i * 128:(i + 1) * 128, :]), writes=[Ba])
                loads[i] = (h_t, Bh, a_t, Ba)
            issue_load(0)
            issue_load(1)
            for i in range(NT):
                if i + 2 < NT:
                    issue_load(i + 2)
                h_t, Bh, a_t, Ba = loads.pop(i)
                ph.op("dve", lambda e, a_t=a_t, h_t=h_t: e.tensor_tensor(out=h_t[:], in0=h_t[:], in1=a_t[:], op=ALU.add), reads=[Ba, Bh], writes=[Bh])
                if not last:
                    ph.dma("sp", lambda e, h_t=h_t, i=i: e.dma_start(out=hdst[i * 128:(i + 1) * 128, :], in_=h_t[:]), reads=[Bh])
                else:
                    s_t, Bs = ss.next()
                    o_t, Bo = ot.next()
                    ph.op("act", lambda e, h_t=h_t, s_t=s_t: e.activation(out=junk[:], in_=h_t[:], func=AF.Square, accum_out=s_t[:]), reads=[Bh], writes=[Bj, Bs])
                    ph.op("dve", lambda e, s_t=s_t: e.tensor_scalar(out=s_t[:], in0=s_t[:], scalar1=1.0 / D, scalar2=EPS, op0=ALU.mult, op1=ALU.add), reads=[Bs], writes=[Bs])
                    ph.op("pool", lambda e, s_t=s_t: e.tensor_tensor(out=s_t[:], in0=s_t[:], in1=self.mhalf[:, 0:1], op=ALU.pow), reads=[Bs], writes=[Bs])
                    ph.op("dve", lambda e, o_t=o_t, h_t=h_t, s_t=s_t: e.scalar_tensor_tensor(out=o_t[:], in0=h_t[:], scalar=s_t[:, 0:1], in1=fg[:], op0=ALU.mult, op1=ALU.mult),
                          reads=[Bh, Bs, Bfg], writes=[Bo])
                    ph.dma("sp", lambda e, o_t=o_t, i=i: e.dma_start(out=hdst[i * 128:(i + 1) * 128, :], in_=o_t[:]), reads=[Bo])
            ph.finish()

    def norm1_tile(self, ph, R, src, t, hT, BhT, x_t, Bx):
        BhTa = BhT.__dict__.setdefault("twin", Buf(BhT.name + "a"))
        ss, Bss = R["ssq"].next()
        xn_t, Bxn = R["xn"].next()
        p_t, BpT = R["pT"].next()
        lq = R.get("lq", "sp")
        ph.dma(lq, lambda e: e.dma_start(out=x_t[:], in_=src), writes=[Bx])
        if "fuse" in R:
            asrc, dst = R["fuse"](t)
            a_t, Ba = R["acr"].next()
            ph.dma(lq, lambda e: e.dma_start(out=a_t[:], in_=asrc), writes=[Ba])
            ph.op("dve", lambda e: e.tensor_tensor(out=x_t[:], in0=x_t[:], in1=a_t[:], op=ALU.add), reads=[Ba, Bx], writes=[Bx])
            ph.dma("sp", lambda e: e.dma_start(out=dst, in_=x_t[:]), reads=[Bx])
        ph.op("act", lambda e: e.activation(out=R["junk"][:], in_=x_t[:], func=AF.Square, accum_out=ss[:]), reads=[Bx], writes=[R["Bjunk"], Bss])
        ph.op("dve", lambda e: e.tensor_scalar(out=ss[:], in0=ss[:], scalar1=1.0 / D, scalar2=EPS, op0=ALU.mult, op1=ALU.add), reads=[Bss], writes=[Bss])
        ph.op("pool", lambda e: e.tensor_tensor(out=ss[:], in0=ss[:], in1=self.mhalf[:, 0:1], op=ALU.pow), reads=[Bss], writes=[Bss])
        ph.op("dve", lambda e: e.tensor_scalar(out=xn_t[:], in0=x_t[:], scalar1=ss[:, 0:1], scalar2=None, op0=ALU.mult), reads=[Bx, Bss], writes=[Bxn])
        for k in range(8):
            ph.op("pe", lambda e, k=k: e.transpose(out=p_t[:, k, :], in_=xn_t[:, k * 128:(k + 1) * 128], identity=self.ident_b[:]),
                  reads=[Bxn], writes=[BpT], signal=(k == 7))
        for k in range(8):
            wr = [BhT if t % 2 == 0 else BhTa] if k in (0, 7) else []
            if t % 2 == 0:
                ph.op("dve", lambda e, k=k: e.tensor_scalar(out=hT[:, k, t * 128:(t + 1) * 128], in0=p_t[:, k, :], scalar1=self.A1T[:, k, 0:1],
                                                            scalar2=self.mT[:, k, 0:1], op0=ALU.mult, op1=ALU.add), reads=[BpT], writes=wr)
            else:
                ph.op("act", lambda e, k=k: e.activation(out=hT[:, k, t * 128:(t + 1) * 128], in_=p_t[:, k, :], func=AF.Identity,
                                                         scale=self.A1T[:, k, 0:1], bias=self.mT[:, k, 0:1]), reads=[BpT], writes=wr)

    def l1p1(self, es_out):
        nc = self.nc
        self.cxT = self.sb(es_out, "cxT", [128, 8, S + 2], BF16)
        with contextlib.ExitStack() as es:
            w_cx = self.sb(es, "w_cx", [128, 8, 2 * D], BF16)
            R = {"ssq": self.rb(es, "ssq", [128, 1], F32, 3), "xn": self.rb(es, "xn", [128, D], BF16, 2),
                 "pT": self.rps(es, "pT", [128, 8, 128], BF16, 2), "junk": self.sb(es, "junk", [128, D], BF16), "Bjunk": Buf("junk")}
            xt = self.rb(es, "xt", [128, D], F32, 3)
            hnT = self.rb(es, "hnT", [128, 8, 512], BF16, 2)
            xvs = self.rb(es, "xvs", [128, 512], F32, 2)
            acr = self.rb(es, "acr1", [128, D], F32, 2)
            pC = self.rps(es, "pC", [128, 512], F32, 2)
            pX = self.rps(es, "pX", [128, 512], F32, 2)
            ph = Phase(nc, "l1p1")
            Bw, Bout = Buf("w"), Buf("out")
            ph.dma("pool", lambda e: e.dma_start(out=w_cx[:], in_=self.sc_w_in[:, D:3 * D].rearrange("(k p) n -> p k n", p=128)), writes=[Bw])
            ph.op("pool", lambda e: e.memset(self.cxT[:, :, 0:1], 0.0), writes=[])
            ph.op("pool", lambda e: e.memset(self.cxT[:, :, S + 1:S + 2], 0.0), writes=[])
            for st in range(8):
                hT, BhT = hnT.next()
                R["acr"] = acr
                R["lq"] = "act"
                R["fuse"] = lambda t, st=st: (self.acc[(st * 4 + t) * 128:(st * 4 + t + 1) * 128, :], self.hbuf2[(st * 4 + t) * 128:(st * 4 + t + 1) * 128, :])
                for t in range(4):
                    i = st * 4 + t
                    x_t, Bx = xt.next()
                    self.norm1_tile(ph, R, self.hbuf[i * 128:(i + 1) * 128, :], t, hT, BhT, x_t, Bx)
                for ch in range(8):
                    pc_t, Bpc = pC.next()
                    px_t, Bpx = pX.next()
                    for k in range(8):
                        ph.op("pe", lambda e, pc_t=pc_t, k=k, ch=ch, hT=hT: e.matmul(pc_t[:], lhsT=w_cx[:, k, ch * 128:(ch + 1) * 128], rhs=hT[:, k, :], start=(k == 0), stop=(k == 7)),
                              reads=[Bw, BhT, BhT.twin], writes=[Bpc], signal=(k == 7))
                    for k in range(8):
                        ph.op("pe", lambda e, px_t=px_t, k=k, ch=ch, hT=hT: e.matmul(px_t[:], lhsT=w_cx[:, k, D + ch * 128:D + (ch + 1) * 128], rhs=hT[:, k, :], start=(k == 0), stop=(k == 7)),
                              reads=[Bw, BhT, BhT.twin], writes=[Bpx], signal=(k == 7))
                    xv_t, Bxv = xvs.next()
                    ph.op("act", lambda e, xv_t=xv_t, px_t=px_t: e.activation(out=xv_t[:], in_=px_t[:], func=AF.Copy), reads=[Bpx], writes=[Bxv])
                    ph.op("dve", lambda e, xv_t=xv_t, pc_t=pc_t, ch=ch, st=st: e.tensor_tensor(out=self.cxT[:, ch, 1 + st * 512:1 + (st + 1) * 512], in0=pc_t[:], in1=xv_t[:], op=ALU.mult),
                          reads=[Bpc, Bxv], writes=[])
            ph.finish()

    def l1p2(self):
        nc = self.nc
        with contextlib.ExitStack() as es:
            w_b = self.sb(es, "w_b", [128, 8, D], BF16)
            w_o = self.sb(es, "w_o", [128, 8, D], BF16)
            dg3 = self.sb(es, "dg3", [128, 3, 8, 128], BF16)
            cw3 = self.sb(es, "cw3", [128, 8, 3], F32)
            R = {"ssq": self.rb(es, "ssq", [128, 1], F32, 3), "xn": self.rb(es, "xn", [128, D], BF16, 2),
                 "pT": self.rps(es, "pT", [128, 8, 128], BF16, 1), "junk": self.sb(es, "junk", [128, D], BF16), "Bjunk": Buf("junk")}
            xt = self.rb(es, "xt", [128, D], F32, 5)
            hnT = self.rb(es, "hnT", [128, 8, 512], BF16, 1)
            zT = self.rb(es, "zT", [128, 8, 512], BF16, 1)
            ycs = self.rb(es, "ycs", [128, 512], F32, 2)
            T = self.tail_alloc(es)
            pB = self.rps(es, "pB", [128, 512], F32, 2)
            pYc = self.rps(es, "pYc", [128, 512], F32, 1)
            po = self.rps(es, "po", [128, 512], F32, 2)
            ph = Phase(nc, "l1p2")
            Bw, Bcw, Bdg = Buf("w"), Buf("cw"), Buf("dg")
            ph.dma("pool", lambda e: e.dma_start(out=w_b[:], in_=self.sc_w_in[:, 0:D].rearrange("(k p) n -> p k n", p=128)), writes=[Bw])
            ph.dma("pool", lambda e: e.dma_start(out=w_o[:], in_=self.sc_w_out.rearrange("(k p) n -> p k n", p=128)), writes=[Bw])
            ph.dma("sp", lambda e: e.dma_start(out=cw3[:], in_=self.sc_conv_wT), writes=[Bcw])
            for j in range(3):
                for ch in range(8):
                    last = (j == 2 and ch == 7)
                    ph.op("dve", lambda e, j=j, ch=ch: e.tensor_scalar(out=dg3[:, j, ch, :], in0=self.ident_b[:], scalar1=cw3[:, ch, j:j + 1], scalar2=None, op0=ALU.mult),
                          reads=[Bcw], writes=([Bdg] if last else []), signal=last)
            for st in range(8):
                hT, BhT = hnT.next()
                z_t, Bz = zT.next()
                tiles = []
                R["lq"] = "act"
                for t in range(4):
                    i = st * 4 + t
                    x_t, Bx = xt.next()
                    self.norm1_tile(ph, R, self.hbuf2[i * 128:(i + 1) * 128, :], t, hT, BhT, x_t, Bx)
                    tiles.append((x_t, Bx))
                for ch in range(8):
                    pb_t, Bpb = pB.next()
                    py_t, Bpy = pYc.next()
                    for k in range(8):
                        ph.op("pe", lambda e, pb_t=pb_t, k=k, ch=ch, hT=hT: e.matmul(pb_t[:], lhsT=w_b[:, k, ch * 128:(ch + 1) * 128], rhs=hT[:, k, :], start=(k == 0), stop=(k == 7)),
                              reads=[Bw, BhT, BhT.twin], writes=[Bpb], signal=(k == 7))
                    for j in range(3):
                        ph.op("pe", lambda e, py_t=py_t, j=j, ch=ch, st=st: e.matmul(py_t[:], lhsT=dg3[:, j, ch, :], rhs=self.cxT[:, ch, st * 512 + j:st * 512 + j + 512], start=(j == 0), stop=(j == 2)),
                              reads=[Bdg], writes=[Bpy], signal=(j == 2))
                    yc_t, Byc = ycs.next()
                    ph.op("act", lambda e, yc_t=yc_t, py_t=py_t: e.activation(out=yc_t[:], in_=py_t[:], func=AF.Copy), reads=[Bpy], writes=[Byc])
                    ph.op("dve", lambda e, yc_t=yc_t, pb_t=pb_t, z_t=z_t, ch=ch: e.tensor_tensor(out=z_t[:, ch, :], in0=pb_t[:], in1=yc_t[:], op=ALU.mult), reads=[Bpb, Byc], writes=[Bz])
                for t in range(4):
                    i = st * 4 + t
                    pol = []
                    for n in range(2):
                        po_t, Bpo = po.next()
                        for ch in range(8):
                            ph.op("pe", lambda e, po_t=po_t, z_t=z_t, ch=ch, n=n, t=t: e.matmul(po_t[:], lhsT=z_t[:, ch, t * 128:(t + 1) * 128], rhs=w_o[:, ch, n * 512:(n + 1) * 512],
                                                                                              start=(ch == 0), stop=(ch == 7)), reads=[Bz, Bw], writes=[Bpo], signal=(ch == 7))
                        pol.append((po_t[:], Bpo))
                    x_t, Bx = tiles[t]
                    self.tail_tile(ph, T, i, pol, x_t, Bx, self.hbuf)
            ph.finish()

    def run(self):
        nc = self.nc
        self.declare()
        for nm, shape, dt in (("mrow", [2, 6 * D], F32), ("qT", [128, 4, S], BF16), ("kT2", [128, 2, NKT * 128], BF16),
                              ("Vaug", [128, NKT, 2, 65], BF16), ("gT", [128, 4, S + 30], BF16), ("attnT", [128, 4, S], BF16),
                              ("convT", [128, 4, S], BF16), ("aff", [128, NT, NE], F32), ("hmid", [S, D], F32), ("h2aug", [S, RW], BF16),
                              ("slotm", [128, NT * NE], F32), ("thr", [16, 8], F32), ("acc", [S, D], F32), ("h1", [S, D], F32)):
            self.dbg_out(nm, shape, dt)
        with contextlib.ExitStack() as es0:
            _SEMS[0] = Sems(nc, es0)
            self.consts(es0)
            self.ada(0)
            with contextlib.ExitStack() as es_l0:
                self.l0a(es_l0)
                if self.stop == "l0a":
                    self.es_a.close()
                    self.finish_dummy()
                    return
                self.attn(es_l0)
                self.es_a.close()
                if self.stop == "attn":
                    self.finish_dummy()
                    return
                self.conv(es_l0)
                self.outproj0()
                if self.stop == "outproj0":
                    self.finish_dummy()
                    return
            with contextlib.ExitStack() as es_m:
                self.route(es_m, 0)
                if self.stop == "route0":
                    self.finish_dummy()
                    return
                self.moe(0)
            if self.stop == "moe0":
                self.dump_h2()
                self.finish_dummy()
                return
            self.ada(1)
            with contextlib.ExitStack() as es_l1:
                self.l1p1(es_l1)
                self.l1p2()
            if self.stop == "l1":
                self.dump_tail()
                self.finish_dummy()
                return
            with contextlib.ExitStack() as es_m:
                self.route(es_m, 1)
                self.moe(1)
            self.resid(1, self.hbuf, self.out)

    def finish_dummy(self):
        ph = Phase(self.nc, "fin")
        B = Buf("z")
        ph.dma("sp", lambda e: e.dma_start(out=self.out[0:128, :], in_=self.zeros[:, 0:D]), reads=[B])
        ph.finish()


def build(debug=None, stop=None):
    nc = bass.Bass("TRN2", target_bir_lowering=False)
    k = Kern(nc, debug, stop)
    k.run()
    return nc, k


def rope_tables():
    rows = S // 64
    row = np.repeat(np.arange(rows, dtype=np.float32), 64)
    col = np.tile(np.arange(64, dtype=np.float32), rows)
    inv = (10000.0 ** (-np.arange(0, 32, 2, dtype=np.float32) / 32)).astype(np.float32)
    ang = np.concatenate([row[:, None] * inv, col[:, None] * inv], axis=-1).astype(np.float32)
    cos = np.cos(ang).astype(np.float32).reshape(NT, 128, 32).transpose(1, 0, 2)
    sin = np.sin(ang).astype(np.float32).reshape(NT, 128, 32).transpose(1, 0, 2)
    return np.ascontiguousarray(cos), np.ascontiguousarray(sin)


def prep_inputs(inp):
    f = lambda a: np.ascontiguousarray(np.asarray(a, dtype=np.float32))
    cos, sin = rope_tables()
    shared = {
        "ada_w": f(inp["ada_w"]),
        "ada_b": f(inp["ada_b"]).reshape(2, 1, 6 * D),
        "g1T": f(np.asarray(inp["norm1_g"]).reshape(2, 8, 128).transpose(0, 2, 1)),
        "g2row": f(inp["norm2_g"]).reshape(2, 1, D),
        "ev_w_in": f(inp["ev_w_in"][0]),
        "ev_w_out": f(inp["ev_w_out"][0]),
        "qkg": f(np.concatenate([np.tile(np.asarray(inp["ev_q_g"][0]), 8), np.tile(np.asarray(inp["ev_k_g"][0]), 2)])).reshape(1, 640),
        "qg": f(inp["ev_q_g"][0]).reshape(1, 64),
        "kg": f(inp["ev_k_g"][0]).reshape(1, 64),
        "conv_wT": f(np.asarray(inp["ev_conv_w"][0]).reshape(31, 4, 128).transpose(2, 1, 0)),
        "conv_vec": f(np.stack([np.asarray(inp["ev_conv_b"][0]).reshape(4, 128).T,
                                np.asarray(inp["ev_ln_g"][0]).reshape(4, 128).T,
                                np.asarray(inp["ev_ln_b"][0]).reshape(4, 128).T], axis=1)),
        "rope_cos": cos,
        "rope_sin": sin,
        "sc_w_in": f(inp["sc_w_in"][0]),
        "sc_conv_wT": f(np.asarray(inp["sc_conv_w"][0]).reshape(3, 8, 128).transpose(2, 1, 0)),
        "sc_w_out": f(inp["sc_w_out"][0]),
        "w_r": f(np.asarray(inp["moe_w_r"]).reshape(2, 8, 128, NE).transpose(0, 2, 1, 3)),
        "w_gate": f(inp["moe_w_gate"]),
        "w_up": f(inp["moe_w_up"]),
        "w_down": f(inp["moe_w_down"]),
        "final_g": f(inp["final_g"]).reshape(1, D),
        "Gm": f((np.arange(128)[:, None] % 16) == (np.arange(128)[None, :] % 16)),
    }
    maps = []
    c = np.asarray(inp["c"], dtype=np.float32)
    cc = np.asarray(inp["c_ctx"], dtype=np.float32)
    for core in range(8):
        b = core % 4
        m = dict(shared)
        m["x"] = f(inp["x"][b])
        m["ctx"] = f(inp["ctx"][b])
        m["cT"] = f(np.stack([c[b].reshape(8, 128).T, cc.reshape(8, 128).T], axis=-1))
        maps.append(m)
    return maps


_CACHE = {}


def kernel(**inputs):
    if "nc" not in _CACHE:
        _CACHE["nc"] = build()[0]
    nc = _CACHE["nc"]
    maps = prep_inputs(inputs)
    res = run_bass_kernel_spmd(nc, maps, core_ids=list(range(8)))
    out = np.stack([np.asarray(res.results[b]["out"], dtype=np.float32) for b in range(4)], axis=0)
    return out
```
